# Optimizing a Trainium2 kernel written in Bass

```python
import jax, jax.numpy as jnp
from jax import lax
import numpy as np

D_MODEL = 1024
BATCH = 8
SEQ = 4096
DEPTH = 4

CHUNK = 64
D_MIX = D_MODEL
GM_WIDTH = D_MIX // 2
GM_HEADS = 8
GM_HEAD_DIM = GM_WIDTH // GM_HEADS
GM_BLOCK = 128
SB_WIDTH = D_MIX - GM_WIDTH
SB_HEADS = 8
SB_HEAD_DIM = SB_WIDTH // SB_HEADS
SB_BLOCK = 128
IN_COLS = 2 * GM_WIDTH + 3 * SB_WIDTH
PEER_HEADS = 8
PEER_QUERY_DIM = 256
PEER_HALF = PEER_QUERY_DIM // 2
N_KEYS = 128
N_EXPERTS = N_KEYS * N_KEYS
PEER_TOPK = 16
PEER_BLOCK = 128
ADA_SCALE = 0.5
EPS = 1e-6

kernel_name = "hybrid_gmlp_stickbreaking_peer_adaln"


def rmsnorm(x, g):
    xf = x.astype(jnp.float32)
    y = xf * lax.rsqrt(jnp.mean(xf * xf, axis=-1, keepdims=True) + EPS)
    return (y * g.astype(jnp.float32)).astype(x.dtype)


def spatial_gating(u, v, ws, bs, v_gain):
    B, S, _ = v.shape
    pos_chunk = jnp.arange(GM_BLOCK) // CHUNK
    mask = pos_chunk[None, :] <= pos_chunk[:, None]
    w = jnp.where(mask[None], ws, jnp.zeros_like(ws))
    vh = v.reshape(B, S // GM_BLOCK, GM_BLOCK, GM_HEADS, GM_HEAD_DIM)
    vh = rmsnorm(vh, v_gain.reshape(GM_HEADS, GM_HEAD_DIM))
    z = jnp.einsum('hts,bnshc->bnthc', w, vh) + bs.T[None, None, :, :, None]
    return u * z.reshape(B, S, GM_WIDTH)


def stick_breaking(q, k, v):
    B, S, H, dh = q.shape
    scale = 1.0 / np.sqrt(dh)
    outs = []
    for i in range(S // SB_BLOCK):
        t0, t1 = i * SB_BLOCK, (i + 1) * SB_BLOCK
        qb, kb, vb = q[:, t0:t1], k[:, :t1], v[:, :t1]
        z = jnp.einsum('bthd,bshd->bhts', qb, kb).astype(jnp.float32) * scale
        t_idx = t0 + jnp.arange(SB_BLOCK)[:, None]
        s_idx = jnp.arange(t1)[None, :]
        causal = s_idx < t_idx
        log_beta = jax.nn.log_sigmoid(z)
        log_keep = jnp.where(causal, jax.nn.log_sigmoid(-z), 0.0)
        later = lax.cumsum(log_keep, axis=3, reverse=True) - log_keep
        a = jnp.where(causal, jnp.exp(log_beta + later), 0.0)
        outs.append(jnp.einsum('bhts,bshd->bthd', a.astype(vb.dtype), vb))
    return jnp.concatenate(outs, axis=1)


def mixer(h, w_in, gm_ws, gm_bs, gm_vnorm, out_norm_a, out_norm_b, w_out):
    B, S, _ = h.shape
    proj = h @ w_in
    a_u, a_v, sq, sk, sv = jnp.split(
        proj, [GM_WIDTH, 2 * GM_WIDTH, 2 * GM_WIDTH + SB_WIDTH, 2 * GM_WIDTH + 2 * SB_WIDTH], axis=-1)
    y_a = spatial_gating(jax.nn.gelu(a_u), jax.nn.gelu(a_v), gm_ws, gm_bs, gm_vnorm)
    hs = (B, S, SB_HEADS, SB_HEAD_DIM)
    y_b = stick_breaking(sq.reshape(hs), sk.reshape(hs), sv.reshape(hs)).reshape(B, S, SB_WIDTH)
    y = jnp.concatenate([rmsnorm(y_a, out_norm_a), rmsnorm(y_b, out_norm_b)], axis=-1)
    return y @ w_out


def peer_ffn(h, wq, k1, k2, u_tab, v_tab):
    B, S, D = h.shape
    hb = h.reshape((B * S) // PEER_BLOCK, PEER_BLOCK, D)

    def block(xb):
        q = (xb @ wq).reshape(PEER_BLOCK, PEER_HEADS, 2, PEER_HALF)
        s1 = jnp.einsum('phd,kd->phk', q[:, :, 0], k1).astype(jnp.float32)
        s2 = jnp.einsum('phd,kd->phk', q[:, :, 1], k2).astype(jnp.float32)
        v1, i1 = lax.top_k(s1, PEER_TOPK)
        v2, i2 = lax.top_k(s2, PEER_TOPK)
        cand = (v1[..., :, None] + v2[..., None, :]).reshape(PEER_BLOCK, PEER_HEADS, PEER_TOPK * PEER_TOPK)
        cand_id = (i1[..., :, None] * N_KEYS + i2[..., None, :]).reshape(PEER_BLOCK, PEER_HEADS, PEER_TOPK * PEER_TOPK)
        top_s, top_pos = lax.top_k(cand, PEER_TOPK)
        eid = jnp.take_along_axis(cand_id, top_pos, axis=-1)
        g = jax.nn.softmax(top_s, axis=-1)
        ue = u_tab[eid]
        ve = v_tab[eid]
        act = jax.nn.gelu(jnp.einsum('pd,phkd->phk', xb, ue))
        return jnp.einsum('phk,phkd->pd', g.astype(xb.dtype) * act, ve)

    return lax.map(block, hb).reshape(B, S, D)


def setup_inputs(seed: int = 0) -> dict:
    key = jax.random.key(seed)
    ks = jax.random.split(key, 20)
    f32 = jnp.float32
    nrm = lambda k, shape, s: jax.random.normal(k, shape, f32) * s
    L, D = DEPTH, D_MODEL
    return {
        "x": nrm(ks[0], (BATCH, SEQ, D), 1.0),
        "c": nrm(ks[1], (BATCH, D), 1.0),
        "ada_w": nrm(ks[2], (L, D, 6 * D), ADA_SCALE * D ** -0.5),
        "ada_b": nrm(ks[3], (L, 6 * D), 0.02),
        "norm_mix": 1.0 + nrm(ks[4], (L, D), 0.05),
        "norm_ffn": 1.0 + nrm(ks[5], (L, D), 0.05),
        "w_in": nrm(ks[6], (L, D, IN_COLS), D ** -0.5),
        "gm_ws": nrm(ks[7], (L, GM_HEADS, GM_BLOCK, GM_BLOCK), GM_BLOCK ** -0.5),
        "gm_bs": 1.0 + nrm(ks[8], (L, GM_HEADS, GM_BLOCK), 0.1),
        "gm_vnorm": 1.0 + nrm(ks[9], (L, GM_WIDTH), 0.05),
        "out_norm_a": 1.0 + nrm(ks[10], (L, GM_WIDTH), 0.05),
        "out_norm_b": 1.0 + nrm(ks[11], (L, SB_WIDTH), 0.05),
        "w_out": nrm(ks[12], (L, D_MIX, D), D_MIX ** -0.5),
        "peer_wq": nrm(ks[13], (L, D, PEER_HEADS * PEER_QUERY_DIM), D ** -0.5),
        "peer_k1": nrm(ks[14], (L, N_KEYS, PEER_HALF), PEER_HALF ** -0.5),
        "peer_k2": nrm(ks[15], (L, N_KEYS, PEER_HALF), PEER_HALF ** -0.5),
        "peer_u": nrm(ks[16], (L, N_EXPERTS, D), D ** -0.5),
        "peer_v": nrm(ks[17], (L, N_EXPERTS, D), PEER_HEADS ** -0.5),
        "final_norm": 1.0 + nrm(ks[18], (D,), 0.05),
    }


def reference(x, c, ada_w, ada_b, norm_mix, norm_ffn, w_in, gm_ws, gm_bs, gm_vnorm,
              out_norm_a, out_norm_b, w_out, peer_wq, peer_k1, peer_k2, peer_u, peer_v,
              final_norm):
    c_act = jax.nn.silu(c)
    for l in range(DEPTH):
        mod = c_act @ ada_w[l] + ada_b[l]
        sh1, sc1, g1, sh2, sc2, g2 = jnp.split(mod[:, None, :], 6, axis=-1)
        h = rmsnorm(x, norm_mix[l]) * (1.0 + sc1) + sh1
        x = x + g1 * mixer(h, w_in[l], gm_ws[l], gm_bs[l], gm_vnorm[l],
                           out_norm_a[l], out_norm_b[l], w_out[l])
        h = rmsnorm(x, norm_ffn[l]) * (1.0 + sc2) + sh2
        x = x + g2 * peer_ffn(h, peer_wq[l], peer_k1[l], peer_k2[l], peer_u[l], peer_v[l])
    return rmsnorm(x, final_norm)
```

```python
import numpy as np
from contextlib import ExitStack
import concourse.bass as bass
import concourse.mybir as mybir
from concourse.bass_utils import run_bass_kernel_spmd

F32 = mybir.dt.float32
I32 = mybir.dt.int32
U32 = mybir.dt.uint32
AF = mybir.ActivationFunctionType
ALU = mybir.AluOpType
AX = mybir.AxisListType

D = 1024
NEXP = 16384
EPS = 1e-6
NDS = 40
SAME_ENG_SYNC = True


class Dep:
    __slots__ = ("w", "r")

    def __init__(self):
        self.w = None
        self.r = {}


class KB:
    def __init__(self, nc):
        self.nc = nc
        self.es = ExitStack()
        self.eng = {"pe": nc.tensor, "act": nc.scalar, "dve": nc.vector, "pool": nc.gpsimd, "sp": nc.sync}
        self.esem = {e: self.es.enter_context(nc.semaphore("es_" + e)) for e in self.eng}
        self.ecount = {e: 0 for e in self.eng}
        self.known = {e: {} for e in self.eng}
        self.dsem = [self.es.enter_context(nc.semaphore("ds%d" % i)) for i in range(NDS)]
        self.dcount = [0] * NDS
        self.dnext = 0
        self.nwait = 0
        self.nins = 0

    def _sem(self, key):
        return self.esem[key[1]] if key[0] == "e" else self.dsem[key[1]]

    def _need(self, e, deps):
        need = {}
        for key, val in deps:
            if key[0] == "e" and key[1] == e:
                if e == "pe" or not SAME_ENG_SYNC:
                    continue
            if self.known[e].get(key, 0) >= val:
                continue
            if need.get(key, 0) < val:
                need[key] = val
        return list(need.items())

    def _wait(self, e, deps, keep_one=False):
        need = self._need(e, deps)
        emb = None
        if keep_one and need:
            emb = need.pop()
        for key, val in need:
            self.eng[e].wait_ge(self._sem(key), val)
            self.known[e][key] = val
            self.nwait += 1
        return emb

    def _embed(self, e, ins, emb):
        if emb is not None:
            key, val = emb
            ins._wait_ge(self._sem(key), val)
            self.known[e][key] = val

    @staticmethod
    def _collect(reads, writes):
        deps = []
        for d in reads:
            if d.w is not None:
                deps.append(d.w)
        for d in writes:
            if d.w is not None:
                deps.append(d.w)
            deps.extend(d.r.items())
        return deps

    @staticmethod
    def _record(ev, reads, writes):
        key, val = ev
        for d in reads:
            if d.r.get(key, 0) < val:
                d.r[key] = val
        for d in writes:
            d.w = ev
            d.r = {}

    def op(self, e, fn, reads=(), writes=()):
        emb = self._wait(e, self._collect(reads, writes), keep_one=True)
        ins = fn(self.eng[e])
        self._embed(e, ins, emb)
        self.ecount[e] += 1
        ins.then_inc(self.esem[e], 1)
        self._record((("e", e), self.ecount[e]), reads, writes)
        self.nins += 1

    def dma(self, q, fn, reads=(), writes=()):
        deps = self._collect(reads, writes)
        k = self.dnext
        self.dnext = (k + 1) % NDS
        if self.dcount[k] > 0:
            deps.append((("d", k), self.dcount[k]))
        emb = self._wait(q, deps, keep_one=True)
        ins = fn(self.eng[q])
        self._embed(q, ins, emb)
        self.dcount[k] += 16
        ins.then_inc(self.dsem[k], 16)
        self._record((("d", k), self.dcount[k]), reads, writes)
        self.nins += 1

    def barrier(self):
        allev = [(("e", e), c) for e, c in self.ecount.items() if c > 0]
        allev += [(("d", k), c) for k, c in enumerate(self.dcount) if c > 0]
        for e in self.eng:
            for key, val in allev:
                if self.known[e].get(key, 0) < val:
                    self.eng[e].wait_ge(self._sem(key), val)
                    self.known[e][key] = val
                    self.nwait += 1


class Pool:
    uid = 0

    def __init__(self, es, nc, name, shape, dt, n, psum=False):
        self.t = []
        for i in range(n):
            mk = nc.psum_tensor if psum else nc.sbuf_tensor
            Pool.uid += 1
            self.t.append((es.enter_context(mk("%s_%d_%d" % (name, i, Pool.uid), shape, dt)), Dep()))
        self.i = 0

    def get(self):
        r = self.t[self.i]
        self.i = (self.i + 1) % len(self.t)
        return r


def make_consts():
    c = np.zeros((128, 128 * 3 + 4 * 512 + 1), np.float32)
    c[:, 0:128] = np.eye(128, dtype=np.float32)
    j = np.arange(128)[:, None]
    s = np.arange(128)[None, :]
    c[:, 128:256] = -(j >= s).astype(np.float32)
    c[:, 256:384] = -1.0
    t = np.arange(512)[None, :]
    for jd in range(4):
        c[:, 384 + jd * 512:384 + (jd + 1) * 512] = ((128 * jd + j) < t).astype(np.float32)
    c[:, 384 + 2048] = 1.0
    return c


def build(L, S, dbg=False, phases="ABCDEF"):
    nc = bass.Bass("TRN2", target_bir_lowering=False)
    NT = S // 128
    NG = S // 512
    assert S % 512 == 0

    def din(name, shape, dt=F32):
        return nc.dram_tensor(name, shape, dt, kind="ExternalInput").ap()

    def dscr(name, shape, dt=F32):
        return nc.dram_tensor(name, shape, dt, kind="Internal").ap()

    x_in = din("x", [S, D])
    c_in = din("c", [128, 8])
    ada_w = din("ada_w", [L, D, 6 * D])
    ada_b = din("ada_b", [L, 6 * D])
    norm_mix = din("norm_mix", [L, D])
    norm_ffn = din("norm_ffn", [L, D])
    w_in = din("w_in", [L, D, 2560])
    gm_wsT = din("gm_wsT", [L, 128, 8, 128])
    gm_bsT = din("gm_bsT", [L, 128, 8])
    gm_vnorm = din("gm_vnorm", [L, 512])
    out_norm_a = din("out_norm_a", [L, 512])
    out_norm_bT = din("out_norm_bT", [L, 128, 4])
    w_out = din("w_out", [L, D, D])
    peer_wq = din("peer_wq", [L, D, 2048])
    peer_k1T = din("peer_k1T", [L, 128, 128])
    peer_k2T = din("peer_k2T", [L, 128, 128])
    peer_uv = din("peer_uv", [L * NEXP, 2 * D])
    final_norm = din("final_norm", [D])
    consts_in = din("consts", [128, 2433])
    out = nc.dram_tensor("out", [S, D], F32, kind="ExternalOutput").ap()

    xs = dscr("xs", [S, D])
    qT_s = dscr("qT_s", [512, S])
    kT_s = dscr("kT_s", [512, S])
    v_s = dscr("v_s", [S, 512])
    ya_s = dscr("ya_s", [S, 512])
    ybT_s = dscr("ybT_s", [512, S])
    eid_s = dscr("eid_s", [S, 128], I32)
    g_s = dscr("g_s", [S, 128])
    dbg_out = {}

    kb = KB(nc)
    top = kb.es

    def sbt(es, name, shape, dt=F32):
        Pool.uid += 1
        return es.enter_context(nc.sbuf_tensor("%s_t%d" % (name, Pool.uid), shape, dt))

    consts = sbt(top, "consts", [128, 2433]); d_consts = Dep()
    ident = consts[:, 0:128]
    negtri = consts[:, 128:256]
    negones = consts[:, 256:384]
    masks = [consts[:, 384 + jd * 512:384 + (jd + 1) * 512] for jd in range(4)]
    ones_col = consts[:, 2432:2433]
    mod = sbt(top, "mod", [128, 6, D]); d_mod = Dep()
    c_act = sbt(top, "c_act", [128, 8]); d_cact = Dep()
    psum = Pool(top, nc, "ps", [128, 512], F32, 6, psum=True)
    psum_acc = Pool(top, nc, "psacc", [128, 512], F32, 2, psum=True)

    kb.dma("sp", lambda q: q.dma_start(out=consts[:], in_=consts_in), writes=[d_consts])
    kb.dma("sp", lambda q: q.dma_start(out=c_act[:], in_=c_in), writes=[d_cact])
    with ExitStack() as es:
        sg = sbt(es, "sg", [128, 8]); d_sg = Dep()
        kb.op("act", lambda e: e.activation(out=sg[:], in_=c_act[:], func=AF.Sigmoid), reads=[d_cact], writes=[d_sg])
        kb.op("dve", lambda e: e.tensor_tensor(out=c_act[:], in0=c_act[:], in1=sg[:], op=ALU.mult), reads=[d_sg, d_cact], writes=[d_cact])
        kb.barrier()

    def rms_scale(es_pool, src, d_src, nfeat, ss_pool, junk, d_junk):
        ss, d_ss = ss_pool.get()
        s1 = ss[:, 0:1]
        kb.op("dve", lambda e: e.memset(s1, 0.0), writes=[d_ss])
        kb.op("dve", lambda e: e.scalar_tensor_tensor(out=junk, in0=src, scalar=1.0, in1=src, op0=ALU.mult, op1=ALU.mult,
                                                      accum_out=s1), reads=[d_src], writes=[d_junk, d_ss])
        kb.op("dve", lambda e: e.tensor_scalar(out=s1, in0=s1, scalar1=1.0 / nfeat, scalar2=EPS, op0=ALU.mult, op1=ALU.add),
              reads=[d_ss], writes=[d_ss])
        kb.op("act", lambda e: e.sqrt(out=s1, in_=s1), reads=[d_ss], writes=[d_ss])
        kb.op("dve", lambda e: e.reciprocal(out=s1, in_=s1), reads=[d_ss], writes=[d_ss])
        return ss, d_ss

    for l in range(L):
        x_src = x_in if l == 0 else xs
        with ExitStack() as es:
            c_rep = sbt(es, "c_rep", [128, 8, 128]); d_crep = Dep()
            kb.op("dve", lambda e: e.tensor_copy(out=c_rep[:], in_=c_act[:].unsqueeze(2).to_broadcast([128, 8, 128])),
                  reads=[d_cact], writes=[d_crep])
            wpool = Pool(es, nc, "adaw", [128, 8, 512], F32, 2)
            bpool = Pool(es, nc, "adab", [128, 512], F32, 2)
            nm = sbt(es, "nm", [128, 2, D]); d_nm = Dep()
            kb.dma("sp", lambda q: q.dma_start(out=nm[:, 0, :], in_=norm_mix[l].partition_broadcast(128)), writes=[d_nm])
            kb.dma("sp", lambda q: q.dma_start(out=nm[:, 1, :], in_=norm_ffn[l].partition_broadcast(128)), writes=[d_nm])
            for n in range(12):
                wt, d_wt = wpool.get()
                bt, d_bt = bpool.get()
                kb.dma("sp", lambda q: q.dma_start(out=wt[:], in_=ada_w[l][:, n * 512:(n + 1) * 512].rearrange("(kc p) n -> p kc n", p=128)),
                       writes=[d_wt])
                kb.dma("sp", lambda q: q.dma_start(out=bt[:], in_=ada_b[l, n * 512:(n + 1) * 512].partition_broadcast(128)), writes=[d_bt])
                pt, d_pt = psum.get()
                for kc in range(8):
                    kb.op("pe", lambda e: e.matmul(pt[:], lhsT=c_rep[:, kc, :], rhs=wt[:, kc, :], start=(kc == 0), stop=(kc == 7)),
                          reads=[d_crep, d_wt], writes=[d_pt])
                dst = mod[:, n // 2, (n % 2) * 512:(n % 2 + 1) * 512]
                kb.op("dve", lambda e: e.tensor_tensor(out=dst, in0=pt[:], in1=bt[:], op=ALU.add), reads=[d_pt, d_bt], writes=[d_mod])
            kb.op("dve", lambda e: e.scalar_tensor_tensor(out=mod[:, 1, :], in0=mod[:, 1, :], scalar=1.0, in1=nm[:, 0, :], op0=ALU.add, op1=ALU.mult),
                  reads=[d_nm, d_mod], writes=[d_mod])
            kb.op("dve", lambda e: e.scalar_tensor_tensor(out=mod[:, 4, :], in0=mod[:, 4, :], scalar=1.0, in1=nm[:, 1, :], op0=ALU.add, op1=ALU.mult),
                  reads=[d_nm, d_mod], writes=[d_mod])
            kb.barrier()
        sh1, A1, g1, sh2, A2, g2 = (mod[:, i, :] for i in range(6))

        def norm_mod(es, xt, d_xt, A, sh, sspool, junk, d_junk, h, d_h):
            rs, d_rs = rms_scale(es, xt[:], d_xt, D, sspool, junk[:], d_junk)
            kb.op("dve", lambda e: e.scalar_tensor_tensor(out=h[:], in0=xt[:], scalar=rs[:, 0:1], in1=A, op0=ALU.mult, op1=ALU.mult),
                  reads=[d_xt, d_rs, d_mod], writes=[d_h])
            kb.op("pool", lambda e: e.tensor_tensor(out=h[:], in0=h[:], in1=sh, op=ALU.add), reads=[d_h, d_mod], writes=[d_h])

        def transpose_to(src, d_src, nchunk, dst_fn, d_dst, eng="act"):
            for c0 in range(0, nchunk, 4):
                pt, d_pt = psum.get()
                n = min(4, nchunk - c0)
                for c in range(c0, c0 + n):
                    kb.op("pe", lambda e: e.transpose(out=pt[:, (c - c0) * 128:(c - c0 + 1) * 128], in_=src[:, c * 128:(c + 1) * 128], identity=ident),
                          reads=[d_src, d_consts], writes=[d_pt])
                for c in range(c0, c0 + n):
                    if eng == "act":
                        kb.op("act", lambda e: e.copy(out=dst_fn(c), in_=pt[:, (c - c0) * 128:(c - c0 + 1) * 128]), reads=[d_pt], writes=[d_dst])
                    else:
                        kb.op("dve", lambda e: e.tensor_copy(out=dst_fn(c), in_=pt[:, (c - c0) * 128:(c - c0 + 1) * 128]), reads=[d_pt], writes=[d_dst])

        if "B" in phases:
          with ExitStack() as es:
            wi = sbt(es, "wi", [128, 8, 2560]); d_wi = [Dep() for _ in range(8)]
            for kc in range(8):
                kb.dma("sp", lambda q: q.dma_start(out=wi[:, kc, :], in_=w_in[l][kc * 128:(kc + 1) * 128, :]), writes=[d_wi[kc]])
            wsT = sbt(es, "wsT", [128, 8, 128]); d_wsT = Dep()
            kb.dma("sp", lambda q: q.dma_start(out=wsT[:], in_=gm_wsT[l]), writes=[d_wsT])
            kb.op("dve", lambda e: e.memset(wsT[64:128, :, 0:64], 0.0), writes=[d_wsT])
            bsT = sbt(es, "bsT", [128, 8]); d_bsT = Dep()
            kb.dma("sp", lambda q: q.dma_start(out=bsT[:], in_=gm_bsT[l]), writes=[d_bsT])
            vg = sbt(es, "vg", [128, 512]); d_vg = Dep()
            kb.dma("sp", lambda q: q.dma_start(out=vg[:], in_=gm_vnorm[l].partition_broadcast(128)), writes=[d_vg])
            ona = sbt(es, "ona", [128, 512]); d_ona = Dep()
            kb.dma("sp", lambda q: q.dma_start(out=ona[:], in_=out_norm_a[l].partition_broadcast(128)), writes=[d_ona])
            xpool = Pool(es, nc, "bx", [128, D], F32, 2)
            hpool = Pool(es, nc, "bh", [128, D], F32, 2)
            junk = sbt(es, "bjunk", [128, D]); d_junk = Dep()
            sspool = Pool(es, nc, "bss", [128, 8], F32, 4)
            hTpool = Pool(es, nc, "bhT", [128, 8, 512], F32, 2)
            t512 = Pool(es, nc, "bt", [128, 512], F32, 12)
            for g in range(NG):
                hT, d_hT = hTpool.get()
                for j in range(4):
                    tb = g * 4 + j
                    xt, d_xt = xpool.get()
                    kb.dma("sp", lambda q: q.dma_start(out=xt[:], in_=x_src[tb * 128:(tb + 1) * 128, :]), writes=[d_xt])
                    h, d_h = hpool.get()
                    norm_mod(es, xt, d_xt, A1, sh1, sspool, junk, d_junk, h, d_h)
                    transpose_to(h, d_h, 8, lambda c: hT[:, c, j * 128:(j + 1) * 128], d_hT)
                for j in range(4):
                    tb = g * 4 + j
                    rows = slice(tb * 128, (tb + 1) * 128)
                    res = {}
                    for name, c0 in (("u", 0), ("v", 512), ("va", 2048)):
                        pt, d_pt = psum.get()
                        for kc in range(8):
                            kb.op("pe", lambda e: e.matmul(pt[:], lhsT=hT[:, kc, j * 128:(j + 1) * 128], rhs=wi[:, kc, c0:c0 + 512],
                                                           start=(kc == 0), stop=(kc == 7)), reads=[d_hT, d_wi[kc]], writes=[d_pt])
                        t, d_t = t512.get()
                        if name == "va":
                            kb.op("act", lambda e: e.copy(out=t[:], in_=pt[:]), reads=[d_pt], writes=[d_t])
                            kb.dma("sp", lambda q: q.dma_start(out=v_s[rows, :], in_=t[:]), reads=[d_t])
                        else:
                            kb.op("act", lambda e: e.activation(out=t[:], in_=pt[:], func=AF.Gelu_apprx_tanh), reads=[d_pt], writes=[d_t])
                        res[name] = (t, d_t)
                    gu, d_gu = res["u"]
                    gv, d_gv = res["v"]
                    sq, d_sq = t512.get()
                    kb.op("pool", lambda e: e.tensor_tensor(out=sq[:], in0=gv[:], in1=gv[:], op=ALU.mult), reads=[d_gv], writes=[d_sq])
                    rh, d_rh = sspool.get()
                    kb.op("dve", lambda e: e.tensor_reduce(out=rh[:], in_=sq[:].rearrange("p (h c) -> p h c", h=8), axis=AX.X, op=ALU.add),
                          reads=[d_sq], writes=[d_rh])
                    kb.op("dve", lambda e: e.tensor_scalar(out=rh[:], in0=rh[:], scalar1=1.0 / 64, scalar2=EPS, op0=ALU.mult, op1=ALU.add),
                          reads=[d_rh], writes=[d_rh])
                    kb.op("act", lambda e: e.sqrt(out=rh[:], in_=rh[:]), reads=[d_rh], writes=[d_rh])
                    kb.op("dve", lambda e: e.reciprocal(out=rh[:], in_=rh[:]), reads=[d_rh], writes=[d_rh])
                    vh, d_vh = t512.get()
                    kb.op("dve", lambda e: e.tensor_tensor(out=vh[:].rearrange("p (h c) -> p h c", h=8), in0=gv[:].rearrange("p (h c) -> p h c", h=8),
                                                           in1=rh[:].unsqueeze(2).to_broadcast([128, 8, 64]), op=ALU.mult),
                          reads=[d_gv, d_rh], writes=[d_vh])
                    kb.op("pool", lambda e: e.tensor_tensor(out=vh[:], in0=vh[:], in1=vg[:], op=ALU.mult), reads=[d_vh, d_vg], writes=[d_vh])
                    pz, d_pz = psum.get()
                    for hh in range(8):
                        kb.op("pe", lambda e: e.matmul(pz[:, hh * 64:(hh + 1) * 64], lhsT=wsT[:, hh, :], rhs=vh[:, hh * 64:(hh + 1) * 64],
                                                       start=True, stop=True), reads=[d_wsT, d_vh], writes=[d_pz])
                    ya, d_ya = t512.get()
                    kb.op("dve", lambda e: e.tensor_tensor(out=ya[:].rearrange("p (h c) -> p h c", h=8), in0=pz[:].rearrange("p (h c) -> p h c", h=8),
                                                           in1=bsT[:].unsqueeze(2).to_broadcast([128, 8, 64]), op=ALU.add),
                          reads=[d_pz, d_bsT], writes=[d_ya])
                    kb.op("pool", lambda e: e.tensor_tensor(out=ya[:], in0=ya[:], in1=gu[:], op=ALU.mult), reads=[d_ya, d_gu], writes=[d_ya])
                    ra, d_ra = rms_scale(es, ya[:], d_ya, 512, sspool, junk[:, 0:512], d_junk)
                    yan, d_yan = t512.get()
                    kb.op("dve", lambda e: e.scalar_tensor_tensor(out=yan[:], in0=ya[:], scalar=ra[:, 0:1], in1=ona[:], op0=ALU.mult, op1=ALU.mult),
                          reads=[d_ya, d_ra, d_ona], writes=[d_yan])
                    kb.dma("sp", lambda q: q.dma_start(out=ya_s[rows, :], in_=yan[:]), reads=[d_yan])
                for cc in range(8):
                    col0 = 1024 + cc * 128
                    pt, d_pt = psum.get()
                    for kc in range(8):
                        kb.op("pe", lambda e: e.matmul(pt[:], lhsT=wi[:, kc, col0:col0 + 128], rhs=hT[:, kc, :], start=(kc == 0), stop=(kc == 7)),
                              reads=[d_hT, d_wi[kc]], writes=[d_pt])
                    t, d_t = t512.get()
                    if cc < 4:
                        kb.op("act", lambda e: e.activation(out=t[:], in_=pt[:], func=AF.Copy, scale=0.125), reads=[d_pt], writes=[d_t])
                        dst = qT_s[cc * 128:(cc + 1) * 128, g * 512:(g + 1) * 512]
                    else:
                        kb.op("act", lambda e: e.copy(out=t[:], in_=pt[:]), reads=[d_pt], writes=[d_t])
                        dst = kT_s[(cc - 4) * 128:(cc - 3) * 128, g * 512:(g + 1) * 512]
                    kb.dma("sp", lambda q: q.dma_start(out=dst, in_=t[:]), reads=[d_t])
            kb.barrier()

        if "C" in phases:
          with ExitStack() as es:
            qpool = Pool(es, nc, "cq", [128, S], F32, 2)
            kpool = Pool(es, nc, "ck", [128, S], F32, 2)
            vpool = Pool(es, nc, "cv", [128, NT, 128], F32, 2)
            epool = Pool(es, nc, "ce", [128, 512], F32, 3)
            sppool = Pool(es, nc, "csp", [128, 512], F32, 4)
            apool = Pool(es, nc, "ca", [128, 512], F32, 4)
            cspool = Pool(es, nc, "ccs", [128, 512], F32, 3)
            ybpool = Pool(es, nc, "cyb", [64, 512], F32, 2)
            tiles = []
            for hp in range(4):
                for hh in range(2):
                    for g in range(NG):
                        nkb = 4 * g + 4
                        for idx, kbk in enumerate(reversed(range(nkb))):
                            tiles.append((hp, hh, g, idx, kbk, nkb))
            loaded = {}
            grp = {}
            st = {}

            def operands(hp):
                if hp not in loaded:
                    qT, d_q = qpool.get()
                    kT, d_k = kpool.get()
                    vv, d_v = vpool.get()
                    kb.dma("sp", lambda q: q.dma_start(out=qT[:], in_=qT_s[hp * 128:(hp + 1) * 128, :]), writes=[d_q])
                    kb.dma("sp", lambda q: q.dma_start(out=kT[:], in_=kT_s[hp * 128:(hp + 1) * 128, :]), writes=[d_k])
                    kb.dma("sp", lambda q: q.dma_start(out=vv[:], in_=v_s[:, hp * 128:(hp + 1) * 128].rearrange("(kb p) d -> p kb d", p=128)),
                           writes=[d_v])
                    loaded[hp] = (qT, d_q, kT, d_k, vv, d_v)
                return loaded[hp]

            def stage1(i):
                hp, hh, g, idx, kbk, nkb = tiles[i]
                qT, d_q, kT, d_k, vv, d_v = operands(hp)
                if hp + 1 < 4 and hh == 1 and g == 0 and idx == 0:
                    operands(hp + 1)
                pr = slice(hh * 64, (hh + 1) * 64)
                qs = slice(g * 512, (g + 1) * 512)
                ks = slice(kbk * 128, (kbk + 1) * 128)
                jd = kbk - 4 * g
                if idx == 0:
                    grp[(hp, hh, g)] = psum_acc.get() + cspool.get()
                pz, d_pz = psum.get()
                kb.op("pe", lambda e: e.matmul(pz[:], lhsT=kT[pr, ks], rhs=qT[pr, qs], start=True, stop=False),
                      reads=[d_q, d_k], writes=[d_pz])
                et, d_et = epool.get()
                kb.op("act", lambda e: e.activation(out=et[:], in_=pz[:], func=AF.Exp), reads=[d_pz], writes=[d_et])
                spt, d_spt = sppool.get()
                kb.op("act", lambda e: e.activation(out=spt[:], in_=et[:], func=AF.Ln, bias=1.0), reads=[d_et], writes=[d_spt])
                if jd >= 0:
                    kb.op("pool", lambda e: e.tensor_tensor(out=spt[:], in0=spt[:], in1=masks[jd], op=ALU.mult),
                          reads=[d_spt, d_consts], writes=[d_spt])
                st[i] = (pz, d_pz, spt, d_spt)

            def stage2(i):
                hp, hh, g, idx, kbk, nkb = tiles[i]
                jd = kbk - 4 * g
                pz, d_pz, spt, d_spt = st[i]
                po, d_po, cs, d_cs = grp[(hp, hh, g)]
                kb.op("pe", lambda e: e.matmul(pz[:], lhsT=negtri, rhs=spt[:], start=False, stop=(idx == 0)),
                      reads=[d_spt, d_consts], writes=[d_pz])
                if idx > 0:
                    kb.op("pe", lambda e: e.matmul(pz[:], lhsT=negones, rhs=cs[:], start=False, stop=True),
                          reads=[d_cs, d_consts], writes=[d_pz])
                at, d_at = apool.get()
                kb.op("act", lambda e: e.activation(out=at[:], in_=pz[:], func=AF.Exp), reads=[d_pz], writes=[d_at])
                if jd >= 0:
                    kb.op("pool", lambda e: e.tensor_tensor(out=at[:], in0=at[:], in1=masks[jd], op=ALU.mult),
                          reads=[d_at, d_consts], writes=[d_at])
                if idx < nkb - 1:
                    if idx == 0:
                        kb.op("dve", lambda e: e.tensor_copy(out=cs[:], in_=spt[:]), reads=[d_spt], writes=[d_cs])
                    else:
                        kb.op("dve", lambda e: e.tensor_tensor(out=cs[:], in0=cs[:], in1=spt[:], op=ALU.add),
                              reads=[d_spt, d_cs], writes=[d_cs])
                st[i] = (at, d_at)

            def stage3(i):
                hp, hh, g, idx, kbk, nkb = tiles[i]
                qT, d_q, kT, d_k, vv, d_v = operands(hp)
                pr = slice(hh * 64, (hh + 1) * 64)
                qs = slice(g * 512, (g + 1) * 512)
                at, d_at = st.pop(i)
                po, d_po, cs, d_cs = grp[(hp, hh, g)]
                kb.op("pe", lambda e: e.matmul(po[0:64, :], lhsT=vv[:, kbk, pr], rhs=at[:], start=(idx == 0), stop=(idx == nkb - 1)),
                      reads=[d_v, d_at], writes=[d_po])
                if idx == nkb - 1:
                    head = hp * 2 + hh
                    yb, d_yb = ybpool.get()
                    kb.op("dve", lambda e: e.tensor_copy(out=yb[:], in_=po[0:64, :]), reads=[d_po], writes=[d_yb])
                    kb.dma("sp", lambda q: q.dma_start(out=ybT_s[head * 64:(head + 1) * 64, qs], in_=yb[:]), reads=[d_yb])
                    del grp[(hp, hh, g)]

            nt = len(tiles)
            for step in range(nt + 2):
                if step < nt:
                    stage1(step)
                if 0 <= step - 1 < nt:
                    stage2(step - 1)
                if 0 <= step - 2 < nt:
                    stage3(step - 2)
            kb.barrier()

        if "D" in phases:
          with ExitStack() as es:
            wo = sbt(es, "wo", [128, 8, D]); d_wo = [Dep() for _ in range(8)]
            for kc in range(8):
                kb.dma("sp", lambda q: q.dma_start(out=wo[:, kc, :], in_=w_out[l][kc * 128:(kc + 1) * 128, :]), writes=[d_wo[kc]])
            gb = sbt(es, "gb", [128, 4]); d_gb = Dep()
            kb.dma("sp", lambda q: q.dma_start(out=gb[:], in_=out_norm_bT[l]), writes=[d_gb])
            xpool = Pool(es, nc, "dx", [128, D], F32, 2)
            xnpool = Pool(es, nc, "dxn", [128, D], F32, 2)
            yanpool = Pool(es, nc, "dyan", [128, 512], F32, 2)
            ybpool = Pool(es, nc, "dyb", [128, 4, 128], F32, 2)
            t512 = Pool(es, nc, "dt", [128, 512], F32, 8)
            sspool = Pool(es, nc, "dss", [128, 8], F32, 4)
            for tb in range(NT):
                rows = slice(tb * 128, (tb + 1) * 128)
                xt, d_xt = xpool.get()
                kb.dma("sp", lambda q: q.dma_start(out=xt[:], in_=x_src[rows, :]), writes=[d_xt])
                yan, d_yan = yanpool.get()
                kb.dma("sp", lambda q: q.dma_start(out=yan[:], in_=ya_s[rows, :]), writes=[d_yan])
                ybT, d_ybT = ybpool.get()
                kb.dma("sp", lambda q: q.dma_start(out=ybT[:], in_=ybT_s[:, rows].rearrange("(c p) t -> p c t", p=128)), writes=[d_ybT])
                yaT, d_yaT = t512.get()
                transpose_to(yan, d_yan, 4, lambda c: yaT[:, c * 128:(c + 1) * 128], d_yaT)
                sqb, d_sqb = t512.get()
                kb.op("pool", lambda e: e.tensor_tensor(out=sqb[:], in0=ybT[:].rearrange("p c t -> p (c t)"), in1=ybT[:].rearrange("p c t -> p (c t)"), op=ALU.mult),
                      reads=[d_ybT], writes=[d_sqb])
                pq, d_pq = psum.get()
                for c in range(4):
                    kb.op("pe", lambda e: e.matmul(pq[:, 0:1], lhsT=sqb[:, c * 128:(c + 1) * 128], rhs=ones_col, start=(c == 0), stop=(c == 3)),
                          reads=[d_sqb, d_consts], writes=[d_pq])
                rb, d_rb = sspool.get()
                kb.op("dve", lambda e: e.tensor_scalar(out=rb[:, 0:1], in0=pq[:, 0:1], scalar1=1.0 / 512, scalar2=EPS, op0=ALU.mult, op1=ALU.add),
                      reads=[d_pq], writes=[d_rb])
                kb.op("act", lambda e: e.sqrt(out=rb[:, 0:1], in_=rb[:, 0:1]), reads=[d_rb], writes=[d_rb])
                kb.op("dve", lambda e: e.reciprocal(out=rb[:, 0:1], in_=rb[:, 0:1]), reads=[d_rb], writes=[d_rb])
                ybg, d_ybg = t512.get()
                for c in range(4):
                    kb.op("pool", lambda e: e.tensor_scalar(out=ybg[:, c * 128:(c + 1) * 128], in0=ybT[:, c, :], scalar1=gb[:, c:c + 1], scalar2=None, op0=ALU.mult),
                          reads=[d_ybT, d_gb], writes=[d_ybg])
                xn, d_xn = xnpool.get()
                for half in range(2):
                    hs = slice(half * 512, (half + 1) * 512)
                    pA, d_pA = psum.get()
                    for c in range(4):
                        kb.op("pe", lambda e: e.matmul(pA[:], lhsT=yaT[:, c * 128:(c + 1) * 128], rhs=wo[:, c, hs], start=(c == 0), stop=(c == 3)),
                              reads=[d_yaT, d_wo[c]], writes=[d_pA])
                    pB, d_pB = psum.get()
                    for c in range(4):
                        kb.op("pe", lambda e: e.matmul(pB[:], lhsT=ybg[:, c * 128:(c + 1) * 128], rhs=wo[:, 4 + c, hs], start=(c == 0), stop=(c == 3)),
                              reads=[d_ybg, d_wo[4 + c]], writes=[d_pB])
                    pas, d_pas = t512.get()
                    kb.op("act", lambda e: e.copy(out=pas[:], in_=pA[:]), reads=[d_pA], writes=[d_pas])
                    mix, d_mix = t512.get()
                    kb.op("dve", lambda e: e.scalar_tensor_tensor(out=mix[:], in0=pB[:], scalar=rb[:, 0:1], in1=pas[:], op0=ALU.mult, op1=ALU.add),
                          reads=[d_pB, d_rb, d_pas], writes=[d_mix])
                    kb.op("pool", lambda e: e.tensor_tensor(out=mix[:], in0=mix[:], in1=g1[:, hs], op=ALU.mult), reads=[d_mix, d_mod], writes=[d_mix])
                    kb.op("dve", lambda e: e.tensor_tensor(out=xn[:, hs], in0=xt[:, hs], in1=mix[:], op=ALU.add), reads=[d_mix, d_xt], writes=[d_xn])
                kb.dma("sp", lambda q: q.dma_start(out=xs[rows, :], in_=xn[:]), reads=[d_xn])
            kb.barrier()

        if "E" in phases:
          with ExitStack() as es:
            wq = sbt(es, "wq", [128, 8, 2048]); d_wq = [Dep() for _ in range(8)]
            for kc in range(8):
                kb.dma("sp", lambda q: q.dma_start(out=wq[:, kc, :], in_=peer_wq[l][kc * 128:(kc + 1) * 128, :]), writes=[d_wq[kc]])
            kT = sbt(es, "pk", [128, 2, 128]); d_kT = Dep()
            kb.dma("sp", lambda q: q.dma_start(out=kT[:, 0, :], in_=peer_k1T[l]), writes=[d_kT])
            kb.dma("sp", lambda q: q.dma_start(out=kT[:, 1, :], in_=peer_k2T[l]), writes=[d_kT])
            xpool = Pool(es, nc, "ex", [128, D], F32, 2)
            hpool = Pool(es, nc, "eh", [128, D], F32, 2)
            junk = sbt(es, "ejunk", [128, D]); d_junk = Dep()
            sspool = Pool(es, nc, "ess", [128, 8], F32, 4)
            hTpool = Pool(es, nc, "ehT", [128, 8, 128], F32, 2)
            qTpool = Pool(es, nc, "eqT", [128, 16, 128], F32, 1)
            scpool = Pool(es, nc, "esc", [128, 16, 128], F32, 1)
            wkpool = Pool(es, nc, "ewk", [128, 128], F32, 2)
            v16pool = Pool(es, nc, "ev16", [128, 16, 16], F32, 1)
            i16pool = Pool(es, nc, "ei16", [128, 16, 16], U32, 1)
            i16fpool = Pool(es, nc, "ei16f", [128, 16, 16], F32, 1)
            candpool = Pool(es, nc, "ecand", [128, 16, 16], F32, 2)
            cidpool = Pool(es, nc, "ecid", [128, 16, 16], F32, 2)
            wk2pool = Pool(es, nc, "ewk2", [128, 256], F32, 2)
            j2pool = Pool(es, nc, "ej2", [128, 256], F32, 1)
            tspool = Pool(es, nc, "ets", [128, 8, 16], F32, 2)
            eidfpool = Pool(es, nc, "eeidf", [128, 128], F32, 2)
            eidipool = Pool(es, nc, "eeidi", [128, 128], I32, 2)
            gpool = Pool(es, nc, "eg", [128, 8, 16], F32, 2)
            for tb in range(NT):
                rows = slice(tb * 128, (tb + 1) * 128)
                xt, d_xt = xpool.get()
                kb.dma("sp", lambda q: q.dma_start(out=xt[:], in_=xs[rows, :]), writes=[d_xt])
                h, d_h = hpool.get()
                norm_mod(es, xt, d_xt, A2, sh2, sspool, junk, d_junk, h, d_h)
                hT, d_hT = hTpool.get()
                transpose_to(h, d_h, 8, lambda c: hT[:, c, :], d_hT)
                qT, d_qT = qTpool.get()
                for j0 in range(0, 16, 4):
                    pt, d_pt = psum.get()
                    for j in range(j0, j0 + 4):
                        for kc in range(8):
                            kb.op("pe", lambda e: e.matmul(pt[:, (j - j0) * 128:(j - j0 + 1) * 128], lhsT=wq[:, kc, j * 128:(j + 1) * 128], rhs=hT[:, kc, :],
                                                           start=(kc == 0), stop=(kc == 7)), reads=[d_hT, d_wq[kc]], writes=[d_pt])
                    kb.op("act", lambda e: e.copy(out=qT[:, j0:j0 + 4, :].rearrange("p a b -> p (a b)"), in_=pt[:]), reads=[d_pt], writes=[d_qT])
                sc, d_sc = scpool.get()
                for j0 in range(0, 16, 4):
                    pt, d_pt = psum.get()
                    for j in range(j0, j0 + 4):
                        kb.op("pe", lambda e: e.matmul(pt[:, (j - j0) * 128:(j - j0 + 1) * 128], lhsT=qT[:, j, :], rhs=kT[:, j % 2, :], start=True, stop=True),
                              reads=[d_qT, d_kT], writes=[d_pt])
                    kb.op("act", lambda e: e.copy(out=sc[:, j0:j0 + 4, :].rearrange("p a b -> p (a b)"), in_=pt[:]), reads=[d_pt], writes=[d_sc])
                v16, d_v16 = v16pool.get()
                i16, d_i16 = i16pool.get()
                for j in range(16):
                    wk, d_wk = wkpool.get()
                    kb.op("dve", lambda e: e.max(out=v16[:, j, 0:8], in_=sc[:, j, :]), reads=[d_sc], writes=[d_v16])
                    kb.op("dve", lambda e: e.max_index(out=i16[:, j, 0:8], in_max=v16[:, j, 0:8], in_values=sc[:, j, :]), reads=[d_sc, d_v16], writes=[d_i16])
                    kb.op("dve", lambda e: e.match_replace(out=wk[:], in_to_replace=v16[:, j, 0:8], in_values=sc[:, j, :], imm_value=-1e30),
                          reads=[d_sc, d_v16], writes=[d_wk])
                    kb.op("dve", lambda e: e.max(out=v16[:, j, 8:16], in_=wk[:]), reads=[d_wk], writes=[d_v16])
                    kb.op("dve", lambda e: e.max_index(out=i16[:, j, 8:16], in_max=v16[:, j, 8:16], in_values=wk[:]), reads=[d_wk, d_v16], writes=[d_i16])
                i16f, d_i16f = i16fpool.get()
                kb.op("dve", lambda e: e.tensor_copy(out=i16f[:], in_=i16[:]), reads=[d_i16], writes=[d_i16f])
                ts, d_ts = tspool.get()
                eidf, d_eidf = eidfpool.get()
                kb.op("pool", lambda e: e.memset(eidf[:], 0.0), writes=[d_eidf])
                for hd in range(8):
                    j1, j2 = 2 * hd, 2 * hd + 1
                    cand, d_cand = candpool.get()
                    cid, d_cid = cidpool.get()
                    kb.op("dve", lambda e: e.tensor_tensor(out=cand[:], in0=v16[:, j1, :].unsqueeze(2).to_broadcast([128, 16, 16]),
                                                           in1=v16[:, j2:j2 + 1, :].to_broadcast([128, 16, 16]), op=ALU.add),
                          reads=[d_v16], writes=[d_cand])
                    kb.op("dve", lambda e: e.scalar_tensor_tensor(out=cid[:], in0=i16f[:, j1, :].unsqueeze(2).to_broadcast([128, 16, 16]), scalar=128.0,
                                                                   in1=i16f[:, j2:j2 + 1, :].to_broadcast([128, 16, 16]), op0=ALU.mult, op1=ALU.add),
                          reads=[d_i16f], writes=[d_cid])
                    candf = cand[:].rearrange("p a b -> p (a b)")
                    cidf = cid[:].rearrange("p a b -> p (a b)")
                    wk2, d_wk2 = wk2pool.get()
                    kb.op("dve", lambda e: e.max(out=ts[:, hd, 0:8], in_=candf), reads=[d_cand], writes=[d_ts])
                    kb.op("dve", lambda e: e.match_replace(out=wk2[:], in_to_replace=ts[:, hd, 0:8], in_values=candf, imm_value=-1e30),
                          reads=[d_cand, d_ts], writes=[d_wk2])
                    kb.op("dve", lambda e: e.max(out=ts[:, hd, 8:16], in_=wk2[:]), reads=[d_wk2], writes=[d_ts])
                    j2t, d_j2t = j2pool.get()
                    for sl in range(16):
                        kb.op("dve", lambda e: e.scalar_tensor_tensor(out=j2t[:], in0=candf, scalar=ts[:, hd, sl:sl + 1], in1=cidf, op0=ALU.is_equal, op1=ALU.mult,
                                                                      accum_out=eidf[:, hd * 16 + sl:hd * 16 + sl + 1]),
                              reads=[d_cand, d_cid, d_ts], writes=[d_j2t, d_eidf])
                eidi, d_eidi = eidipool.get()
                kb.op("dve", lambda e: e.tensor_scalar(out=eidf[:], in0=eidf[:], scalar1=float(NEXP - 1), scalar2=float(l * NEXP), op0=ALU.min, op1=ALU.add),
                      reads=[d_eidf], writes=[d_eidf])
                kb.op("dve", lambda e: e.tensor_copy(out=eidi[:], in_=eidf[:]), reads=[d_eidf], writes=[d_eidi])
                kb.dma("sp", lambda q: q.dma_start(out=eid_s[rows, :], in_=eidi[:]), reads=[d_eidi])
                gt, d_gt = gpool.get()
                kb.op("dve", lambda e: e.tensor_tensor(out=gt[:], in0=ts[:], in1=ts[:, :, 0:1].to_broadcast([128, 8, 16]), op=ALU.subtract),
                      reads=[d_ts], writes=[d_gt])
                kb.op("act", lambda e: e.activation(out=gt[:], in_=gt[:], func=AF.Exp), reads=[d_gt], writes=[d_gt])
                sm, d_sm = sspool.get()
                kb.op("dve", lambda e: e.tensor_reduce(out=sm[:], in_=gt[:], axis=AX.X, op=ALU.add), reads=[d_gt], writes=[d_sm])
                kb.op("dve", lambda e: e.reciprocal(out=sm[:], in_=sm[:]), reads=[d_sm], writes=[d_sm])
                kb.op("dve", lambda e: e.tensor_tensor(out=gt[:], in0=gt[:], in1=sm[:].unsqueeze(2).to_broadcast([128, 8, 16]), op=ALU.mult),
                      reads=[d_gt, d_sm], writes=[d_gt])
                kb.dma("sp", lambda q: q.dma_start(out=g_s[rows, :], in_=gt[:].rearrange("p a b -> p (a b)")), reads=[d_gt])
            kb.barrier()

          with ExitStack() as es:
            NB, CH, NDVE = 12, 4, 1
            NCH = 128 // CH
            xpool = Pool(es, nc, "fx", [128, D], F32, 2)
            hpool = Pool(es, nc, "fh", [128, D], F32, 2)
            junk = sbt(es, "fjunk", [128, D]); d_junk = Dep()
            sspool = Pool(es, nc, "fss", [128, 8], F32, 4)
            uvpool = Pool(es, nc, "fuv", [128, 2 * D], F32, NB)
            tmppool = Pool(es, nc, "ftmp", [128, D], F32, 3)
            eidpool = Pool(es, nc, "feid", [128, 128], I32, 2)
            gpool = Pool(es, nc, "fg", [128, 128], F32, 2)
            actpool = Pool(es, nc, "fact", [128, 128], F32, 2)
            coefpool = Pool(es, nc, "fcoef", [128, 128], F32, 2)
            accpool = Pool(es, nc, "facc", [128, D], F32, 2)
            fn_bc = None
            if l == L - 1:
                fn_bc = sbt(es, "fnbc", [128, D]); d_fn = Dep()
                kb.dma("sp", lambda q: q.dma_start(out=fn_bc[:], in_=final_norm.partition_broadcast(128)), writes=[d_fn])

            def e2_loads(tb):
                rows = slice(tb * 128, (tb + 1) * 128)
                xt, d_xt = xpool.get()
                kb.dma("sp", lambda q: q.dma_start(out=xt[:], in_=xs[rows, :]), writes=[d_xt])
                eid, d_eid = eidpool.get()
                kb.dma("sp", lambda q: q.dma_start(out=eid[:], in_=eid_s[rows, :]), writes=[d_eid])
                gt, d_gt = gpool.get()
                kb.dma("sp", lambda q: q.dma_start(out=gt[:], in_=g_s[rows, :]), writes=[d_gt])
                return dict(xt=xt, d_xt=d_xt, eid=eid, d_eid=d_eid, gt=gt, d_gt=d_gt, bufs={})

            def gathers(bk, c):
                for j in range(c * CH, (c + 1) * CH):
                    uv, d_uv = uvpool.get()
                    kb.dma("pool", lambda q: q.indirect_dma_start(out=uv[:], out_offset=None, in_=peer_uv,
                                                                  in_offset=bass.IndirectOffsetOnAxis(ap=bk["eid"][:, j:j + 1], axis=0)),
                           reads=[bk["d_eid"]], writes=[d_uv])
                    bk["bufs"][j] = (uv, d_uv)

            chunkdeps = {}
            cur = e2_loads(0)
            gathers(cur, 0)
            for tb in range(NT):
                rows = slice(tb * 128, (tb + 1) * 128)
                nxt = e2_loads(tb + 1) if tb + 1 < NT else None
                xt, d_xt, gt, d_gt = cur["xt"], cur["d_xt"], cur["gt"], cur["d_gt"]
                h, d_h = hpool.get()
                norm_mod(es, xt, d_xt, A2, sh2, sspool, junk, d_junk, h, d_h)
                act, _ = actpool.get()
                coef, _ = coefpool.get()
                d_actc = chunkdeps.setdefault(id(act), [Dep() for _ in range(NCH)])
                d_coefc = chunkdeps.setdefault(id(coef), [Dep() for _ in range(NCH)])
                acc, d_acc = accpool.get()
                pa0, d_pa0 = psum.get()
                pa1, d_pa1 = psum.get()
                state = {"pe": 0, "dve": 0}

                def dots(c):
                    cs_ = slice(c * CH, (c + 1) * CH)
                    kb.op("dve", lambda e: e.memset(act[:, cs_], 0.0), reads=[], writes=[d_actc[c]])
                    for j in range(c * CH, (c + 1) * CH):
                        uv, d_uv = cur["bufs"][j]
                        kb.op("dve", lambda e: e.scalar_tensor_tensor(out=junk[:], in0=uv[:, 0:D], scalar=1.0, in1=h[:], op0=ALU.mult, op1=ALU.mult,
                                                                      accum_out=act[:, j:j + 1]), reads=[d_uv, d_h], writes=[d_junk, d_actc[c]])
                    kb.op("act", lambda e: e.activation(out=coef[:, cs_], in_=act[:, cs_], func=AF.Gelu_apprx_tanh), reads=[d_actc[c]], writes=[d_coefc[c]])
                    kb.op("dve", lambda e: e.tensor_tensor(out=coef[:, cs_], in0=coef[:, cs_], in1=gt[:, cs_], op=ALU.mult),
                          reads=[d_coefc[c], d_gt], writes=[d_coefc[c]])

                def vside(c):
                    for jj, j in enumerate(range(c * CH, (c + 1) * CH)):
                        uv, d_uv = cur["bufs"].pop(j)
                        if jj < NDVE:
                            if state["dve"] == 0:
                                kb.op("dve", lambda e: e.tensor_scalar(out=acc[:], in0=uv[:, D:2 * D], scalar1=coef[:, j:j + 1], scalar2=None, op0=ALU.mult),
                                      reads=[d_uv, d_coefc[c]], writes=[d_acc])
                            else:
                                kb.op("dve", lambda e: e.scalar_tensor_tensor(out=acc[:], in0=uv[:, D:2 * D], scalar=coef[:, j:j + 1], in1=acc[:],
                                                                              op0=ALU.mult, op1=ALU.add), reads=[d_uv, d_coefc[c], d_acc], writes=[d_acc])
                            state["dve"] += 1
                        else:
                            tmp, d_tmp = tmppool.get()
                            kb.op("act", lambda e: e.activation(out=tmp[:], in_=uv[:, D:2 * D], func=AF.Copy, scale=coef[:, j:j + 1]),
                                  reads=[d_uv, d_coefc[c]], writes=[d_tmp])
                            first = state["pe"] == 0
                            last = (j == 127)
                            kb.op("pe", lambda e: e.matmul(pa0[:], lhsT=ident, rhs=tmp[:, 0:512], start=first, stop=last), reads=[d_tmp, d_consts], writes=[d_pa0])
                            kb.op("pe", lambda e: e.matmul(pa1[:], lhsT=ident, rhs=tmp[:, 512:1024], start=first, stop=last), reads=[d_tmp, d_consts], writes=[d_pa1])
                            state["pe"] += 1

                for c in range(NCH + 1):
                    if c + 1 < NCH:
                        gathers(cur, c + 1)
                    elif c + 1 == NCH and nxt is not None:
                        gathers(nxt, 0)
                    if c < NCH:
                        dots(c)
                    if c >= 1:
                        vside(c - 1)
                kb.op("dve", lambda e: e.tensor_tensor(out=acc[:, 0:512], in0=pa0[:], in1=acc[:, 0:512], op=ALU.add), reads=[d_pa0, d_acc], writes=[d_acc])
                kb.op("dve", lambda e: e.tensor_tensor(out=acc[:, 512:1024], in0=pa1[:], in1=acc[:, 512:1024], op=ALU.add), reads=[d_pa1, d_acc], writes=[d_acc])
                kb.op("dve", lambda e: e.tensor_tensor(out=acc[:], in0=acc[:], in1=g2, op=ALU.mult), reads=[d_acc, d_mod], writes=[d_acc])
                kb.op("dve", lambda e: e.tensor_tensor(out=acc[:], in0=acc[:], in1=xt[:], op=ALU.add), reads=[d_acc, d_xt], writes=[d_acc])
                if l == L - 1:
                    rs, d_rs = rms_scale(es, acc[:], d_acc, D, sspool, junk[:], d_junk)
                    kb.op("dve", lambda e: e.scalar_tensor_tensor(out=acc[:], in0=acc[:], scalar=rs[:, 0:1], in1=fn_bc[:], op0=ALU.mult, op1=ALU.mult),
                          reads=[d_acc, d_rs, d_fn], writes=[d_acc])
                    kb.dma("sp", lambda q: q.dma_start(out=out[rows, :], in_=acc[:]), reads=[d_acc])
                else:
                    kb.dma("sp", lambda q: q.dma_start(out=xs[rows, :], in_=acc[:]), reads=[d_acc])
                cur = nxt
            kb.barrier()

    if dbg:
        for name, src, shape, dt in (("d_xs", xs, [S, D], F32), ("d_qT", qT_s, [512, S], F32), ("d_kT", kT_s, [512, S], F32),
                                     ("d_v", v_s, [S, 512], F32), ("d_ya", ya_s, [S, 512], F32), ("d_ybT", ybT_s, [512, S], F32),
                                     ("d_eid", eid_s, [S, 128], I32), ("d_g", g_s, [S, 128], F32)):
            o = nc.dram_tensor(name, shape, dt, kind="ExternalOutput").ap()
            kb.dma("sp", lambda q: q.dma_start(out=o, in_=src))
        kb.barrier()
    print("instructions", kb.nins, "waits", kb.nwait)
    return nc


def prep_inputs(inp, b, L):
    f = lambda a: np.ascontiguousarray(a, dtype=np.float32)
    m = {
        "x": f(inp["x"][b]),
        "c": f(inp["c"][b].reshape(8, 128).T),
        "ada_w": f(inp["ada_w"][:L]),
        "ada_b": f(inp["ada_b"][:L]),
        "norm_mix": f(inp["norm_mix"][:L]),
        "norm_ffn": f(inp["norm_ffn"][:L]),
        "w_in": f(inp["w_in"][:L]),
        "gm_wsT": f(np.transpose(inp["gm_ws"][:L], (0, 3, 1, 2))),
        "gm_bsT": f(np.transpose(inp["gm_bs"][:L], (0, 2, 1))),
        "gm_vnorm": f(inp["gm_vnorm"][:L]),
        "out_norm_a": f(inp["out_norm_a"][:L]),
        "out_norm_bT": f(np.transpose(inp["out_norm_b"][:L].reshape(L, 4, 128), (0, 2, 1))),
        "w_out": f(inp["w_out"][:L]),
        "peer_wq": f(inp["peer_wq"][:L]),
        "peer_k1T": f(np.transpose(inp["peer_k1"][:L], (0, 2, 1))),
        "peer_k2T": f(np.transpose(inp["peer_k2"][:L], (0, 2, 1))),
        "peer_uv": np.concatenate([f(inp["peer_u"][:L]).reshape(L * NEXP, D), f(inp["peer_v"][:L]).reshape(L * NEXP, D)], axis=1),
        "final_norm": f(inp["final_norm"]),
        "consts": make_consts(),
    }
    return m


def kernel(**inputs):
    inp = {k: np.asarray(v) for k, v in inputs.items()}
    B, S, _ = inp["x"].shape
    L = inp["ada_w"].shape[0]
    nc = build(L, S)
    shared = prep_inputs(inp, 0, L)
    in_maps = []
    for b in range(B):
        m = dict(shared)
        m["x"] = np.ascontiguousarray(inp["x"][b], dtype=np.float32)
        m["c"] = np.ascontiguousarray(inp["c"][b].reshape(8, 128).T, dtype=np.float32)
        in_maps.append(m)
    res = run_bass_kernel_spmd(nc, in_maps, core_ids=list(range(B)))
    return np.stack([r["out"] for r in res.results], axis=0).astype(np.float32)
```

```python
import numpy as np
from contextlib import ExitStack
import concourse.bass as bass
import concourse.mybir as mybir
from concourse.bass_utils import run_bass_kernel_spmd

F32 = mybir.dt.float32
I32 = mybir.dt.int32
U32 = mybir.dt.uint32
AF = mybir.ActivationFunctionType
ALU = mybir.AluOpType
AX = mybir.AxisListType

D = 1024
NEXP = 16384
EPS = 1e-6
NDS = 40
SAME_ENG_SYNC = True


class Dep:
    __slots__ = ("w", "r")

    def __init__(self):
        self.w = None
        self.r = {}


class KB:
    def __init__(self, nc):
        self.nc = nc
        self.es = ExitStack()
        self.eng = {"pe": nc.tensor, "act": nc.scalar, "dve": nc.vector, "pool": nc.gpsimd, "sp": nc.sync}
        self.esem = {e: self.es.enter_context(nc.semaphore("es_" + e)) for e in self.eng}
        self.ecount = {e: 0 for e in self.eng}
        self.known = {e: {} for e in self.eng}
        self.dsem = [self.es.enter_context(nc.semaphore("ds%d" % i)) for i in range(NDS)]
        self.dcount = [0] * NDS
        self.dnext = 0
        self.nwait = 0
        self.nins = 0

    def _sem(self, key):
        return self.esem[key[1]] if key[0] == "e" else self.dsem[key[1]]

    def _need(self, e, deps):
        need = {}
        for key, val in deps:
            if key[0] == "e" and key[1] == e:
                if e == "pe" or not SAME_ENG_SYNC:
                    continue
            if self.known[e].get(key, 0) >= val:
                continue
            if need.get(key, 0) < val:
                need[key] = val
        return list(need.items())

    def _wait(self, e, deps, keep_one=False):
        need = self._need(e, deps)
        emb = None
        if keep_one and need:
            emb = need.pop()
        for key, val in need:
            self.eng[e].wait_ge(self._sem(key), val)
            self.known[e][key] = val
            self.nwait += 1
        return emb

    def _embed(self, e, ins, emb):
        if emb is not None:
            key, val = emb
            ins._wait_ge(self._sem(key), val)
            self.known[e][key] = val

    @staticmethod
    def _collect(reads, writes):
        deps = []
        for d in reads:
            if d.w is not None:
                deps.append(d.w)
        for d in writes:
            if d.w is not None:
                deps.append(d.w)
            deps.extend(d.r.items())
        return deps

    @staticmethod
    def _record(ev, reads, writes):
        key, val = ev
        for d in reads:
            if d.r.get(key, 0) < val:
                d.r[key] = val
        for d in writes:
            d.w = ev
            d.r = {}

    def op(self, e, fn, reads=(), writes=()):
        emb = self._wait(e, self._collect(reads, writes), keep_one=True)
        ins = fn(self.eng[e])
        self._embed(e, ins, emb)
        self.ecount[e] += 1
        ins.then_inc(self.esem[e], 1)
        self._record((("e", e), self.ecount[e]), reads, writes)
        self.nins += 1

    def dma(self, q, fn, reads=(), writes=()):
        deps = self._collect(reads, writes)
        k = self.dnext
        self.dnext = (k + 1) % NDS
        if self.dcount[k] > 0:
            deps.append((("d", k), self.dcount[k]))
        emb = self._wait(q, deps, keep_one=True)
        ins = fn(self.eng[q])
        self._embed(q, ins, emb)
        self.dcount[k] += 16
        ins.then_inc(self.dsem[k], 16)
        self._record((("d", k), self.dcount[k]), reads, writes)
        self.nins += 1

    def barrier(self):
        allev = [(("e", e), c) for e, c in self.ecount.items() if c > 0]
        allev += [(("d", k), c) for k, c in enumerate(self.dcount) if c > 0]
        for e in self.eng:
            for key, val in allev:
                if self.known[e].get(key, 0) < val:
                    self.eng[e].wait_ge(self._sem(key), val)
                    self.known[e][key] = val
                    self.nwait += 1


class Pool:
    uid = 0

    def __init__(self, es, nc, name, shape, dt, n, psum=False):
        self.t = []
        for i in range(n):
            mk = nc.psum_tensor if psum else nc.sbuf_tensor
            Pool.uid += 1
            self.t.append((es.enter_context(mk("%s_%d_%d" % (name, i, Pool.uid), shape, dt)), Dep()))
        self.i = 0

    def get(self):
        r = self.t[self.i]
        self.i = (self.i + 1) % len(self.t)
        return r


def make_consts():
    c = np.zeros((128, 128 * 3 + 4 * 512 + 1), np.float32)
    c[:, 0:128] = np.eye(128, dtype=np.float32)
    j = np.arange(128)[:, None]
    s = np.arange(128)[None, :]
    c[:, 128:256] = -(j >= s).astype(np.float32)
    c[:, 256:384] = -1.0
    t = np.arange(512)[None, :]
    for jd in range(4):
        c[:, 384 + jd * 512:384 + (jd + 1) * 512] = ((128 * jd + j) < t).astype(np.float32)
    c[:, 384 + 2048] = 1.0
    return c


def build(L, S, dbg=False, phases="ABCDEF"):
    nc = bass.Bass("TRN2", target_bir_lowering=False)
    NT = S // 128
    NG = S // 512
    assert S % 512 == 0

    def din(name, shape, dt=F32):
        return nc.dram_tensor(name, shape, dt, kind="ExternalInput").ap()

    def dscr(name, shape, dt=F32):
        return nc.dram_tensor(name, shape, dt, kind="Internal").ap()

    x_in = din("x", [S, D])
    c_in = din("c", [128, 8])
    ada_w = din("ada_w", [L, D, 6 * D])
    ada_b = din("ada_b", [L, 6 * D])
    norm_mix = din("norm_mix", [L, D])
    norm_ffn = din("norm_ffn", [L, D])
    w_in = din("w_in", [L, D, 2560])
    gm_wsT = din("gm_wsT", [L, 128, 8, 128])
    gm_bsT = din("gm_bsT", [L, 128, 8])
    gm_vnorm = din("gm_vnorm", [L, 512])
    out_norm_a = din("out_norm_a", [L, 512])
    out_norm_bT = din("out_norm_bT", [L, 128, 4])
    w_out = din("w_out", [L, D, D])
    peer_wq = din("peer_wq", [L, D, 2048])
    peer_k1T = din("peer_k1T", [L, 128, 128])
    peer_k2T = din("peer_k2T", [L, 128, 128])
    peer_uv = din("peer_uv", [L * NEXP, 2 * D])
    final_norm = din("final_norm", [D])
    consts_in = din("consts", [128, 2433])
    out = nc.dram_tensor("out", [S, D], F32, kind="ExternalOutput").ap()

    xs = dscr("xs", [S, D])
    qT_s = dscr("qT_s", [512, S])
    kT_s = dscr("kT_s", [512, S])
    v_s = dscr("v_s", [S, 512])
    ya_s = dscr("ya_s", [S, 512])
    ybT_s = dscr("ybT_s", [512, S])
    eid_s = dscr("eid_s", [S, 128], I32)
    g_s = dscr("g_s", [S, 128])
    dbg_out = {}

    kb = KB(nc)
    top = kb.es

    def sbt(es, name, shape, dt=F32):
        Pool.uid += 1
        return es.enter_context(nc.sbuf_tensor("%s_t%d" % (name, Pool.uid), shape, dt))

    consts = sbt(top, "consts", [128, 2433]); d_consts = Dep()
    ident = consts[:, 0:128]
    negtri = consts[:, 128:256]
    negones = consts[:, 256:384]
    masks = [consts[:, 384 + jd * 512:384 + (jd + 1) * 512] for jd in range(4)]
    ones_col = consts[:, 2432:2433]
    mod = sbt(top, "mod", [128, 6, D]); d_mod = Dep()
    c_act = sbt(top, "c_act", [128, 8]); d_cact = Dep()
    psum = Pool(top, nc, "ps", [128, 512], F32, 6, psum=True)
    psum_acc = Pool(top, nc, "psacc", [128, 512], F32, 2, psum=True)

    kb.dma("sp", lambda q: q.dma_start(out=consts[:], in_=consts_in), writes=[d_consts])
    kb.dma("sp", lambda q: q.dma_start(out=c_act[:], in_=c_in), writes=[d_cact])
    with ExitStack() as es:
        sg = sbt(es, "sg", [128, 8]); d_sg = Dep()
        kb.op("act", lambda e: e.activation(out=sg[:], in_=c_act[:], func=AF.Sigmoid), reads=[d_cact], writes=[d_sg])
        kb.op("dve", lambda e: e.tensor_tensor(out=c_act[:], in0=c_act[:], in1=sg[:], op=ALU.mult), reads=[d_sg, d_cact], writes=[d_cact])
        kb.barrier()

    def rms_scale(es_pool, src, d_src, nfeat, ss_pool, junk, d_junk):
        ss, d_ss = ss_pool.get()
        s1 = ss[:, 0:1]
        kb.op("dve", lambda e: e.memset(s1, 0.0), writes=[d_ss])
        kb.op("dve", lambda e: e.scalar_tensor_tensor(out=junk, in0=src, scalar=1.0, in1=src, op0=ALU.mult, op1=ALU.mult,
                                                      accum_out=s1), reads=[d_src], writes=[d_junk, d_ss])
        kb.op("dve", lambda e: e.tensor_scalar(out=s1, in0=s1, scalar1=1.0 / nfeat, scalar2=EPS, op0=ALU.mult, op1=ALU.add),
              reads=[d_ss], writes=[d_ss])
        kb.op("act", lambda e: e.sqrt(out=s1, in_=s1), reads=[d_ss], writes=[d_ss])
        kb.op("dve", lambda e: e.reciprocal(out=s1, in_=s1), reads=[d_ss], writes=[d_ss])
        return ss, d_ss

    for l in range(L):
        x_src = x_in if l == 0 else xs
        with ExitStack() as es:
            c_rep = sbt(es, "c_rep", [128, 8, 128]); d_crep = Dep()
            kb.op("dve", lambda e: e.tensor_copy(out=c_rep[:], in_=c_act[:].unsqueeze(2).to_broadcast([128, 8, 128])),
                  reads=[d_cact], writes=[d_crep])
            wpool = Pool(es, nc, "adaw", [128, 8, 512], F32, 2)
            bpool = Pool(es, nc, "adab", [128, 512], F32, 2)
            nm = sbt(es, "nm", [128, 2, D]); d_nm = Dep()
            kb.dma("sp", lambda q: q.dma_start(out=nm[:, 0, :], in_=norm_mix[l].partition_broadcast(128)), writes=[d_nm])
            kb.dma("sp", lambda q: q.dma_start(out=nm[:, 1, :], in_=norm_ffn[l].partition_broadcast(128)), writes=[d_nm])
            for n in range(12):
                wt, d_wt = wpool.get()
                bt, d_bt = bpool.get()
                kb.dma("sp", lambda q: q.dma_start(out=wt[:], in_=ada_w[l][:, n * 512:(n + 1) * 512].rearrange("(kc p) n -> p kc n", p=128)),
                       writes=[d_wt])
                kb.dma("sp", lambda q: q.dma_start(out=bt[:], in_=ada_b[l, n * 512:(n + 1) * 512].partition_broadcast(128)), writes=[d_bt])
                pt, d_pt = psum.get()
                for kc in range(8):
                    kb.op("pe", lambda e: e.matmul(pt[:], lhsT=c_rep[:, kc, :], rhs=wt[:, kc, :], start=(kc == 0), stop=(kc == 7)),
                          reads=[d_crep, d_wt], writes=[d_pt])
                dst = mod[:, n // 2, (n % 2) * 512:(n % 2 + 1) * 512]
                kb.op("dve", lambda e: e.tensor_tensor(out=dst, in0=pt[:], in1=bt[:], op=ALU.add), reads=[d_pt, d_bt], writes=[d_mod])
            kb.op("dve", lambda e: e.scalar_tensor_tensor(out=mod[:, 1, :], in0=mod[:, 1, :], scalar=1.0, in1=nm[:, 0, :], op0=ALU.add, op1=ALU.mult),
                  reads=[d_nm, d_mod], writes=[d_mod])
            kb.op("dve", lambda e: e.scalar_tensor_tensor(out=mod[:, 4, :], in0=mod[:, 4, :], scalar=1.0, in1=nm[:, 1, :], op0=ALU.add, op1=ALU.mult),
                  reads=[d_nm, d_mod], writes=[d_mod])
            kb.barrier()
        sh1, A1, g1, sh2, A2, g2 = (mod[:, i, :] for i in range(6))

        def norm_mod(es, xt, d_xt, A, sh, sspool, junk, d_junk, h, d_h):
            rs, d_rs = rms_scale(es, xt[:], d_xt, D, sspool, junk[:], d_junk)
            kb.op("dve", lambda e: e.scalar_tensor_tensor(out=h[:], in0=xt[:], scalar=rs[:, 0:1], in1=A, op0=ALU.mult, op1=ALU.mult),
                  reads=[d_xt, d_rs, d_mod], writes=[d_h])
            kb.op("pool", lambda e: e.tensor_tensor(out=h[:], in0=h[:], in1=sh, op=ALU.add), reads=[d_h, d_mod], writes=[d_h])

        def transpose_to(src, d_src, nchunk, dst_fn, d_dst, eng="act"):
            for c0 in range(0, nchunk, 4):
                pt, d_pt = psum.get()
                n = min(4, nchunk - c0)
                for c in range(c0, c0 + n):
                    kb.op("pe", lambda e: e.transpose(out=pt[:, (c - c0) * 128:(c - c0 + 1) * 128], in_=src[:, c * 128:(c + 1) * 128], identity=ident),
                          reads=[d_src, d_consts], writes=[d_pt])
                for c in range(c0, c0 + n):
                    if eng == "act":
                        kb.op("act", lambda e: e.copy(out=dst_fn(c), in_=pt[:, (c - c0) * 128:(c - c0 + 1) * 128]), reads=[d_pt], writes=[d_dst])
                    else:
                        kb.op("dve", lambda e: e.tensor_copy(out=dst_fn(c), in_=pt[:, (c - c0) * 128:(c - c0 + 1) * 128]), reads=[d_pt], writes=[d_dst])

        if "B" in phases:
          with ExitStack() as es:
            wi = sbt(es, "wi", [128, 8, 2560]); d_wi = [Dep() for _ in range(8)]
            for kc in range(8):
                kb.dma("sp", lambda q: q.dma_start(out=wi[:, kc, :], in_=w_in[l][kc * 128:(kc + 1) * 128, :]), writes=[d_wi[kc]])
            wsT = sbt(es, "wsT", [128, 8, 128]); d_wsT = Dep()
            kb.dma("sp", lambda q: q.dma_start(out=wsT[:], in_=gm_wsT[l]), writes=[d_wsT])
            kb.op("dve", lambda e: e.memset(wsT[64:128, :, 0:64], 0.0), writes=[d_wsT])
            bsT = sbt(es, "bsT", [128, 8]); d_bsT = Dep()
            kb.dma("sp", lambda q: q.dma_start(out=bsT[:], in_=gm_bsT[l]), writes=[d_bsT])
            vg = sbt(es, "vg", [128, 512]); d_vg = Dep()
            kb.dma("sp", lambda q: q.dma_start(out=vg[:], in_=gm_vnorm[l].partition_broadcast(128)), writes=[d_vg])
            ona = sbt(es, "ona", [128, 512]); d_ona = Dep()
            kb.dma("sp", lambda q: q.dma_start(out=ona[:], in_=out_norm_a[l].partition_broadcast(128)), writes=[d_ona])
            xpool = Pool(es, nc, "bx", [128, D], F32, 2)
            hpool = Pool(es, nc, "bh", [128, D], F32, 2)
            junk = sbt(es, "bjunk", [128, D]); d_junk = Dep()
            sspool = Pool(es, nc, "bss", [128, 8], F32, 4)
            hTpool = Pool(es, nc, "bhT", [128, 8, 512], F32, 2)
            t512 = Pool(es, nc, "bt", [128, 512], F32, 12)
            for g in range(NG):
                hT, d_hT = hTpool.get()
                for j in range(4):
                    tb = g * 4 + j
                    xt, d_xt = xpool.get()
                    kb.dma("sp", lambda q: q.dma_start(out=xt[:], in_=x_src[tb * 128:(tb + 1) * 128, :]), writes=[d_xt])
                    h, d_h = hpool.get()
                    norm_mod(es, xt, d_xt, A1, sh1, sspool, junk, d_junk, h, d_h)
                    transpose_to(h, d_h, 8, lambda c: hT[:, c, j * 128:(j + 1) * 128], d_hT)
                for j in range(4):
                    tb = g * 4 + j
                    rows = slice(tb * 128, (tb + 1) * 128)
                    res = {}
                    for name, c0 in (("u", 0), ("v", 512), ("va", 2048)):
                        pt, d_pt = psum.get()
                        for kc in range(8):
                            kb.op("pe", lambda e: e.matmul(pt[:], lhsT=hT[:, kc, j * 128:(j + 1) * 128], rhs=wi[:, kc, c0:c0 + 512],
                                                           start=(kc == 0), stop=(kc == 7)), reads=[d_hT, d_wi[kc]], writes=[d_pt])
                        t, d_t = t512.get()
                        if name == "va":
                            kb.op("act", lambda e: e.copy(out=t[:], in_=pt[:]), reads=[d_pt], writes=[d_t])
                            kb.dma("sp", lambda q: q.dma_start(out=v_s[rows, :], in_=t[:]), reads=[d_t])
                        else:
                            kb.op("act", lambda e: e.activation(out=t[:], in_=pt[:], func=AF.Gelu_apprx_tanh), reads=[d_pt], writes=[d_t])
                        res[name] = (t, d_t)
                    gu, d_gu = res["u"]
                    gv, d_gv = res["v"]
                    sq, d_sq = t512.get()
                    kb.op("pool", lambda e: e.tensor_tensor(out=sq[:], in0=gv[:], in1=gv[:], op=ALU.mult), reads=[d_gv], writes=[d_sq])
                    rh, d_rh = sspool.get()
                    kb.op("dve", lambda e: e.tensor_reduce(out=rh[:], in_=sq[:].rearrange("p (h c) -> p h c", h=8), axis=AX.X, op=ALU.add),
                          reads=[d_sq], writes=[d_rh])
                    kb.op("dve", lambda e: e.tensor_scalar(out=rh[:], in0=rh[:], scalar1=1.0 / 64, scalar2=EPS, op0=ALU.mult, op1=ALU.add),
                          reads=[d_rh], writes=[d_rh])
                    kb.op("act", lambda e: e.sqrt(out=rh[:], in_=rh[:]), reads=[d_rh], writes=[d_rh])
                    kb.op("dve", lambda e: e.reciprocal(out=rh[:], in_=rh[:]), reads=[d_rh], writes=[d_rh])
                    vh, d_vh = t512.get()
                    kb.op("dve", lambda e: e.tensor_tensor(out=vh[:].rearrange("p (h c) -> p h c", h=8), in0=gv[:].rearrange("p (h c) -> p h c", h=8),
                                                           in1=rh[:].unsqueeze(2).to_broadcast([128, 8, 64]), op=ALU.mult),
                          reads=[d_gv, d_rh], writes=[d_vh])
                    kb.op("pool", lambda e: e.tensor_tensor(out=vh[:], in0=vh[:], in1=vg[:], op=ALU.mult), reads=[d_vh, d_vg], writes=[d_vh])
                    pz, d_pz = psum.get()
                    for hh in range(8):
                        kb.op("pe", lambda e: e.matmul(pz[:, hh * 64:(hh + 1) * 64], lhsT=wsT[:, hh, :], rhs=vh[:, hh * 64:(hh + 1) * 64],
                                                       start=True, stop=True), reads=[d_wsT, d_vh], writes=[d_pz])
                    ya, d_ya = t512.get()
                    kb.op("dve", lambda e: e.tensor_tensor(out=ya[:].rearrange("p (h c) -> p h c", h=8), in0=pz[:].rearrange("p (h c) -> p h c", h=8),
                                                           in1=bsT[:].unsqueeze(2).to_broadcast([128, 8, 64]), op=ALU.add),
                          reads=[d_pz, d_bsT], writes=[d_ya])
                    kb.op("pool", lambda e: e.tensor_tensor(out=ya[:], in0=ya[:], in1=gu[:], op=ALU.mult), reads=[d_ya, d_gu], writes=[d_ya])
                    ra, d_ra = rms_scale(es, ya[:], d_ya, 512, sspool, junk[:, 0:512], d_junk)
                    yan, d_yan = t512.get()
                    kb.op("dve", lambda e: e.scalar_tensor_tensor(out=yan[:], in0=ya[:], scalar=ra[:, 0:1], in1=ona[:], op0=ALU.mult, op1=ALU.mult),
                          reads=[d_ya, d_ra, d_ona], writes=[d_yan])
                    kb.dma("sp", lambda q: q.dma_start(out=ya_s[rows, :], in_=yan[:]), reads=[d_yan])
                for cc in range(8):
                    col0 = 1024 + cc * 128
                    pt, d_pt = psum.get()
                    for kc in range(8):
                        kb.op("pe", lambda e: e.matmul(pt[:], lhsT=wi[:, kc, col0:col0 + 128], rhs=hT[:, kc, :], start=(kc == 0), stop=(kc == 7)),
                              reads=[d_hT, d_wi[kc]], writes=[d_pt])
                    t, d_t = t512.get()
                    if cc < 4:
                        kb.op("act", lambda e: e.activation(out=t[:], in_=pt[:], func=AF.Copy, scale=0.125), reads=[d_pt], writes=[d_t])
                        dst = qT_s[cc * 128:(cc + 1) * 128, g * 512:(g + 1) * 512]
                    else:
                        kb.op("act", lambda e: e.copy(out=t[:], in_=pt[:]), reads=[d_pt], writes=[d_t])
                        dst = kT_s[(cc - 4) * 128:(cc - 3) * 128, g * 512:(g + 1) * 512]
                    kb.dma("sp", lambda q: q.dma_start(out=dst, in_=t[:]), reads=[d_t])
            kb.barrier()

        if "C" in phases:
          with ExitStack() as es:
            qpool = Pool(es, nc, "cq", [128, S], F32, 2)
            kpool = Pool(es, nc, "ck", [128, S], F32, 2)
            vpool = Pool(es, nc, "cv", [128, NT, 128], F32, 2)
            epool = Pool(es, nc, "ce", [128, 512], F32, 3)
            sppool = Pool(es, nc, "csp", [128, 512], F32, 4)
            apool = Pool(es, nc, "ca", [128, 512], F32, 4)
            cspool = Pool(es, nc, "ccs", [128, 512], F32, 3)
            ybpool = Pool(es, nc, "cyb", [64, 512], F32, 2)
            tiles = []
            for hp in range(4):
                for hh in range(2):
                    for g in range(NG):
                        nkb = 4 * g + 4
                        for idx, kbk in enumerate(reversed(range(nkb))):
                            tiles.append((hp, hh, g, idx, kbk, nkb))
            loaded = {}
            grp = {}
            st = {}

            def operands(hp):
                if hp not in loaded:
                    qT, d_q = qpool.get()
                    kT, d_k = kpool.get()
                    vv, d_v = vpool.get()
                    kb.dma("sp", lambda q: q.dma_start(out=qT[:], in_=qT_s[hp * 128:(hp + 1) * 128, :]), writes=[d_q])
                    kb.dma("sp", lambda q: q.dma_start(out=kT[:], in_=kT_s[hp * 128:(hp + 1) * 128, :]), writes=[d_k])
                    kb.dma("sp", lambda q: q.dma_start(out=vv[:], in_=v_s[:, hp * 128:(hp + 1) * 128].rearrange("(kb p) d -> p kb d", p=128)),
                           writes=[d_v])
                    loaded[hp] = (qT, d_q, kT, d_k, vv, d_v)
                return loaded[hp]

            def stage1(i):
                hp, hh, g, idx, kbk, nkb = tiles[i]
                qT, d_q, kT, d_k, vv, d_v = operands(hp)
                if hp + 1 < 4 and hh == 1 and g == 0 and idx == 0:
                    operands(hp + 1)
                pr = slice(hh * 64, (hh + 1) * 64)
                qs = slice(g * 512, (g + 1) * 512)
                ks = slice(kbk * 128, (kbk + 1) * 128)
                jd = kbk - 4 * g
                if idx == 0:
                    grp[(hp, hh, g)] = psum_acc.get() + cspool.get()
                pz, d_pz = psum.get()
                kb.op("pe", lambda e: e.matmul(pz[:], lhsT=kT[pr, ks], rhs=qT[pr, qs], start=True, stop=False),
                      reads=[d_q, d_k], writes=[d_pz])
                et, d_et = epool.get()
                kb.op("act", lambda e: e.activation(out=et[:], in_=pz[:], func=AF.Exp), reads=[d_pz], writes=[d_et])
                spt, d_spt = sppool.get()
                kb.op("act", lambda e: e.activation(out=spt[:], in_=et[:], func=AF.Ln, bias=1.0), reads=[d_et], writes=[d_spt])
                if jd >= 0:
                    kb.op("pool", lambda e: e.tensor_tensor(out=spt[:], in0=spt[:], in1=masks[jd], op=ALU.mult),
                          reads=[d_spt, d_consts], writes=[d_spt])
                st[i] = (pz, d_pz, spt, d_spt)

            def stage2(i):
                hp, hh, g, idx, kbk, nkb = tiles[i]
                jd = kbk - 4 * g
                pz, d_pz, spt, d_spt = st[i]
                po, d_po, cs, d_cs = grp[(hp, hh, g)]
                kb.op("pe", lambda e: e.matmul(pz[:], lhsT=negtri, rhs=spt[:], start=False, stop=(idx == 0)),
                      reads=[d_spt, d_consts], writes=[d_pz])
                if idx > 0:
                    kb.op("pe", lambda e: e.matmul(pz[:], lhsT=negones, rhs=cs[:], start=False, stop=True),
                          reads=[d_cs, d_consts], writes=[d_pz])
                at, d_at = apool.get()
                kb.op("act", lambda e: e.activation(out=at[:], in_=pz[:], func=AF.Exp), reads=[d_pz], writes=[d_at])
                if jd >= 0:
                    kb.op("pool", lambda e: e.tensor_tensor(out=at[:], in0=at[:], in1=masks[jd], op=ALU.mult),
                          reads=[d_at, d_consts], writes=[d_at])
                if idx < nkb - 1:
                    if idx == 0:
                        kb.op("dve", lambda e: e.tensor_copy(out=cs[:], in_=spt[:]), reads=[d_spt], writes=[d_cs])
                    else:
                        kb.op("dve", lambda e: e.tensor_tensor(out=cs[:], in0=cs[:], in1=spt[:], op=ALU.add),
                              reads=[d_spt, d_cs], writes=[d_cs])
                st[i] = (at, d_at)

            def stage3(i):
                hp, hh, g, idx, kbk, nkb = tiles[i]
                qT, d_q, kT, d_k, vv, d_v = operands(hp)
                pr = slice(hh * 64, (hh + 1) * 64)
                qs = slice(g * 512, (g + 1) * 512)
                at, d_at = st.pop(i)
                po, d_po, cs, d_cs = grp[(hp, hh, g)]
                kb.op("pe", lambda e: e.matmul(po[0:64, :], lhsT=vv[:, kbk, pr], rhs=at[:], start=(idx == 0), stop=(idx == nkb - 1)),
                      reads=[d_v, d_at], writes=[d_po])
                if idx == nkb - 1:
                    head = hp * 2 + hh
                    yb, d_yb = ybpool.get()
                    kb.op("dve", lambda e: e.tensor_copy(out=yb[:], in_=po[0:64, :]), reads=[d_po], writes=[d_yb])
                    kb.dma("sp", lambda q: q.dma_start(out=ybT_s[head * 64:(head + 1) * 64, qs], in_=yb[:]), reads=[d_yb])
                    del grp[(hp, hh, g)]

            nt = len(tiles)
            for step in range(nt + 2):
                if step < nt:
                    stage1(step)
                if 0 <= step - 1 < nt:
                    stage2(step - 1)
                if 0 <= step - 2 < nt:
                    stage3(step - 2)
            kb.barrier()

        if "D" in phases:
          with ExitStack() as es:
            wo = sbt(es, "wo", [128, 8, D]); d_wo = [Dep() for _ in range(8)]
            for kc in range(8):
                kb.dma("sp", lambda q: q.dma_start(out=wo[:, kc, :], in_=w_out[l][kc * 128:(kc + 1) * 128, :]), writes=[d_wo[kc]])
            gb = sbt(es, "gb", [128, 4]); d_gb = Dep()
            kb.dma("sp", lambda q: q.dma_start(out=gb[:], in_=out_norm_bT[l]), writes=[d_gb])
            xpool = Pool(es, nc, "dx", [128, D], F32, 2)
            xnpool = Pool(es, nc, "dxn", [128, D], F32, 2)
            yanpool = Pool(es, nc, "dyan", [128, 512], F32, 2)
            ybpool = Pool(es, nc, "dyb", [128, 4, 128], F32, 2)
            t512 = Pool(es, nc, "dt", [128, 512], F32, 8)
            sspool = Pool(es, nc, "dss", [128, 8], F32, 4)
            for tb in range(NT):
                rows = slice(tb * 128, (tb + 1) * 128)
                xt, d_xt = xpool.get()
                kb.dma("sp", lambda q: q.dma_start(out=xt[:], in_=x_src[rows, :]), writes=[d_xt])
                yan, d_yan = yanpool.get()
                kb.dma("sp", lambda q: q.dma_start(out=yan[:], in_=ya_s[rows, :]), writes=[d_yan])
                ybT, d_ybT = ybpool.get()
                kb.dma("sp", lambda q: q.dma_start(out=ybT[:], in_=ybT_s[:, rows].rearrange("(c p) t -> p c t", p=128)), writes=[d_ybT])
                yaT, d_yaT = t512.get()
                transpose_to(yan, d_yan, 4, lambda c: yaT[:, c * 128:(c + 1) * 128], d_yaT)
                sqb, d_sqb = t512.get()
                kb.op("pool", lambda e: e.tensor_tensor(out=sqb[:], in0=ybT[:].rearrange("p c t -> p (c t)"), in1=ybT[:].rearrange("p c t -> p (c t)"), op=ALU.mult),
                      reads=[d_ybT], writes=[d_sqb])
                pq, d_pq = psum.get()
                for c in range(4):
                    kb.op("pe", lambda e: e.matmul(pq[:, 0:1], lhsT=sqb[:, c * 128:(c + 1) * 128], rhs=ones_col, start=(c == 0), stop=(c == 3)),
                          reads=[d_sqb, d_consts], writes=[d_pq])
                rb, d_rb = sspool.get()
                kb.op("dve", lambda e: e.tensor_scalar(out=rb[:, 0:1], in0=pq[:, 0:1], scalar1=1.0 / 512, scalar2=EPS, op0=ALU.mult, op1=ALU.add),
                      reads=[d_pq], writes=[d_rb])
                kb.op("act", lambda e: e.sqrt(out=rb[:, 0:1], in_=rb[:, 0:1]), reads=[d_rb], writes=[d_rb])
                kb.op("dve", lambda e: e.reciprocal(out=rb[:, 0:1], in_=rb[:, 0:1]), reads=[d_rb], writes=[d_rb])
                ybg, d_ybg = t512.get()
                for c in range(4):
                    kb.op("pool", lambda e: e.tensor_scalar(out=ybg[:, c * 128:(c + 1) * 128], in0=ybT[:, c, :], scalar1=gb[:, c:c + 1], scalar2=None, op0=ALU.mult),
                          reads=[d_ybT, d_gb], writes=[d_ybg])
                xn, d_xn = xnpool.get()
                for half in range(2):
                    hs = slice(half * 512, (half + 1) * 512)
                    pA, d_pA = psum.get()
                    for c in range(4):
                        kb.op("pe", lambda e: e.matmul(pA[:], lhsT=yaT[:, c * 128:(c + 1) * 128], rhs=wo[:, c, hs], start=(c == 0), stop=(c == 3)),
                              reads=[d_yaT, d_wo[c]], writes=[d_pA])
                    pB, d_pB = psum.get()
                    for c in range(4):
                        kb.op("pe", lambda e: e.matmul(pB[:], lhsT=ybg[:, c * 128:(c + 1) * 128], rhs=wo[:, 4 + c, hs], start=(c == 0), stop=(c == 3)),
                              reads=[d_ybg, d_wo[4 + c]], writes=[d_pB])
                    pas, d_pas = t512.get()
                    kb.op("act", lambda e: e.copy(out=pas[:], in_=pA[:]), reads=[d_pA], writes=[d_pas])
                    mix, d_mix = t512.get()
                    kb.op("dve", lambda e: e.scalar_tensor_tensor(out=mix[:], in0=pB[:], scalar=rb[:, 0:1], in1=pas[:], op0=ALU.mult, op1=ALU.add),
                          reads=[d_pB, d_rb, d_pas], writes=[d_mix])
                    kb.op("pool", lambda e: e.tensor_tensor(out=mix[:], in0=mix[:], in1=g1[:, hs], op=ALU.mult), reads=[d_mix, d_mod], writes=[d_mix])
                    kb.op("dve", lambda e: e.tensor_tensor(out=xn[:, hs], in0=xt[:, hs], in1=mix[:], op=ALU.add), reads=[d_mix, d_xt], writes=[d_xn])
                kb.dma("sp", lambda q: q.dma_start(out=xs[rows, :], in_=xn[:]), reads=[d_xn])
            kb.barrier()

        if "E" in phases:
          with ExitStack() as es:
            NB, CH, NDVE = 8, 4, 0
            NCH = 128 // CH
            kT = sbt(es, "pk", [128, 2, 128]); d_kT = Dep()
            kb.dma("sp", lambda q: q.dma_start(out=kT[:, 0, :], in_=peer_k1T[l]), writes=[d_kT])
            kb.dma("sp", lambda q: q.dma_start(out=kT[:, 1, :], in_=peer_k2T[l]), writes=[d_kT])
            fn_bc = None
            if l == L - 1:
                fn_bc = sbt(es, "fnbc", [128, D]); d_fn = Dep()
                kb.dma("sp", lambda q: q.dma_start(out=fn_bc[:], in_=final_norm.partition_broadcast(128)), writes=[d_fn])
            xpool = Pool(es, nc, "ex", [128, D], F32, 2)
            hpool = Pool(es, nc, "eh", [128, D], F32, 2)
            junk = sbt(es, "ejunk", [128, D]); d_junk = Dep()
            sspool = Pool(es, nc, "ess", [128, 8], F32, 6)
            hTpool = Pool(es, nc, "ehT", [128, 8, 128], F32, 1)
            wqpool = Pool(es, nc, "ewq", [128, 8, 256], F32, 2)
            qTpool = Pool(es, nc, "eqT", [128, 4, 128], F32, 2)
            scpool = Pool(es, nc, "esc", [128, 4, 128], F32, 2)
            wkpool = Pool(es, nc, "ewk", [128, 128], F32, 2)
            v16pool = Pool(es, nc, "ev16", [128, 16, 16], F32, 2)
            i16pool = Pool(es, nc, "ei16", [128, 16, 16], U32, 2)
            i16fpool = Pool(es, nc, "ei16f", [128, 16, 16], F32, 1)
            candpool = Pool(es, nc, "ecand", [128, 16, 16], F32, 2)
            cidpool = Pool(es, nc, "ecid", [128, 16, 16], F32, 2)
            wk2pool = Pool(es, nc, "ewk2", [128, 256], F32, 2)
            j2pool = Pool(es, nc, "ej2", [128, 256], F32, 1)
            tspool = Pool(es, nc, "ets", [128, 8, 16], F32, 2)
            eidfpool = Pool(es, nc, "eeidf", [128, 128], F32, 2)
            eidipool = Pool(es, nc, "eeidi", [128, 128], I32, 2)
            gpool = Pool(es, nc, "eg", [128, 8, 16], F32, 2)
            uvpool = Pool(es, nc, "fuv", [128, 2 * D], F32, NB)
            tmppool = Pool(es, nc, "ftmp", [128, D], F32, 3)
            actpool = Pool(es, nc, "fact", [128, 128], F32, 2)
            coefpool = Pool(es, nc, "fcoef", [128, 128], F32, 2)
            accpool = Pool(es, nc, "facc", [128, D], F32, 2)
            blocks = {}
            chunkdeps = {}

            def p1_gen(tb):
                rows = slice(tb * 128, (tb + 1) * 128)
                xt, d_xt = xpool.get()
                kb.dma("sp", lambda q: q.dma_start(out=xt[:], in_=xs[rows, :]), writes=[d_xt])
                h, d_h = hpool.get()
                norm_mod(es, xt, d_xt, A2, sh2, sspool, junk, d_junk, h, d_h)
                hT, d_hT = hTpool.get()
                transpose_to(h, d_h, 8, lambda c: hT[:, c, :], d_hT)
                v16, d_v16 = v16pool.get()
                i16, d_i16 = i16pool.get()
                yield
                for j0 in range(0, 16, 4):
                    pt, d_pt = psum.get()
                    for jp in range(j0, j0 + 4, 2):
                        wqt, d_wqt = wqpool.get()
                        kb.dma("sp", lambda q: q.dma_start(out=wqt[:], in_=peer_wq[l][:, jp * 128:(jp + 2) * 128].rearrange("(kc p) n -> p kc n", p=128)),
                               writes=[d_wqt])
                        for j in (jp, jp + 1):
                            for kc in range(8):
                                kb.op("pe", lambda e: e.matmul(pt[:, (j - j0) * 128:(j - j0 + 1) * 128], lhsT=wqt[:, kc, (j - jp) * 128:(j - jp + 1) * 128],
                                                               rhs=hT[:, kc, :], start=(kc == 0), stop=(kc == 7)), reads=[d_hT, d_wqt], writes=[d_pt])
                    qT4, d_qT4 = qTpool.get()
                    kb.op("act", lambda e: e.copy(out=qT4[:].rearrange("p a b -> p (a b)"), in_=pt[:]), reads=[d_pt], writes=[d_qT4])
                    psc, d_psc = psum.get()
                    for j in range(j0, j0 + 4):
                        kb.op("pe", lambda e: e.matmul(psc[:, (j - j0) * 128:(j - j0 + 1) * 128], lhsT=qT4[:, j - j0, :], rhs=kT[:, j % 2, :], start=True, stop=True),
                              reads=[d_qT4, d_kT], writes=[d_psc])
                    sc4, d_sc4 = scpool.get()
                    kb.op("act", lambda e: e.copy(out=sc4[:].rearrange("p a b -> p (a b)"), in_=psc[:]), reads=[d_psc], writes=[d_sc4])
                    yield
                    for j in range(j0, j0 + 4):
                        scj = sc4[:, j - j0, :]
                        wk, d_wk = wkpool.get()
                        kb.op("dve", lambda e: e.max(out=v16[:, j, 0:8], in_=scj), reads=[d_sc4], writes=[d_v16])
                        kb.op("dve", lambda e: e.max_index(out=i16[:, j, 0:8], in_max=v16[:, j, 0:8], in_values=scj), reads=[d_sc4, d_v16], writes=[d_i16])
                        kb.op("dve", lambda e: e.match_replace(out=wk[:], in_to_replace=v16[:, j, 0:8], in_values=scj, imm_value=-1e30),
                              reads=[d_sc4, d_v16], writes=[d_wk])
                        kb.op("dve", lambda e: e.max(out=v16[:, j, 8:16], in_=wk[:]), reads=[d_wk], writes=[d_v16])
                        kb.op("dve", lambda e: e.max_index(out=i16[:, j, 8:16], in_max=v16[:, j, 8:16], in_values=wk[:]), reads=[d_wk, d_v16], writes=[d_i16])
                    yield
                i16f, d_i16f = i16fpool.get()
                kb.op("dve", lambda e: e.tensor_copy(out=i16f[:], in_=i16[:]), reads=[d_i16], writes=[d_i16f])
                ts, d_ts = tspool.get()
                eidf, d_eidf = eidfpool.get()
                kb.op("dve", lambda e: e.memset(eidf[:], 0.0), writes=[d_eidf])
                for hd in range(8):
                    j1, j2 = 2 * hd, 2 * hd + 1
                    cand, d_cand = candpool.get()
                    cid, d_cid = cidpool.get()
                    kb.op("dve", lambda e: e.tensor_tensor(out=cand[:], in0=v16[:, j1, :].unsqueeze(2).to_broadcast([128, 16, 16]),
                                                           in1=v16[:, j2:j2 + 1, :].to_broadcast([128, 16, 16]), op=ALU.add),
                          reads=[d_v16], writes=[d_cand])
                    kb.op("dve", lambda e: e.scalar_tensor_tensor(out=cid[:], in0=i16f[:, j1, :].unsqueeze(2).to_broadcast([128, 16, 16]), scalar=128.0,
                                                                  in1=i16f[:, j2:j2 + 1, :].to_broadcast([128, 16, 16]), op0=ALU.mult, op1=ALU.add),
                          reads=[d_i16f], writes=[d_cid])
                    candf = cand[:].rearrange("p a b -> p (a b)")
                    cidf = cid[:].rearrange("p a b -> p (a b)")
                    wk2, d_wk2 = wk2pool.get()
                    kb.op("dve", lambda e: e.max(out=ts[:, hd, 0:8], in_=candf), reads=[d_cand], writes=[d_ts])
                    kb.op("dve", lambda e: e.match_replace(out=wk2[:], in_to_replace=ts[:, hd, 0:8], in_values=candf, imm_value=-1e30),
                          reads=[d_cand, d_ts], writes=[d_wk2])
                    kb.op("dve", lambda e: e.max(out=ts[:, hd, 8:16], in_=wk2[:]), reads=[d_wk2], writes=[d_ts])
                    j2t, d_j2t = j2pool.get()
                    for sl in range(16):
                        kb.op("dve", lambda e: e.scalar_tensor_tensor(out=j2t[:], in0=candf, scalar=ts[:, hd, sl:sl + 1], in1=cidf, op0=ALU.is_equal, op1=ALU.mult,
                                                                      accum_out=eidf[:, hd * 16 + sl:hd * 16 + sl + 1]),
                              reads=[d_cand, d_cid, d_ts], writes=[d_j2t, d_eidf])
                        if sl % 8 == 7:
                            yield
                eidi, d_eidi = eidipool.get()
                kb.op("dve", lambda e: e.tensor_scalar(out=eidf[:], in0=eidf[:], scalar1=float(NEXP - 1), scalar2=float(l * NEXP), op0=ALU.min, op1=ALU.add),
                      reads=[d_eidf], writes=[d_eidf])
                kb.op("dve", lambda e: e.tensor_copy(out=eidi[:], in_=eidf[:]), reads=[d_eidf], writes=[d_eidi])
                gt, d_gt = gpool.get()
                kb.op("dve", lambda e: e.tensor_tensor(out=gt[:], in0=ts[:], in1=ts[:, :, 0:1].to_broadcast([128, 8, 16]), op=ALU.subtract),
                      reads=[d_ts], writes=[d_gt])
                kb.op("act", lambda e: e.activation(out=gt[:], in_=gt[:], func=AF.Exp), reads=[d_gt], writes=[d_gt])
                sm, d_sm = sspool.get()
                kb.op("dve", lambda e: e.tensor_reduce(out=sm[:], in_=gt[:], axis=AX.X, op=ALU.add), reads=[d_gt], writes=[d_sm])
                kb.op("dve", lambda e: e.reciprocal(out=sm[:], in_=sm[:]), reads=[d_sm], writes=[d_sm])
                kb.op("dve", lambda e: e.tensor_tensor(out=gt[:], in0=gt[:], in1=sm[:].unsqueeze(2).to_broadcast([128, 8, 16]), op=ALU.mult),
                      reads=[d_gt, d_sm], writes=[d_gt])
                blocks[tb] = dict(xt=xt, d_xt=d_xt, h=h, d_h=d_h, eid=eidi, d_eid=d_eidi, gt=gt[:].rearrange("p a b -> p (a b)"), d_gt=d_gt, bufs={})

            def gathers(bk, c):
                for j in range(c * CH, (c + 1) * CH):
                    uv, d_uv = uvpool.get()
                    kb.dma("pool", lambda q: q.indirect_dma_start(out=uv[:], out_offset=None, in_=peer_uv,
                                                                  in_offset=bass.IndirectOffsetOnAxis(ap=bk["eid"][:, j:j + 1], axis=0)),
                           reads=[bk["d_eid"]], writes=[d_uv])
                    bk["bufs"][j] = (uv, d_uv)

            for _ in p1_gen(0):
                pass
            gathers(blocks[0], 0)
            for tb in range(NT):
                rows = slice(tb * 128, (tb + 1) * 128)
                cur = blocks.pop(tb)
                gen = p1_gen(tb + 1) if tb + 1 < NT else iter(())
                xt, d_xt, gt, d_gt, h, d_h = cur["xt"], cur["d_xt"], cur["gt"], cur["d_gt"], cur["h"], cur["d_h"]
                act, _ = actpool.get()
                coef, _ = coefpool.get()
                d_actc = chunkdeps.setdefault(id(act), [Dep() for _ in range(NCH)])
                d_coefc = chunkdeps.setdefault(id(coef), [Dep() for _ in range(NCH)])
                acc, d_acc = accpool.get()
                pa0, d_pa0 = psum_acc.get()
                pa1, d_pa1 = psum_acc.get()
                state = {"pe": 0, "dve": 0}

                def dots(c):
                    cs_ = slice(c * CH, (c + 1) * CH)
                    kb.op("dve", lambda e: e.memset(act[:, cs_], 0.0), reads=[], writes=[d_actc[c]])
                    for j in range(c * CH, (c + 1) * CH):
                        uv, d_uv = cur["bufs"][j]
                        kb.op("dve", lambda e: e.scalar_tensor_tensor(out=junk[:], in0=uv[:, 0:D], scalar=1.0, in1=h[:], op0=ALU.mult, op1=ALU.mult,
                                                                      accum_out=act[:, j:j + 1]), reads=[d_uv, d_h], writes=[d_junk, d_actc[c]])
                    kb.op("act", lambda e: e.activation(out=coef[:, cs_], in_=act[:, cs_], func=AF.Gelu_apprx_tanh), reads=[d_actc[c]], writes=[d_coefc[c]])
                    kb.op("dve", lambda e: e.tensor_tensor(out=coef[:, cs_], in0=coef[:, cs_], in1=gt[:, cs_], op=ALU.mult),
                          reads=[d_coefc[c], d_gt], writes=[d_coefc[c]])

                def vside(c):
                    for jj, j in enumerate(range(c * CH, (c + 1) * CH)):
                        uv, d_uv = cur["bufs"].pop(j)
                        if jj < NDVE:
                            if state["dve"] == 0:
                                kb.op("dve", lambda e: e.tensor_scalar(out=acc[:], in0=uv[:, D:2 * D], scalar1=coef[:, j:j + 1], scalar2=None, op0=ALU.mult),
                                      reads=[d_uv, d_coefc[c]], writes=[d_acc])
                            else:
                                kb.op("dve", lambda e: e.scalar_tensor_tensor(out=acc[:], in0=uv[:, D:2 * D], scalar=coef[:, j:j + 1], in1=acc[:],
                                                                              op0=ALU.mult, op1=ALU.add), reads=[d_uv, d_coefc[c], d_acc], writes=[d_acc])
                            state["dve"] += 1
                        else:
                            tmp, d_tmp = tmppool.get()
                            kb.op("act", lambda e: e.activation(out=tmp[:], in_=uv[:, D:2 * D], func=AF.Copy, scale=coef[:, j:j + 1]),
                                  reads=[d_uv, d_coefc[c]], writes=[d_tmp])
                            first = state["pe"] == 0
                            last = (j == 127)
                            kb.op("pe", lambda e: e.matmul(pa0[:], lhsT=ident, rhs=tmp[:, 0:512], start=first, stop=last), reads=[d_tmp, d_consts], writes=[d_pa0])
                            kb.op("pe", lambda e: e.matmul(pa1[:], lhsT=ident, rhs=tmp[:, 512:1024], start=first, stop=last), reads=[d_tmp, d_consts], writes=[d_pa1])
                            state["pe"] += 1

                for c in range(NCH):
                    if c + 1 < NCH:
                        gathers(cur, c + 1)
                    else:
                        for _ in gen:
                            pass
                        if tb + 1 < NT:
                            gathers(blocks[tb + 1], 0)
                    dots(c)
                    next(gen, None)
                    vside(c)
                if NDVE > 0:
                    kb.op("dve", lambda e: e.tensor_tensor(out=acc[:, 0:512], in0=pa0[:], in1=acc[:, 0:512], op=ALU.add), reads=[d_pa0, d_acc], writes=[d_acc])
                    kb.op("dve", lambda e: e.tensor_tensor(out=acc[:, 512:1024], in0=pa1[:], in1=acc[:, 512:1024], op=ALU.add), reads=[d_pa1, d_acc], writes=[d_acc])
                    kb.op("dve", lambda e: e.tensor_tensor(out=acc[:], in0=acc[:], in1=g2, op=ALU.mult), reads=[d_acc, d_mod], writes=[d_acc])
                else:
                    kb.op("dve", lambda e: e.tensor_tensor(out=acc[:, 0:512], in0=pa0[:], in1=g2[:, 0:512], op=ALU.mult), reads=[d_pa0, d_mod], writes=[d_acc])
                    kb.op("dve", lambda e: e.tensor_tensor(out=acc[:, 512:1024], in0=pa1[:], in1=g2[:, 512:1024], op=ALU.mult), reads=[d_pa1, d_mod], writes=[d_acc])
                kb.op("dve", lambda e: e.tensor_tensor(out=acc[:], in0=acc[:], in1=xt[:], op=ALU.add), reads=[d_acc, d_xt], writes=[d_acc])
                if l == L - 1:
                    rs, d_rs = rms_scale(es, acc[:], d_acc, D, sspool, junk[:], d_junk)
                    kb.op("dve", lambda e: e.scalar_tensor_tensor(out=acc[:], in0=acc[:], scalar=rs[:, 0:1], in1=fn_bc[:], op0=ALU.mult, op1=ALU.mult),
                          reads=[d_acc, d_rs, d_fn], writes=[d_acc])
                    kb.dma("sp", lambda q: q.dma_start(out=out[rows, :], in_=acc[:]), reads=[d_acc])
                else:
                    kb.dma("sp", lambda q: q.dma_start(out=xs[rows, :], in_=acc[:]), reads=[d_acc])
            kb.barrier()

    if dbg:
        for name, src, shape, dt in (("d_xs", xs, [S, D], F32), ("d_qT", qT_s, [512, S], F32), ("d_kT", kT_s, [512, S], F32),
                                     ("d_v", v_s, [S, 512], F32), ("d_ya", ya_s, [S, 512], F32), ("d_ybT", ybT_s, [512, S], F32),
                                     ("d_eid", eid_s, [S, 128], I32), ("d_g", g_s, [S, 128], F32)):
            o = nc.dram_tensor(name, shape, dt, kind="ExternalOutput").ap()
            kb.dma("sp", lambda q: q.dma_start(out=o, in_=src))
        kb.barrier()
    print("instructions", kb.nins, "waits", kb.nwait)
    return nc


def prep_inputs(inp, b, L):
    f = lambda a: np.ascontiguousarray(a, dtype=np.float32)
    m = {
        "x": f(inp["x"][b]),
        "c": f(inp["c"][b].reshape(8, 128).T),
        "ada_w": f(inp["ada_w"][:L]),
        "ada_b": f(inp["ada_b"][:L]),
        "norm_mix": f(inp["norm_mix"][:L]),
        "norm_ffn": f(inp["norm_ffn"][:L]),
        "w_in": f(inp["w_in"][:L]),
        "gm_wsT": f(np.transpose(inp["gm_ws"][:L], (0, 3, 1, 2))),
        "gm_bsT": f(np.transpose(inp["gm_bs"][:L], (0, 2, 1))),
        "gm_vnorm": f(inp["gm_vnorm"][:L]),
        "out_norm_a": f(inp["out_norm_a"][:L]),
        "out_norm_bT": f(np.transpose(inp["out_norm_b"][:L].reshape(L, 4, 128), (0, 2, 1))),
        "w_out": f(inp["w_out"][:L]),
        "peer_wq": f(inp["peer_wq"][:L]),
        "peer_k1T": f(np.transpose(inp["peer_k1"][:L], (0, 2, 1))),
        "peer_k2T": f(np.transpose(inp["peer_k2"][:L], (0, 2, 1))),
        "peer_uv": np.concatenate([f(inp["peer_u"][:L]).reshape(L * NEXP, D), f(inp["peer_v"][:L]).reshape(L * NEXP, D)], axis=1),
        "final_norm": f(inp["final_norm"]),
        "consts": make_consts(),
    }
    return m


def kernel(**inputs):
    inp = {k: np.asarray(v) for k, v in inputs.items()}
    B, S, _ = inp["x"].shape
    L = inp["ada_w"].shape[0]
    nc = build(L, S)
    shared = prep_inputs(inp, 0, L)
    in_maps = []
    for b in range(B):
        m = dict(shared)
        m["x"] = np.ascontiguousarray(inp["x"][b], dtype=np.float32)
        m["c"] = np.ascontiguousarray(inp["c"][b].reshape(8, 128).T, dtype=np.float32)
        in_maps.append(m)
    res = run_bass_kernel_spmd(nc, in_maps, core_ids=list(range(B)))
    return np.stack([r["out"] for r in res.results], axis=0).astype(np.float32)
```

```python
import numpy as np
from contextlib import ExitStack
import concourse.bass as bass
import concourse.mybir as mybir
from concourse.bass_utils import run_bass_kernel_spmd

F32 = mybir.dt.float32
I32 = mybir.dt.int32
U32 = mybir.dt.uint32
AF = mybir.ActivationFunctionType
ALU = mybir.AluOpType
AX = mybir.AxisListType

D = 1024
NEXP = 16384
EPS = 1e-6
NDS = 40
NSW = 16
SAME_ENG_SYNC = True


class Dep:
    __slots__ = ("w", "r")

    def __init__(self):
        self.w = None
        self.r = {}


class KB:
    def __init__(self, nc):
        self.nc = nc
        self.es = ExitStack()
        self.eng = {"pe": nc.tensor, "act": nc.scalar, "dve": nc.vector, "pool": nc.gpsimd, "sp": nc.sync}
        self.esem = {e: self.es.enter_context(nc.semaphore("es_" + e)) for e in self.eng}
        self.ecount = {e: 0 for e in self.eng}
        self.known = {e: {} for e in self.eng}
        self.dsem = [self.es.enter_context(nc.semaphore("ds%d" % i)) for i in range(NDS + NSW)]
        self.dcount = [0] * (NDS + NSW)
        self.dnext = 0
        self.swnext = 0
        self.nwait = 0
        self.nins = 0

    def _sem(self, key):
        return self.esem[key[1]] if key[0] == "e" else self.dsem[key[1]]

    def _need(self, e, deps):
        need = {}
        for key, val in deps:
            if key[0] == "e" and key[1] == e:
                if e == "pe" or not SAME_ENG_SYNC:
                    continue
            if self.known[e].get(key, 0) >= val:
                continue
            if need.get(key, 0) < val:
                need[key] = val
        return list(need.items())

    def _wait(self, e, deps, keep_one=False):
        need = self._need(e, deps)
        emb = None
        if keep_one and need:
            emb = need.pop()
        for key, val in need:
            self.eng[e].wait_ge(self._sem(key), val)
            self.known[e][key] = val
            self.nwait += 1
        return emb

    def _embed(self, e, ins, emb):
        if emb is not None:
            key, val = emb
            ins._wait_ge(self._sem(key), val)
            self.known[e][key] = val

    @staticmethod
    def _collect(reads, writes):
        deps = []
        for d in reads:
            if d.w is not None:
                deps.append(d.w)
        for d in writes:
            if d.w is not None:
                deps.append(d.w)
            deps.extend(d.r.items())
        return deps

    @staticmethod
    def _record(ev, reads, writes):
        key, val = ev
        for d in reads:
            if d.r.get(key, 0) < val:
                d.r[key] = val
        for d in writes:
            d.w = ev
            d.r = {}

    def op(self, e, fn, reads=(), writes=()):
        emb = self._wait(e, self._collect(reads, writes), keep_one=True)
        ins = fn(self.eng[e])
        self._embed(e, ins, emb)
        self.ecount[e] += 1
        ins.then_inc(self.esem[e], 1)
        self._record((("e", e), self.ecount[e]), reads, writes)
        self.nins += 1

    def dma(self, q, fn, reads=(), writes=()):
        deps = self._collect(reads, writes)
        if q == "pool":
            k = NDS + self.swnext
            self.swnext = (self.swnext + 1) % NSW
        else:
            k = self.dnext
            self.dnext = (k + 1) % NDS
        if self.dcount[k] > 0:
            deps.append((("d", k), self.dcount[k]))
        emb = self._wait(q, deps, keep_one=True)
        ins = fn(self.eng[q])
        self._embed(q, ins, emb)
        self.dcount[k] += 16
        ins.then_inc(self.dsem[k], 16)
        self._record((("d", k), self.dcount[k]), reads, writes)
        self.nins += 1

    def barrier(self):
        allev = [(("e", e), c) for e, c in self.ecount.items() if c > 0]
        allev += [(("d", k), c) for k, c in enumerate(self.dcount) if c > 0]
        for e in self.eng:
            for key, val in allev:
                if self.known[e].get(key, 0) < val:
                    self.eng[e].wait_ge(self._sem(key), val)
                    self.known[e][key] = val
                    self.nwait += 1


class Pool:
    uid = 0

    def __init__(self, es, nc, name, shape, dt, n, psum=False):
        self.t = []
        for i in range(n):
            mk = nc.psum_tensor if psum else nc.sbuf_tensor
            Pool.uid += 1
            self.t.append((es.enter_context(mk("%s_%d_%d" % (name, i, Pool.uid), shape, dt)), Dep()))
        self.i = 0

    def get(self):
        r = self.t[self.i]
        self.i = (self.i + 1) % len(self.t)
        return r

    @classmethod
    def join(cls, *pools):
        p = cls.__new__(cls)
        p.t = [x for q in pools for x in q.t]
        p.i = 0
        return p


def make_consts():
    c = np.zeros((128, 128 * 3 + 4 * 512 + 1), np.float32)
    c[:, 0:128] = np.eye(128, dtype=np.float32)
    j = np.arange(128)[:, None]
    s = np.arange(128)[None, :]
    c[:, 128:256] = -(j >= s).astype(np.float32)
    c[:, 256:384] = -1.0
    t = np.arange(512)[None, :]
    for jd in range(4):
        c[:, 384 + jd * 512:384 + (jd + 1) * 512] = ((128 * jd + j) < t).astype(np.float32)
    c[:, 384 + 2048] = 1.0
    return c


def build(L, S, dbg=False, phases="ABCDEF"):
    nc = bass.Bass("TRN2", target_bir_lowering=False)
    NT = S // 128
    NG = S // 512
    assert S % 512 == 0

    def din(name, shape, dt=F32):
        return nc.dram_tensor(name, shape, dt, kind="ExternalInput").ap()

    def dscr(name, shape, dt=F32):
        return nc.dram_tensor(name, shape, dt, kind="Internal").ap()

    x_in = din("x", [S, D])
    c_in = din("c", [128, 8])
    ada_w = din("ada_w", [L, D, 6 * D])
    ada_b = din("ada_b", [L, 6 * D])
    norm_mix = din("norm_mix", [L, D])
    norm_ffn = din("norm_ffn", [L, D])
    w_in = din("w_in", [L, D, 2560])
    gm_wsT = din("gm_wsT", [L, 128, 8, 128])
    gm_bsT = din("gm_bsT", [L, 128, 8])
    gm_vnorm = din("gm_vnorm", [L, 512])
    out_norm_a = din("out_norm_a", [L, 512])
    out_norm_bT = din("out_norm_bT", [L, 128, 4])
    w_out = din("w_out", [L, D, D])
    peer_wq = din("peer_wq", [L, D, 2048])
    peer_k1T = din("peer_k1T", [L, 128, 128])
    peer_k2T = din("peer_k2T", [L, 128, 128])
    peer_uv = din("peer_uv", [L * NEXP, 2 * D])
    final_norm = din("final_norm", [D])
    consts_in = din("consts", [128, 2433])
    out = nc.dram_tensor("out", [S, D], F32, kind="ExternalOutput").ap()

    xs = dscr("xs", [S, D])
    qT_s = dscr("qT_s", [512, S])
    kT_s = dscr("kT_s", [512, S])
    v_s = dscr("v_s", [S, 512])
    ya_s = dscr("ya_s", [S, 512])
    ybT_s = dscr("ybT_s", [512, S])

    kb = KB(nc)
    top = kb.es

    def sbt(es, name, shape, dt=F32):
        Pool.uid += 1
        return es.enter_context(nc.sbuf_tensor("%s_t%d" % (name, Pool.uid), shape, dt))

    consts = sbt(top, "consts", [128, 2433]); d_consts = Dep()
    ident = consts[:, 0:128]
    negtri = consts[:, 128:256]
    negones = consts[:, 256:384]
    masks = [consts[:, 384 + jd * 512:384 + (jd + 1) * 512] for jd in range(4)]
    ones_col = consts[:, 2432:2433]
    mod = sbt(top, "mod", [128, 6, D]); d_mod = Dep()
    c_act = sbt(top, "c_act", [128, 8]); d_cact = Dep()
    psum = Pool(top, nc, "ps", [128, 512], F32, 6, psum=True)
    psum_acc = Pool(top, nc, "psacc", [128, 512], F32, 2, psum=True)
    psum8 = Pool.join(psum, psum_acc)

    kb.dma("sp", lambda q: q.dma_start(out=consts[:], in_=consts_in), writes=[d_consts])
    kb.dma("sp", lambda q: q.dma_start(out=c_act[:], in_=c_in), writes=[d_cact])
    with ExitStack() as es:
        sg = sbt(es, "sg", [128, 8]); d_sg = Dep()
        kb.op("act", lambda e: e.activation(out=sg[:], in_=c_act[:], func=AF.Sigmoid), reads=[d_cact], writes=[d_sg])
        kb.op("dve", lambda e: e.tensor_tensor(out=c_act[:], in0=c_act[:], in1=sg[:], op=ALU.mult), reads=[d_sg, d_cact], writes=[d_cact])
        kb.barrier()

    def rms_scale(es_pool, src, d_src, nfeat, ss_pool, junk, d_junk):
        ss, d_ss = ss_pool.get()
        s1 = ss[:, 0:1]
        kb.op("dve", lambda e: e.memset(s1, 0.0), writes=[d_ss])
        kb.op("dve", lambda e: e.scalar_tensor_tensor(out=junk, in0=src, scalar=1.0, in1=src, op0=ALU.mult, op1=ALU.mult,
                                                      accum_out=s1), reads=[d_src], writes=[d_junk, d_ss])
        kb.op("dve", lambda e: e.tensor_scalar(out=s1, in0=s1, scalar1=1.0 / nfeat, scalar2=EPS, op0=ALU.mult, op1=ALU.add),
              reads=[d_ss], writes=[d_ss])
        kb.op("act", lambda e: e.sqrt(out=s1, in_=s1), reads=[d_ss], writes=[d_ss])
        kb.op("dve", lambda e: e.reciprocal(out=s1, in_=s1), reads=[d_ss], writes=[d_ss])
        return ss, d_ss

    for l in range(L):
        x_src = x_in if l == 0 else xs
        with ExitStack() as es:
            c_rep = sbt(es, "c_rep", [128, 8, 128]); d_crep = Dep()
            kb.op("dve", lambda e: e.tensor_copy(out=c_rep[:], in_=c_act[:].unsqueeze(2).to_broadcast([128, 8, 128])),
                  reads=[d_cact], writes=[d_crep])
            wpool = Pool(es, nc, "adaw", [128, 8, 512], F32, 2)
            bpool = Pool(es, nc, "adab", [128, 512], F32, 2)
            nm = sbt(es, "nm", [128, 2, D]); d_nm = Dep()
            kb.dma("sp", lambda q: q.dma_start(out=nm[:, 0, :], in_=norm_mix[l].partition_broadcast(128)), writes=[d_nm])
            kb.dma("sp", lambda q: q.dma_start(out=nm[:, 1, :], in_=norm_ffn[l].partition_broadcast(128)), writes=[d_nm])
            for n in range(12):
                wt, d_wt = wpool.get()
                bt, d_bt = bpool.get()
                kb.dma("sp", lambda q: q.dma_start(out=wt[:], in_=ada_w[l][:, n * 512:(n + 1) * 512].rearrange("(kc p) n -> p kc n", p=128)),
                       writes=[d_wt])
                kb.dma("sp", lambda q: q.dma_start(out=bt[:], in_=ada_b[l, n * 512:(n + 1) * 512].partition_broadcast(128)), writes=[d_bt])
                pt, d_pt = psum.get()
                for kc in range(8):
                    kb.op("pe", lambda e: e.matmul(pt[:], lhsT=c_rep[:, kc, :], rhs=wt[:, kc, :], start=(kc == 0), stop=(kc == 7)),
                          reads=[d_crep, d_wt], writes=[d_pt])
                dst = mod[:, n // 2, (n % 2) * 512:(n % 2 + 1) * 512]
                kb.op("dve", lambda e: e.tensor_tensor(out=dst, in0=pt[:], in1=bt[:], op=ALU.add), reads=[d_pt, d_bt], writes=[d_mod])
            kb.op("dve", lambda e: e.scalar_tensor_tensor(out=mod[:, 1, :], in0=mod[:, 1, :], scalar=1.0, in1=nm[:, 0, :], op0=ALU.add, op1=ALU.mult),
                  reads=[d_nm, d_mod], writes=[d_mod])
            kb.op("dve", lambda e: e.scalar_tensor_tensor(out=mod[:, 4, :], in0=mod[:, 4, :], scalar=1.0, in1=nm[:, 1, :], op0=ALU.add, op1=ALU.mult),
                  reads=[d_nm, d_mod], writes=[d_mod])
            kb.barrier()
        sh1, A1, g1, sh2, A2, g2 = (mod[:, i, :] for i in range(6))

        def norm_mod(es, xt, d_xt, A, sh, sspool, junk, d_junk, h, d_h):
            rs, d_rs = rms_scale(es, xt[:], d_xt, D, sspool, junk[:], d_junk)
            kb.op("dve", lambda e: e.scalar_tensor_tensor(out=h[:], in0=xt[:], scalar=rs[:, 0:1], in1=A, op0=ALU.mult, op1=ALU.mult),
                  reads=[d_xt, d_rs, d_mod], writes=[d_h])
            kb.op("pool", lambda e: e.tensor_tensor(out=h[:], in0=h[:], in1=sh, op=ALU.add), reads=[d_h, d_mod], writes=[d_h])

        def transpose_to(src, d_src, nchunk, dst_fn, d_dst, eng="act", pp=None):
            pp = pp or psum
            for c0 in range(0, nchunk, 4):
                pt, d_pt = pp.get()
                n = min(4, nchunk - c0)
                for c in range(c0, c0 + n):
                    kb.op("pe", lambda e: e.transpose(out=pt[:, (c - c0) * 128:(c - c0 + 1) * 128], in_=src[:, c * 128:(c + 1) * 128], identity=ident),
                          reads=[d_src, d_consts], writes=[d_pt])
                for c in range(c0, c0 + n):
                    if eng == "act":
                        kb.op("act", lambda e: e.copy(out=dst_fn(c), in_=pt[:, (c - c0) * 128:(c - c0 + 1) * 128]), reads=[d_pt], writes=[d_dst])
                    else:
                        kb.op("dve", lambda e: e.tensor_copy(out=dst_fn(c), in_=pt[:, (c - c0) * 128:(c - c0 + 1) * 128]), reads=[d_pt], writes=[d_dst])

        if "B" in phases:
          with ExitStack() as es:
            wi = sbt(es, "wi", [128, 8, 2560]); d_wi = [Dep() for _ in range(8)]
            for kc in range(8):
                kb.dma("sp", lambda q: q.dma_start(out=wi[:, kc, :], in_=w_in[l][kc * 128:(kc + 1) * 128, :]), writes=[d_wi[kc]])
            wsT = sbt(es, "wsT", [128, 8, 128]); d_wsT = Dep()
            kb.dma("sp", lambda q: q.dma_start(out=wsT[:], in_=gm_wsT[l]), writes=[d_wsT])
            kb.op("dve", lambda e: e.memset(wsT[64:128, :, 0:64], 0.0), writes=[d_wsT])
            bsT = sbt(es, "bsT", [128, 8]); d_bsT = Dep()
            kb.dma("sp", lambda q: q.dma_start(out=bsT[:], in_=gm_bsT[l]), writes=[d_bsT])
            vg = sbt(es, "vg", [128, 512]); d_vg = Dep()
            kb.dma("sp", lambda q: q.dma_start(out=vg[:], in_=gm_vnorm[l].partition_broadcast(128)), writes=[d_vg])
            ona = sbt(es, "ona", [128, 512]); d_ona = Dep()
            kb.dma("sp", lambda q: q.dma_start(out=ona[:], in_=out_norm_a[l].partition_broadcast(128)), writes=[d_ona])
            xpool = Pool(es, nc, "bx", [128, D], F32, 2)
            hpool = Pool(es, nc, "bh", [128, D], F32, 4)
            junk = sbt(es, "bjunk", [128, D]); d_junk = Dep()
            sspool = Pool(es, nc, "bss", [128, 8], F32, 8)
            hTpool = Pool(es, nc, "bhT", [128, 8, 512], F32, 1)
            gupool = Pool(es, nc, "bgu", [128, 512], F32, 4)
            gvpool = Pool(es, nc, "bgv", [128, 512], F32, 4)
            vapool = Pool(es, nc, "bva", [128, 512], F32, 2)
            qkpool = Pool(es, nc, "bqk", [128, 512], F32, 2)
            wkpool_b = Pool(es, nc, "bwk", [128, 512], F32, 5)
            hTs = {}

            def b_prep_norm(g):
                hs_ = []
                for j in range(4):
                    tb = g * 4 + j
                    xt, d_xt = xpool.get()
                    kb.dma("sp", lambda q: q.dma_start(out=xt[:], in_=x_src[tb * 128:(tb + 1) * 128, :]), writes=[d_xt])
                    h, d_h = hpool.get()
                    norm_mod(es, xt, d_xt, A1, sh1, sspool, junk, d_junk, h, d_h)
                    hs_.append((h, d_h))
                hTs[("n", g)] = hs_

            def b_prep_T(g):
                hT, d_hT = hTpool.get()
                for j, (h, d_h) in enumerate(hTs.pop(("n", g))):
                    transpose_to(h, d_h, 8, lambda c: hT[:, c, j * 128:(j + 1) * 128], d_hT, pp=psum8)
                hTs[g] = (hT, d_hT)

            def b_proj(g):
                hT, d_hT = hTs[g]
                res = {}
                for j in range(4):
                    tb = g * 4 + j
                    rows = slice(tb * 128, (tb + 1) * 128)
                    for name, c0 in (("u", 0), ("v", 512), ("va", 2048)):
                        pt, d_pt = psum8.get()
                        for kc in range(8):
                            kb.op("pe", lambda e: e.matmul(pt[:], lhsT=hT[:, kc, j * 128:(j + 1) * 128], rhs=wi[:, kc, c0:c0 + 512],
                                                           start=(kc == 0), stop=(kc == 7)), reads=[d_hT, d_wi[kc]], writes=[d_pt])
                        if name == "va":
                            t, d_t = vapool.get()
                            kb.op("act", lambda e: e.copy(out=t[:], in_=pt[:]), reads=[d_pt], writes=[d_t])
                            kb.dma("sp", lambda q: q.dma_start(out=v_s[rows, :], in_=t[:]), reads=[d_t])
                        else:
                            t, d_t = (gupool if name == "u" else gvpool).get()
                            kb.op("act", lambda e: e.activation(out=t[:], in_=pt[:], func=AF.Gelu_apprx_tanh), reads=[d_pt], writes=[d_t])
                        res[(j, name)] = (t, d_t)
                for cc in range(8):
                    col0 = 1024 + cc * 128
                    pt, d_pt = psum8.get()
                    for kc in range(8):
                        kb.op("pe", lambda e: e.matmul(pt[:], lhsT=wi[:, kc, col0:col0 + 128], rhs=hT[:, kc, :], start=(kc == 0), stop=(kc == 7)),
                              reads=[d_hT, d_wi[kc]], writes=[d_pt])
                    t, d_t = qkpool.get()
                    if cc < 4:
                        kb.op("act", lambda e: e.activation(out=t[:], in_=pt[:], func=AF.Copy, scale=0.125), reads=[d_pt], writes=[d_t])
                        dst = qT_s[cc * 128:(cc + 1) * 128, g * 512:(g + 1) * 512]
                    else:
                        kb.op("act", lambda e: e.copy(out=t[:], in_=pt[:]), reads=[d_pt], writes=[d_t])
                        dst = kT_s[(cc - 4) * 128:(cc - 3) * 128, g * 512:(g + 1) * 512]
                    kb.dma("sp", lambda q: q.dma_start(out=dst, in_=t[:]), reads=[d_t])
                return res

            def b_gmlp(g, res):
                for j in range(4):
                    tb = g * 4 + j
                    rows = slice(tb * 128, (tb + 1) * 128)
                    gu, d_gu = res[(j, "u")]
                    gv, d_gv = res[(j, "v")]
                    sq, d_sq = wkpool_b.get()
                    kb.op("pool", lambda e: e.tensor_tensor(out=sq[:], in0=gv[:], in1=gv[:], op=ALU.mult), reads=[d_gv], writes=[d_sq])
                    rh, d_rh = sspool.get()
                    kb.op("dve", lambda e: e.tensor_reduce(out=rh[:], in_=sq[:].rearrange("p (h c) -> p h c", h=8), axis=AX.X, op=ALU.add),
                          reads=[d_sq], writes=[d_rh])
                    kb.op("dve", lambda e: e.tensor_scalar(out=rh[:], in0=rh[:], scalar1=1.0 / 64, scalar2=EPS, op0=ALU.mult, op1=ALU.add),
                          reads=[d_rh], writes=[d_rh])
                    kb.op("act", lambda e: e.sqrt(out=rh[:], in_=rh[:]), reads=[d_rh], writes=[d_rh])
                    kb.op("dve", lambda e: e.reciprocal(out=rh[:], in_=rh[:]), reads=[d_rh], writes=[d_rh])
                    vh, d_vh = wkpool_b.get()
                    kb.op("dve", lambda e: e.tensor_tensor(out=vh[:].rearrange("p (h c) -> p h c", h=8), in0=gv[:].rearrange("p (h c) -> p h c", h=8),
                                                           in1=rh[:].unsqueeze(2).to_broadcast([128, 8, 64]), op=ALU.mult),
                          reads=[d_gv, d_rh], writes=[d_vh])
                    kb.op("pool", lambda e: e.tensor_tensor(out=vh[:], in0=vh[:], in1=vg[:], op=ALU.mult), reads=[d_vh, d_vg], writes=[d_vh])
                    pz, d_pz = psum8.get()
                    for hh in range(8):
                        kb.op("pe", lambda e: e.matmul(pz[:, hh * 64:(hh + 1) * 64], lhsT=wsT[:, hh, :], rhs=vh[:, hh * 64:(hh + 1) * 64],
                                                       start=True, stop=True), reads=[d_wsT, d_vh], writes=[d_pz])
                    ya, d_ya = wkpool_b.get()
                    kb.op("dve", lambda e: e.tensor_tensor(out=ya[:].rearrange("p (h c) -> p h c", h=8), in0=pz[:].rearrange("p (h c) -> p h c", h=8),
                                                           in1=bsT[:].unsqueeze(2).to_broadcast([128, 8, 64]), op=ALU.add),
                          reads=[d_pz, d_bsT], writes=[d_ya])
                    kb.op("pool", lambda e: e.tensor_tensor(out=ya[:], in0=ya[:], in1=gu[:], op=ALU.mult), reads=[d_ya, d_gu], writes=[d_ya])
                    ra, d_ra = rms_scale(es, ya[:], d_ya, 512, sspool, junk[:, 0:512], d_junk)
                    yan, d_yan = wkpool_b.get()
                    kb.op("dve", lambda e: e.scalar_tensor_tensor(out=yan[:], in0=ya[:], scalar=ra[:, 0:1], in1=ona[:], op0=ALU.mult, op1=ALU.mult),
                          reads=[d_ya, d_ra, d_ona], writes=[d_yan])
                    kb.dma("sp", lambda q: q.dma_start(out=ya_s[rows, :], in_=yan[:]), reads=[d_yan])

            b_prep_norm(0)
            b_prep_T(0)
            for g in range(NG):
                if g + 1 < NG:
                    b_prep_norm(g + 1)
                res = b_proj(g)
                if g + 1 < NG:
                    b_prep_T(g + 1)
                b_gmlp(g, res)
            kb.barrier()

        if "C" in phases:
          with ExitStack() as es:
            qpool = Pool(es, nc, "cq", [128, S], F32, 2)
            kpool = Pool(es, nc, "ck", [128, S], F32, 2)
            vpool = Pool(es, nc, "cv", [128, NT, 128], F32, 2)
            epool = Pool(es, nc, "ce", [128, 512], F32, 4)
            sppool = Pool(es, nc, "csp", [128, 512], F32, 8)
            apool = Pool(es, nc, "ca", [128, 512], F32, 8)
            cspool = Pool(es, nc, "ccs", [128, 512], F32, 6)
            ybpool = Pool(es, nc, "cyb", [128, 512], F32, 2)
            tiles = []
            for hp in range(4):
                for g in range(NG):
                    nkb = 4 * g + 4
                    for idx, kbk in enumerate(reversed(range(nkb))):
                        tiles.append((hp, g, idx, kbk, nkb))
            loaded = {}
            grp = {}
            st = {}

            def operands(hp):
                if hp not in loaded:
                    qT, d_q = qpool.get()
                    kT, d_k = kpool.get()
                    vv, d_v = vpool.get()
                    kb.dma("sp", lambda q: q.dma_start(out=qT[:], in_=qT_s[hp * 128:(hp + 1) * 128, :]), writes=[d_q])
                    kb.dma("sp", lambda q: q.dma_start(out=kT[:], in_=kT_s[hp * 128:(hp + 1) * 128, :]), writes=[d_k])
                    kb.dma("sp", lambda q: q.dma_start(out=vv[:], in_=v_s[:, hp * 128:(hp + 1) * 128].rearrange("(kb p) d -> p kb d", p=128)),
                           writes=[d_v])
                    loaded[hp] = (qT, d_q, kT, d_k, vv, d_v)
                return loaded[hp]

            def stage1(i):
                hp, g, idx, kbk, nkb = tiles[i]
                qT, d_q, kT, d_k, vv, d_v = operands(hp)
                if hp + 1 < 4 and g == 0 and idx == 3:
                    operands(hp + 1)
                qs = slice(g * 512, (g + 1) * 512)
                ks = slice(kbk * 128, (kbk + 1) * 128)
                jd = kbk - 4 * g
                if idx == 0:
                    grp[(hp, g)] = psum_acc.get() + cspool.get() + cspool.get()
                pzs = [psum.get() for _ in range(2)]
                for hh in range(2):
                    pr = slice(hh * 64, (hh + 1) * 64)
                    pz, d_pz = pzs[hh]
                    kb.op("pe", lambda e: e.matmul(pz[:], lhsT=kT[pr, ks], rhs=qT[pr, qs], start=True, stop=False, skip_group_check=True),
                          reads=[d_q, d_k], writes=[d_pz])
                sps = []
                for hh in range(2):
                    pz, d_pz = pzs[hh]
                    et, d_et = epool.get()
                    kb.op("act", lambda e: e.activation(out=et[:], in_=pz[:], func=AF.Exp), reads=[d_pz], writes=[d_et])
                    spt, d_spt = sppool.get()
                    kb.op("act", lambda e: e.activation(out=spt[:], in_=et[:], func=AF.Ln, bias=1.0), reads=[d_et], writes=[d_spt])
                    if jd >= 0:
                        kb.op("pool", lambda e: e.tensor_tensor(out=spt[:], in0=spt[:], in1=masks[jd], op=ALU.mult),
                              reads=[d_spt, d_consts], writes=[d_spt])
                    sps.append((spt, d_spt))
                st[i] = (pzs, sps)

            def stage2(i):
                hp, g, idx, kbk, nkb = tiles[i]
                jd = kbk - 4 * g
                pzs, sps = st[i]
                gr = grp[(hp, g)]
                css = [(gr[2], gr[3]), (gr[4], gr[5])]
                for hh in range(2):
                    pz, d_pz = pzs[hh]
                    spt, d_spt = sps[hh]
                    cs, d_cs = css[hh]
                    kb.op("pe", lambda e: e.matmul(pz[:], lhsT=negtri, rhs=spt[:], start=False, stop=(idx == 0), skip_group_check=True),
                          reads=[d_spt, d_consts], writes=[d_pz])
                    if idx > 0:
                        kb.op("pe", lambda e: e.matmul(pz[:], lhsT=negones, rhs=cs[:], start=False, stop=True, skip_group_check=True),
                              reads=[d_cs, d_consts], writes=[d_pz])
                ats = []
                for hh in range(2):
                    pz, d_pz = pzs[hh]
                    spt, d_spt = sps[hh]
                    cs, d_cs = css[hh]
                    at, d_at = apool.get()
                    kb.op("act", lambda e: e.activation(out=at[:], in_=pz[:], func=AF.Exp), reads=[d_pz], writes=[d_at])
                    if jd >= 0:
                        kb.op("pool", lambda e: e.tensor_tensor(out=at[:], in0=at[:], in1=masks[jd], op=ALU.mult),
                              reads=[d_at, d_consts], writes=[d_at])
                    if idx < nkb - 1:
                        if idx == 0:
                            kb.op("dve", lambda e: e.tensor_copy(out=cs[:], in_=spt[:]), reads=[d_spt], writes=[d_cs])
                        else:
                            kb.op("dve", lambda e: e.tensor_tensor(out=cs[:], in0=cs[:], in1=spt[:], op=ALU.add),
                                  reads=[d_spt, d_cs], writes=[d_cs])
                    ats.append((at, d_at))
                st[i] = ats

            def stage3(i):
                hp, g, idx, kbk, nkb = tiles[i]
                qT, d_q, kT, d_k, vv, d_v = operands(hp)
                qs = slice(g * 512, (g + 1) * 512)
                ats = st.pop(i)
                gr = grp[(hp, g)]
                po, d_po = gr[0], gr[1]
                for hh in range(2):
                    pr = slice(hh * 64, (hh + 1) * 64)
                    at, d_at = ats[hh]
                    kb.op("pe", lambda e: e.matmul(po[pr, :], lhsT=vv[:, kbk, pr], rhs=at[:], start=(idx == 0), stop=(idx == nkb - 1)),
                          reads=[d_v, d_at], writes=[d_po])
                if idx == nkb - 1:
                    yb, d_yb = ybpool.get()
                    kb.op("dve", lambda e: e.tensor_copy(out=yb[:], in_=po[:]), reads=[d_po], writes=[d_yb])
                    kb.dma("sp", lambda q: q.dma_start(out=ybT_s[hp * 128:(hp + 1) * 128, qs], in_=yb[:]), reads=[d_yb])
                    del grp[(hp, g)]

            nt = len(tiles)
            for step in range(nt + 2):
                if step < nt:
                    stage1(step)
                if 0 <= step - 1 < nt:
                    stage2(step - 1)
                if 0 <= step - 2 < nt:
                    stage3(step - 2)
            kb.barrier()

        if "D" in phases:
          with ExitStack() as es:
            wo = sbt(es, "wo", [128, 8, D]); d_wo = [Dep() for _ in range(8)]
            for kc in range(8):
                kb.dma("sp", lambda q: q.dma_start(out=wo[:, kc, :], in_=w_out[l][kc * 128:(kc + 1) * 128, :]), writes=[d_wo[kc]])
            gb = sbt(es, "gb", [128, 4]); d_gb = Dep()
            kb.dma("sp", lambda q: q.dma_start(out=gb[:], in_=out_norm_bT[l]), writes=[d_gb])
            xpool = Pool(es, nc, "dx", [128, D], F32, 3)
            xnpool = Pool(es, nc, "dxn", [128, D], F32, 2)
            yanpool = Pool(es, nc, "dyan", [128, 512], F32, 3)
            ybpool = Pool(es, nc, "dyb", [128, 4, 128], F32, 3)
            yaTpool = Pool(es, nc, "dyaT", [128, 512], F32, 2)
            sqbpool = Pool(es, nc, "dsqb", [128, 512], F32, 2)
            ybgpool = Pool(es, nc, "dybg", [128, 512], F32, 2)
            mainpool = Pool(es, nc, "dmain", [128, 512], F32, 4)
            sspool = Pool(es, nc, "dss", [128, 8], F32, 4)
            preps = {}

            def d_prep(tb):
                rows = slice(tb * 128, (tb + 1) * 128)
                xt, d_xt = xpool.get()
                kb.dma("sp", lambda q: q.dma_start(out=xt[:], in_=x_src[rows, :]), writes=[d_xt])
                yan, d_yan = yanpool.get()
                kb.dma("sp", lambda q: q.dma_start(out=yan[:], in_=ya_s[rows, :]), writes=[d_yan])
                ybT, d_ybT = ybpool.get()
                kb.dma("sp", lambda q: q.dma_start(out=ybT[:], in_=ybT_s[:, rows].rearrange("(c p) t -> p c t", p=128)), writes=[d_ybT])
                yaT, d_yaT = yaTpool.get()
                transpose_to(yan, d_yan, 4, lambda c: yaT[:, c * 128:(c + 1) * 128], d_yaT, pp=psum8)
                sqb, d_sqb = sqbpool.get()
                kb.op("act", lambda e: e.activation(out=sqb[:], in_=ybT[:].rearrange("p c t -> p (c t)"), func=AF.Square),
                      reads=[d_ybT], writes=[d_sqb])
                pq, d_pq = psum8.get()
                for c in range(4):
                    kb.op("pe", lambda e: e.matmul(pq[:, 0:1], lhsT=sqb[:, c * 128:(c + 1) * 128], rhs=ones_col, start=(c == 0), stop=(c == 3)),
                          reads=[d_sqb, d_consts], writes=[d_pq])
                rb, d_rb = sspool.get()
                kb.op("dve", lambda e: e.tensor_scalar(out=rb[:, 0:1], in0=pq[:, 0:1], scalar1=1.0 / 512, scalar2=EPS, op0=ALU.mult, op1=ALU.add),
                      reads=[d_pq], writes=[d_rb])
                kb.op("act", lambda e: e.sqrt(out=rb[:, 0:1], in_=rb[:, 0:1]), reads=[d_rb], writes=[d_rb])
                kb.op("dve", lambda e: e.reciprocal(out=rb[:, 0:1], in_=rb[:, 0:1]), reads=[d_rb], writes=[d_rb])
                ybg, d_ybg = ybgpool.get()
                for c in range(4):
                    kb.op("act", lambda e: e.activation(out=ybg[:, c * 128:(c + 1) * 128], in_=ybT[:, c, :], func=AF.Copy, scale=gb[:, c:c + 1]),
                          reads=[d_ybT, d_gb], writes=[d_ybg])
                preps[tb] = (xt, d_xt, yaT, d_yaT, ybg, d_ybg, rb, d_rb)

            def d_main(tb):
                rows = slice(tb * 128, (tb + 1) * 128)
                xt, d_xt, yaT, d_yaT, ybg, d_ybg, rb, d_rb = preps.pop(tb)
                xn, d_xn = xnpool.get()
                for half in range(2):
                    hs = slice(half * 512, (half + 1) * 512)
                    pA, d_pA = psum8.get()
                    for c in range(4):
                        kb.op("pe", lambda e: e.matmul(pA[:], lhsT=yaT[:, c * 128:(c + 1) * 128], rhs=wo[:, c, hs], start=(c == 0), stop=(c == 3)),
                              reads=[d_yaT, d_wo[c]], writes=[d_pA])
                    pB, d_pB = psum8.get()
                    for c in range(4):
                        kb.op("pe", lambda e: e.matmul(pB[:], lhsT=ybg[:, c * 128:(c + 1) * 128], rhs=wo[:, 4 + c, hs], start=(c == 0), stop=(c == 3)),
                              reads=[d_ybg, d_wo[4 + c]], writes=[d_pB])
                    pas, d_pas = mainpool.get()
                    kb.op("act", lambda e: e.copy(out=pas[:], in_=pA[:]), reads=[d_pA], writes=[d_pas])
                    mix, d_mix = mainpool.get()
                    kb.op("dve", lambda e: e.scalar_tensor_tensor(out=mix[:], in0=pB[:], scalar=rb[:, 0:1], in1=pas[:], op0=ALU.mult, op1=ALU.add),
                          reads=[d_pB, d_rb, d_pas], writes=[d_mix])
                    kb.op("dve", lambda e: e.tensor_tensor(out=mix[:], in0=mix[:], in1=g1[:, hs], op=ALU.mult), reads=[d_mix, d_mod], writes=[d_mix])
                    kb.op("dve", lambda e: e.tensor_tensor(out=xn[:, hs], in0=xt[:, hs], in1=mix[:], op=ALU.add), reads=[d_mix, d_xt], writes=[d_xn])
                kb.dma("sp", lambda q: q.dma_start(out=xs[rows, :], in_=xn[:]), reads=[d_xn])

            d_prep(0)
            for tb in range(NT):
                if tb + 1 < NT:
                    d_prep(tb + 1)
                d_main(tb)
            kb.barrier()

        if "E" in phases:
          with ExitStack() as es:
            NB, CH, NDVE = 8, 4, 0
            NCH = 128 // CH
            kT = sbt(es, "pk", [128, 2, 128]); d_kT = Dep()
            kb.dma("sp", lambda q: q.dma_start(out=kT[:, 0, :], in_=peer_k1T[l]), writes=[d_kT])
            kb.dma("sp", lambda q: q.dma_start(out=kT[:, 1, :], in_=peer_k2T[l]), writes=[d_kT])
            fn_bc = None
            if l == L - 1:
                fn_bc = sbt(es, "fnbc", [128, D]); d_fn = Dep()
                kb.dma("sp", lambda q: q.dma_start(out=fn_bc[:], in_=final_norm.partition_broadcast(128)), writes=[d_fn])
            xpool = Pool(es, nc, "ex", [128, D], F32, 2)
            hpool = Pool(es, nc, "eh", [128, D], F32, 2)
            junk = sbt(es, "ejunk", [128, D]); d_junk = Dep()
            sspool = Pool(es, nc, "ess", [128, 8], F32, 6)
            hTpool = Pool(es, nc, "ehT", [128, 8, 128], F32, 1)
            wqpool = Pool(es, nc, "ewq", [128, 8, 256], F32, 2)
            qTpool = Pool(es, nc, "eqT", [128, 4, 128], F32, 2)
            scpool = Pool(es, nc, "esc", [128, 4, 128], F32, 2)
            wkpool = Pool(es, nc, "ewk", [128, 128], F32, 2)
            v16pool = Pool(es, nc, "ev16", [128, 16, 16], F32, 2)
            i16pool = Pool(es, nc, "ei16", [128, 16, 16], U32, 2)
            i16fpool = Pool(es, nc, "ei16f", [128, 16, 16], F32, 1)
            candpool = Pool(es, nc, "ecand", [128, 16, 16], F32, 2)
            cidpool = Pool(es, nc, "ecid", [128, 16, 16], F32, 2)
            wk2pool = Pool(es, nc, "ewk2", [128, 256], F32, 2)
            j2pool = Pool(es, nc, "ej2", [128, 256], F32, 1)
            tspool = Pool(es, nc, "ets", [128, 8, 16], F32, 2)
            eidfpool = Pool(es, nc, "eeidf", [128, 128], F32, 2)
            eidipool = Pool(es, nc, "eeidi", [128, 128], I32, 2)
            gpool = Pool(es, nc, "eg", [128, 8, 16], F32, 2)
            uvpool = Pool(es, nc, "fuv", [128, 2 * D], F32, NB)
            tmppool = Pool(es, nc, "ftmp", [128, D], F32, 3)
            actpool = Pool(es, nc, "fact", [128, 128], F32, 2)
            coefpool = Pool(es, nc, "fcoef", [128, 128], F32, 2)
            accpool = Pool(es, nc, "facc", [128, D], F32, 2)
            blocks = {}
            chunkdeps = {}

            def p1_gen(tb):
                rows = slice(tb * 128, (tb + 1) * 128)
                xt, d_xt = xpool.get()
                kb.dma("sp", lambda q: q.dma_start(out=xt[:], in_=xs[rows, :]), writes=[d_xt])
                h, d_h = hpool.get()
                norm_mod(es, xt, d_xt, A2, sh2, sspool, junk, d_junk, h, d_h)
                hT, d_hT = hTpool.get()
                transpose_to(h, d_h, 8, lambda c: hT[:, c, :], d_hT)
                v16, d_v16 = v16pool.get()
                i16, d_i16 = i16pool.get()
                yield
                for j0 in range(0, 16, 4):
                    pt, d_pt = psum.get()
                    for jp in range(j0, j0 + 4, 2):
                        wqt, d_wqt = wqpool.get()
                        kb.dma("sp", lambda q: q.dma_start(out=wqt[:], in_=peer_wq[l][:, jp * 128:(jp + 2) * 128].rearrange("(kc p) n -> p kc n", p=128)),
                               writes=[d_wqt])
                        for j in (jp, jp + 1):
                            for kc in range(8):
                                kb.op("pe", lambda e: e.matmul(pt[:, (j - j0) * 128:(j - j0 + 1) * 128], lhsT=wqt[:, kc, (j - jp) * 128:(j - jp + 1) * 128],
                                                               rhs=hT[:, kc, :], start=(kc == 0), stop=(kc == 7)), reads=[d_hT, d_wqt], writes=[d_pt])
                    qT4, d_qT4 = qTpool.get()
                    kb.op("act", lambda e: e.copy(out=qT4[:].rearrange("p a b -> p (a b)"), in_=pt[:]), reads=[d_pt], writes=[d_qT4])
                    psc, d_psc = psum.get()
                    for j in range(j0, j0 + 4):
                        kb.op("pe", lambda e: e.matmul(psc[:, (j - j0) * 128:(j - j0 + 1) * 128], lhsT=qT4[:, j - j0, :], rhs=kT[:, j % 2, :], start=True, stop=True),
                              reads=[d_qT4, d_kT], writes=[d_psc])
                    sc4, d_sc4 = scpool.get()
                    kb.op("act", lambda e: e.copy(out=sc4[:].rearrange("p a b -> p (a b)"), in_=psc[:]), reads=[d_psc], writes=[d_sc4])
                    yield
                    for j in range(j0, j0 + 4):
                        scj = sc4[:, j - j0, :]
                        wk, d_wk = wkpool.get()
                        kb.op("dve", lambda e: e.max(out=v16[:, j, 0:8], in_=scj), reads=[d_sc4], writes=[d_v16])
                        kb.op("dve", lambda e: e.max_index(out=i16[:, j, 0:8], in_max=v16[:, j, 0:8], in_values=scj), reads=[d_sc4, d_v16], writes=[d_i16])
                        kb.op("dve", lambda e: e.match_replace(out=wk[:], in_to_replace=v16[:, j, 0:8], in_values=scj, imm_value=-1e30),
                              reads=[d_sc4, d_v16], writes=[d_wk])
                        kb.op("dve", lambda e: e.max(out=v16[:, j, 8:16], in_=wk[:]), reads=[d_wk], writes=[d_v16])
                        kb.op("dve", lambda e: e.max_index(out=i16[:, j, 8:16], in_max=v16[:, j, 8:16], in_values=wk[:]), reads=[d_wk, d_v16], writes=[d_i16])
                    yield
                i16f, d_i16f = i16fpool.get()
                kb.op("dve", lambda e: e.tensor_copy(out=i16f[:], in_=i16[:]), reads=[d_i16], writes=[d_i16f])
                ts, d_ts = tspool.get()
                eidf, d_eidf = eidfpool.get()
                kb.op("dve", lambda e: e.memset(eidf[:], 0.0), writes=[d_eidf])
                for hd in range(8):
                    j1, j2 = 2 * hd, 2 * hd + 1
                    cand, d_cand = candpool.get()
                    cid, d_cid = cidpool.get()
                    kb.op("dve", lambda e: e.tensor_tensor(out=cand[:], in0=v16[:, j1, :].unsqueeze(2).to_broadcast([128, 16, 16]),
                                                           in1=v16[:, j2:j2 + 1, :].to_broadcast([128, 16, 16]), op=ALU.add),
                          reads=[d_v16], writes=[d_cand])
                    kb.op("dve", lambda e: e.scalar_tensor_tensor(out=cid[:], in0=i16f[:, j1, :].unsqueeze(2).to_broadcast([128, 16, 16]), scalar=128.0,
                                                                  in1=i16f[:, j2:j2 + 1, :].to_broadcast([128, 16, 16]), op0=ALU.mult, op1=ALU.add),
                          reads=[d_i16f], writes=[d_cid])
                    candf = cand[:].rearrange("p a b -> p (a b)")
                    cidf = cid[:].rearrange("p a b -> p (a b)")
                    wk2, d_wk2 = wk2pool.get()
                    kb.op("dve", lambda e: e.max(out=ts[:, hd, 0:8], in_=candf), reads=[d_cand], writes=[d_ts])
                    kb.op("dve", lambda e: e.match_replace(out=wk2[:], in_to_replace=ts[:, hd, 0:8], in_values=candf, imm_value=-1e30),
                          reads=[d_cand, d_ts], writes=[d_wk2])
                    kb.op("dve", lambda e: e.max(out=ts[:, hd, 8:16], in_=wk2[:]), reads=[d_wk2], writes=[d_ts])
                    j2t, d_j2t = j2pool.get()
                    for sl in range(16):
                        kb.op("dve", lambda e: e.scalar_tensor_tensor(out=j2t[:], in0=candf, scalar=ts[:, hd, sl:sl + 1], in1=cidf, op0=ALU.is_equal, op1=ALU.mult,
                                                                      accum_out=eidf[:, hd * 16 + sl:hd * 16 + sl + 1]),
                              reads=[d_cand, d_cid, d_ts], writes=[d_j2t, d_eidf])
                        if sl % 8 == 7:
                            yield
                eidi, d_eidi = eidipool.get()
                kb.op("dve", lambda e: e.tensor_scalar(out=eidf[:], in0=eidf[:], scalar1=float(NEXP - 1), scalar2=float(l * NEXP), op0=ALU.min, op1=ALU.add),
                      reads=[d_eidf], writes=[d_eidf])
                kb.op("dve", lambda e: e.tensor_copy(out=eidi[:], in_=eidf[:]), reads=[d_eidf], writes=[d_eidi])
                gt, d_gt = gpool.get()
                kb.op("dve", lambda e: e.tensor_tensor(out=gt[:], in0=ts[:], in1=ts[:, :, 0:1].to_broadcast([128, 8, 16]), op=ALU.subtract),
                      reads=[d_ts], writes=[d_gt])
                kb.op("act", lambda e: e.activation(out=gt[:], in_=gt[:], func=AF.Exp), reads=[d_gt], writes=[d_gt])
                sm, d_sm = sspool.get()
                kb.op("dve", lambda e: e.tensor_reduce(out=sm[:], in_=gt[:], axis=AX.X, op=ALU.add), reads=[d_gt], writes=[d_sm])
                kb.op("dve", lambda e: e.reciprocal(out=sm[:], in_=sm[:]), reads=[d_sm], writes=[d_sm])
                kb.op("dve", lambda e: e.tensor_tensor(out=gt[:], in0=gt[:], in1=sm[:].unsqueeze(2).to_broadcast([128, 8, 16]), op=ALU.mult),
                      reads=[d_gt, d_sm], writes=[d_gt])
                blocks[tb] = dict(xt=xt, d_xt=d_xt, h=h, d_h=d_h, eid=eidi, d_eid=d_eidi, gt=gt[:].rearrange("p a b -> p (a b)"), d_gt=d_gt, bufs={})

            def gathers(bk, c):
                for j in range(c * CH, (c + 1) * CH):
                    uv, d_uv = uvpool.get()
                    kb.dma("pool", lambda q: q.indirect_dma_start(out=uv[:], out_offset=None, in_=peer_uv,
                                                                  in_offset=bass.IndirectOffsetOnAxis(ap=bk["eid"][:, j:j + 1], axis=0)),
                           reads=[bk["d_eid"]], writes=[d_uv])
                    bk["bufs"][j] = (uv, d_uv)

            for _ in p1_gen(0):
                pass
            gathers(blocks[0], 0)
            for tb in range(NT):
                rows = slice(tb * 128, (tb + 1) * 128)
                cur = blocks.pop(tb)
                gen = p1_gen(tb + 1) if tb + 1 < NT else iter(())
                xt, d_xt, gt, d_gt, h, d_h = cur["xt"], cur["d_xt"], cur["gt"], cur["d_gt"], cur["h"], cur["d_h"]
                act, _ = actpool.get()
                coef, _ = coefpool.get()
                d_actc = chunkdeps.setdefault(id(act), [Dep() for _ in range(NCH)])
                d_coefc = chunkdeps.setdefault(id(coef), [Dep() for _ in range(NCH)])
                acc, d_acc = accpool.get()
                pa0, d_pa0 = psum_acc.get()
                pa1, d_pa1 = psum_acc.get()
                state = {"pe": 0, "dve": 0}

                def dots(c):
                    cs_ = slice(c * CH, (c + 1) * CH)
                    kb.op("dve", lambda e: e.memset(act[:, cs_], 0.0), reads=[], writes=[d_actc[c]])
                    for j in range(c * CH, (c + 1) * CH):
                        uv, d_uv = cur["bufs"][j]
                        kb.op("dve", lambda e: e.scalar_tensor_tensor(out=junk[:], in0=uv[:, 0:D], scalar=1.0, in1=h[:], op0=ALU.mult, op1=ALU.mult,
                                                                      accum_out=act[:, j:j + 1]), reads=[d_uv, d_h], writes=[d_junk, d_actc[c]])
                    kb.op("act", lambda e: e.activation(out=coef[:, cs_], in_=act[:, cs_], func=AF.Gelu_apprx_tanh), reads=[d_actc[c]], writes=[d_coefc[c]])
                    kb.op("dve", lambda e: e.tensor_tensor(out=coef[:, cs_], in0=coef[:, cs_], in1=gt[:, cs_], op=ALU.mult),
                          reads=[d_coefc[c], d_gt], writes=[d_coefc[c]])

                def vside(c):
                    for jj, j in enumerate(range(c * CH, (c + 1) * CH)):
                        uv, d_uv = cur["bufs"].pop(j)
                        if jj < NDVE:
                            if state["dve"] == 0:
                                kb.op("dve", lambda e: e.tensor_scalar(out=acc[:], in0=uv[:, D:2 * D], scalar1=coef[:, j:j + 1], scalar2=None, op0=ALU.mult),
                                      reads=[d_uv, d_coefc[c]], writes=[d_acc])
                            else:
                                kb.op("dve", lambda e: e.scalar_tensor_tensor(out=acc[:], in0=uv[:, D:2 * D], scalar=coef[:, j:j + 1], in1=acc[:],
                                                                              op0=ALU.mult, op1=ALU.add), reads=[d_uv, d_coefc[c], d_acc], writes=[d_acc])
                            state["dve"] += 1
                        else:
                            tmp, d_tmp = tmppool.get()
                            kb.op("act", lambda e: e.activation(out=tmp[:], in_=uv[:, D:2 * D], func=AF.Copy, scale=coef[:, j:j + 1]),
                                  reads=[d_uv, d_coefc[c]], writes=[d_tmp])
                            first = state["pe"] == 0
                            last = (j == 127)
                            kb.op("pe", lambda e: e.matmul(pa0[:], lhsT=ident, rhs=tmp[:, 0:512], start=first, stop=last), reads=[d_tmp, d_consts], writes=[d_pa0])
                            kb.op("pe", lambda e: e.matmul(pa1[:], lhsT=ident, rhs=tmp[:, 512:1024], start=first, stop=last), reads=[d_tmp, d_consts], writes=[d_pa1])
                            state["pe"] += 1

                for c in range(NCH):
                    if c + 1 < NCH:
                        gathers(cur, c + 1)
                    else:
                        for _ in gen:
                            pass
                        if tb + 1 < NT:
                            gathers(blocks[tb + 1], 0)
                    dots(c)
                    next(gen, None)
                    vside(c)
                if NDVE > 0:
                    kb.op("dve", lambda e: e.tensor_tensor(out=acc[:, 0:512], in0=pa0[:], in1=acc[:, 0:512], op=ALU.add), reads=[d_pa0, d_acc], writes=[d_acc])
                    kb.op("dve", lambda e: e.tensor_tensor(out=acc[:, 512:1024], in0=pa1[:], in1=acc[:, 512:1024], op=ALU.add), reads=[d_pa1, d_acc], writes=[d_acc])
                    kb.op("dve", lambda e: e.tensor_tensor(out=acc[:], in0=acc[:], in1=g2, op=ALU.mult), reads=[d_acc, d_mod], writes=[d_acc])
                else:
                    kb.op("dve", lambda e: e.tensor_tensor(out=acc[:, 0:512], in0=pa0[:], in1=g2[:, 0:512], op=ALU.mult), reads=[d_pa0, d_mod], writes=[d_acc])
                    kb.op("dve", lambda e: e.tensor_tensor(out=acc[:, 512:1024], in0=pa1[:], in1=g2[:, 512:1024], op=ALU.mult), reads=[d_pa1, d_mod], writes=[d_acc])
                kb.op("dve", lambda e: e.tensor_tensor(out=acc[:], in0=acc[:], in1=xt[:], op=ALU.add), reads=[d_acc, d_xt], writes=[d_acc])
                if l == L - 1:
                    rs, d_rs = rms_scale(es, acc[:], d_acc, D, sspool, junk[:], d_junk)
                    kb.op("dve", lambda e: e.scalar_tensor_tensor(out=acc[:], in0=acc[:], scalar=rs[:, 0:1], in1=fn_bc[:], op0=ALU.mult, op1=ALU.mult),
                          reads=[d_acc, d_rs, d_fn], writes=[d_acc])
                    kb.dma("sp", lambda q: q.dma_start(out=out[rows, :], in_=acc[:]), reads=[d_acc])
                else:
                    kb.dma("sp", lambda q: q.dma_start(out=xs[rows, :], in_=acc[:]), reads=[d_acc])
            kb.barrier()

    if dbg:
        for name, src, shape, dt in (("d_xs", xs, [S, D], F32), ("d_qT", qT_s, [512, S], F32), ("d_kT", kT_s, [512, S], F32),
                                     ("d_v", v_s, [S, 512], F32), ("d_ya", ya_s, [S, 512], F32), ("d_ybT", ybT_s, [512, S], F32),
                                     ):
            o = nc.dram_tensor(name, shape, dt, kind="ExternalOutput").ap()
            kb.dma("sp", lambda q: q.dma_start(out=o, in_=src))
        kb.barrier()
    print("instructions", kb.nins, "waits", kb.nwait)
    return nc


def prep_inputs(inp, b, L):
    f = lambda a: np.ascontiguousarray(a, dtype=np.float32)
    m = {
        "x": f(inp["x"][b]),
        "c": f(inp["c"][b].reshape(8, 128).T),
        "ada_w": f(inp["ada_w"][:L]),
        "ada_b": f(inp["ada_b"][:L]),
        "norm_mix": f(inp["norm_mix"][:L]),
        "norm_ffn": f(inp["norm_ffn"][:L]),
        "w_in": f(inp["w_in"][:L]),
        "gm_wsT": f(np.transpose(inp["gm_ws"][:L], (0, 3, 1, 2))),
        "gm_bsT": f(np.transpose(inp["gm_bs"][:L], (0, 2, 1))),
        "gm_vnorm": f(inp["gm_vnorm"][:L]),
        "out_norm_a": f(inp["out_norm_a"][:L]),
        "out_norm_bT": f(np.transpose(inp["out_norm_b"][:L].reshape(L, 4, 128), (0, 2, 1))),
        "w_out": f(inp["w_out"][:L]),
        "peer_wq": f(inp["peer_wq"][:L]),
        "peer_k1T": f(np.transpose(inp["peer_k1"][:L], (0, 2, 1))),
        "peer_k2T": f(np.transpose(inp["peer_k2"][:L], (0, 2, 1))),
        "peer_uv": np.concatenate([f(inp["peer_u"][:L]).reshape(L * NEXP, D), f(inp["peer_v"][:L]).reshape(L * NEXP, D)], axis=1),
        "final_norm": f(inp["final_norm"]),
        "consts": make_consts(),
    }
    return m


def kernel(**inputs):
    inp = {k: np.asarray(v) for k, v in inputs.items()}
    B, S, _ = inp["x"].shape
    L = inp["ada_w"].shape[0]
    nc = build(L, S)
    shared = prep_inputs(inp, 0, L)
    in_maps = []
    for b in range(B):
        m = dict(shared)
        m["x"] = np.ascontiguousarray(inp["x"][b], dtype=np.float32)
        m["c"] = np.ascontiguousarray(inp["c"][b].reshape(8, 128).T, dtype=np.float32)
        in_maps.append(m)
    res = run_bass_kernel_spmd(nc, in_maps, core_ids=list(range(B)))
    return np.stack([r["out"] for r in res.results], axis=0).astype(np.float32)
```

```python
import numpy as np
from contextlib import ExitStack
import concourse.bass as bass
import concourse.mybir as mybir
from concourse.bass_utils import run_bass_kernel_spmd

F32 = mybir.dt.float32
I32 = mybir.dt.int32
U32 = mybir.dt.uint32
AF = mybir.ActivationFunctionType
ALU = mybir.AluOpType
AX = mybir.AxisListType

D = 1024
NEXP = 16384
EPS = 1e-6
NDS = 40
NSW = 16
SAME_ENG_SYNC = True


class Dep:
    __slots__ = ("w", "r")

    def __init__(self):
        self.w = None
        self.r = {}


class KB:
    def __init__(self, nc):
        self.nc = nc
        self.es = ExitStack()
        self.eng = {"pe": nc.tensor, "act": nc.scalar, "dve": nc.vector, "pool": nc.gpsimd, "sp": nc.sync}
        self.esem = {e: self.es.enter_context(nc.semaphore("es_" + e)) for e in self.eng}
        self.ecount = {e: 0 for e in self.eng}
        self.known = {e: {} for e in self.eng}
        self.dsem = [self.es.enter_context(nc.semaphore("ds%d" % i)) for i in range(NDS + NSW)]
        self.dcount = [0] * (NDS + NSW)
        self.dnext = 0
        self.swnext = 0
        self.nwait = 0
        self.nins = 0

    def _sem(self, key):
        return self.esem[key[1]] if key[0] == "e" else self.dsem[key[1]]

    def _need(self, e, deps):
        need = {}
        for key, val in deps:
            if key[0] == "e" and key[1] == e:
                if e == "pe" or not SAME_ENG_SYNC:
                    continue
            if self.known[e].get(key, 0) >= val:
                continue
            if need.get(key, 0) < val:
                need[key] = val
        return list(need.items())

    def _wait(self, e, deps, keep_one=False):
        need = self._need(e, deps)
        emb = None
        if keep_one and need:
            emb = need.pop()
        for key, val in need:
            self.eng[e].wait_ge(self._sem(key), val)
            self.known[e][key] = val
            self.nwait += 1
        return emb

    def _embed(self, e, ins, emb):
        if emb is not None:
            key, val = emb
            ins._wait_ge(self._sem(key), val)
            self.known[e][key] = val

    @staticmethod
    def _collect(reads, writes):
        deps = []
        for d in reads:
            if d.w is not None:
                deps.append(d.w)
        for d in writes:
            if d.w is not None:
                deps.append(d.w)
            deps.extend(d.r.items())
        return deps

    @staticmethod
    def _record(ev, reads, writes):
        key, val = ev
        for d in reads:
            if d.r.get(key, 0) < val:
                d.r[key] = val
        for d in writes:
            d.w = ev
            d.r = {}

    def op(self, e, fn, reads=(), writes=()):
        emb = self._wait(e, self._collect(reads, writes), keep_one=True)
        ins = fn(self.eng[e])
        self._embed(e, ins, emb)
        self.ecount[e] += 1
        ins.then_inc(self.esem[e], 1)
        self._record((("e", e), self.ecount[e]), reads, writes)
        self.nins += 1

    def dma(self, q, fn, reads=(), writes=()):
        deps = self._collect(reads, writes)
        if q == "pool":
            k = NDS + self.swnext
            self.swnext = (self.swnext + 1) % NSW
        else:
            k = self.dnext
            self.dnext = (k + 1) % NDS
        if self.dcount[k] > 0:
            deps.append((("d", k), self.dcount[k]))
        emb = self._wait(q, deps, keep_one=True)
        ins = fn(self.eng[q])
        self._embed(q, ins, emb)
        self.dcount[k] += 16
        ins.then_inc(self.dsem[k], 16)
        self._record((("d", k), self.dcount[k]), reads, writes)
        self.nins += 1

    def barrier(self):
        allev = [(("e", e), c) for e, c in self.ecount.items() if c > 0]
        allev += [(("d", k), c) for k, c in enumerate(self.dcount) if c > 0]
        for e in self.eng:
            for key, val in allev:
                if self.known[e].get(key, 0) < val:
                    self.eng[e].wait_ge(self._sem(key), val)
                    self.known[e][key] = val
                    self.nwait += 1


class Pool:
    uid = 0

    def __init__(self, es, nc, name, shape, dt, n, psum=False):
        self.t = []
        for i in range(n):
            mk = nc.psum_tensor if psum else nc.sbuf_tensor
            Pool.uid += 1
            self.t.append((es.enter_context(mk("%s_%d_%d" % (name, i, Pool.uid), shape, dt)), Dep()))
        self.i = 0

    def get(self):
        r = self.t[self.i]
        self.i = (self.i + 1) % len(self.t)
        return r

    @classmethod
    def join(cls, *pools):
        p = cls.__new__(cls)
        p.t = [x for q in pools for x in q.t]
        p.i = 0
        return p


def make_consts():
    c = np.zeros((128, 128 * 3 + 4 * 512 + 1), np.float32)
    c[:, 0:128] = np.eye(128, dtype=np.float32)
    j = np.arange(128)[:, None]
    s = np.arange(128)[None, :]
    c[:, 128:256] = -(j >= s).astype(np.float32)
    c[:, 256:384] = -1.0
    t = np.arange(512)[None, :]
    for jd in range(4):
        c[:, 384 + jd * 512:384 + (jd + 1) * 512] = ((128 * jd + j) < t).astype(np.float32)
    c[:, 384 + 2048] = 1.0
    return c


def build(L, S, dbg=False, phases="ABCDEF"):
    nc = bass.Bass("TRN2", target_bir_lowering=False)
    NT = S // 128
    NG = S // 512
    assert S % 512 == 0

    def din(name, shape, dt=F32):
        return nc.dram_tensor(name, shape, dt, kind="ExternalInput").ap()

    def dscr(name, shape, dt=F32):
        return nc.dram_tensor(name, shape, dt, kind="Internal").ap()

    x_in = din("x", [S, D])
    c_in = din("c", [128, 8])
    ada_w = din("ada_w", [L, D, 6 * D])
    ada_b = din("ada_b", [L, 6 * D])
    norm_mix = din("norm_mix", [L, D])
    norm_ffn = din("norm_ffn", [L, D])
    w_in = din("w_in", [L, D, 2560])
    gm_wsT = din("gm_wsT", [L, 128, 8, 128])
    gm_bsT = din("gm_bsT", [L, 128, 8])
    gm_vnorm = din("gm_vnorm", [L, 512])
    out_norm_a = din("out_norm_a", [L, 512])
    out_norm_bT = din("out_norm_bT", [L, 128, 4])
    w_out = din("w_out", [L, D, D])
    peer_wq = din("peer_wq", [L, D, 2048])
    peer_k1T = din("peer_k1T", [L, 128, 128])
    peer_k2T = din("peer_k2T", [L, 128, 128])
    peer_uv = din("peer_uv", [L * NEXP, 2 * D])
    final_norm = din("final_norm", [D])
    consts_in = din("consts", [128, 2433])
    out = nc.dram_tensor("out", [S, D], F32, kind="ExternalOutput").ap()

    xs = dscr("xs", [S, D])
    qT_s = dscr("qT_s", [512, S])
    kT_s = dscr("kT_s", [512, S])
    v_s = dscr("v_s", [S, 512])
    ya_s = dscr("ya_s", [S, 512])
    ybT_s = dscr("ybT_s", [512, S])

    kb = KB(nc)
    top = kb.es

    def sbt(es, name, shape, dt=F32):
        Pool.uid += 1
        return es.enter_context(nc.sbuf_tensor("%s_t%d" % (name, Pool.uid), shape, dt))

    consts = sbt(top, "consts", [128, 2433]); d_consts = Dep()
    ident = consts[:, 0:128]
    negtri = consts[:, 128:256]
    negones = consts[:, 256:384]
    masks = [consts[:, 384 + jd * 512:384 + (jd + 1) * 512] for jd in range(4)]
    ones_col = consts[:, 2432:2433]
    mod = sbt(top, "mod", [128, 6, D]); d_mod = Dep()
    c_act = sbt(top, "c_act", [128, 8]); d_cact = Dep()
    psum = Pool(top, nc, "ps", [128, 512], F32, 6, psum=True)
    psum_acc = Pool(top, nc, "psacc", [128, 512], F32, 2, psum=True)
    psum8 = Pool.join(psum, psum_acc)

    kb.dma("sp", lambda q: q.dma_start(out=consts[:], in_=consts_in), writes=[d_consts])
    kb.dma("sp", lambda q: q.dma_start(out=c_act[:], in_=c_in), writes=[d_cact])
    with ExitStack() as es:
        sg = sbt(es, "sg", [128, 8]); d_sg = Dep()
        kb.op("act", lambda e: e.activation(out=sg[:], in_=c_act[:], func=AF.Sigmoid), reads=[d_cact], writes=[d_sg])
        kb.op("dve", lambda e: e.tensor_tensor(out=c_act[:], in0=c_act[:], in1=sg[:], op=ALU.mult), reads=[d_sg, d_cact], writes=[d_cact])
        kb.barrier()

    def rms_scale(es_pool, src, d_src, nfeat, ss_pool, junk, d_junk):
        ss, d_ss = ss_pool.get()
        s1 = ss[:, 0:1]
        kb.op("dve", lambda e: e.memset(s1, 0.0), writes=[d_ss])
        kb.op("dve", lambda e: e.scalar_tensor_tensor(out=junk, in0=src, scalar=1.0, in1=src, op0=ALU.mult, op1=ALU.mult,
                                                      accum_out=s1), reads=[d_src], writes=[d_junk, d_ss])
        kb.op("dve", lambda e: e.tensor_scalar(out=s1, in0=s1, scalar1=1.0 / nfeat, scalar2=EPS, op0=ALU.mult, op1=ALU.add),
              reads=[d_ss], writes=[d_ss])
        kb.op("act", lambda e: e.sqrt(out=s1, in_=s1), reads=[d_ss], writes=[d_ss])
        kb.op("dve", lambda e: e.reciprocal(out=s1, in_=s1), reads=[d_ss], writes=[d_ss])
        return ss, d_ss

    for l in range(L):
        x_src = x_in if l == 0 else xs
        with ExitStack() as es:
            c_rep = sbt(es, "c_rep", [128, 8, 128]); d_crep = Dep()
            kb.op("dve", lambda e: e.tensor_copy(out=c_rep[:], in_=c_act[:].unsqueeze(2).to_broadcast([128, 8, 128])),
                  reads=[d_cact], writes=[d_crep])
            wpool = Pool(es, nc, "adaw", [128, 8, 512], F32, 2)
            bpool = Pool(es, nc, "adab", [128, 512], F32, 2)
            nm = sbt(es, "nm", [128, 2, D]); d_nm = Dep()
            kb.dma("sp", lambda q: q.dma_start(out=nm[:, 0, :], in_=norm_mix[l].partition_broadcast(128)), writes=[d_nm])
            kb.dma("sp", lambda q: q.dma_start(out=nm[:, 1, :], in_=norm_ffn[l].partition_broadcast(128)), writes=[d_nm])
            for n in range(12):
                wt, d_wt = wpool.get()
                bt, d_bt = bpool.get()
                kb.dma("sp", lambda q: q.dma_start(out=wt[:], in_=ada_w[l][:, n * 512:(n + 1) * 512].rearrange("(kc p) n -> p kc n", p=128)),
                       writes=[d_wt])
                kb.dma("sp", lambda q: q.dma_start(out=bt[:], in_=ada_b[l, n * 512:(n + 1) * 512].partition_broadcast(128)), writes=[d_bt])
                pt, d_pt = psum.get()
                for kc in range(8):
                    kb.op("pe", lambda e: e.matmul(pt[:], lhsT=c_rep[:, kc, :], rhs=wt[:, kc, :], start=(kc == 0), stop=(kc == 7)),
                          reads=[d_crep, d_wt], writes=[d_pt])
                dst = mod[:, n // 2, (n % 2) * 512:(n % 2 + 1) * 512]
                kb.op("dve", lambda e: e.tensor_tensor(out=dst, in0=pt[:], in1=bt[:], op=ALU.add), reads=[d_pt, d_bt], writes=[d_mod])
            kb.op("dve", lambda e: e.scalar_tensor_tensor(out=mod[:, 1, :], in0=mod[:, 1, :], scalar=1.0, in1=nm[:, 0, :], op0=ALU.add, op1=ALU.mult),
                  reads=[d_nm, d_mod], writes=[d_mod])
            kb.op("dve", lambda e: e.scalar_tensor_tensor(out=mod[:, 4, :], in0=mod[:, 4, :], scalar=1.0, in1=nm[:, 1, :], op0=ALU.add, op1=ALU.mult),
                  reads=[d_nm, d_mod], writes=[d_mod])
            kb.barrier()
        sh1, A1, g1, sh2, A2, g2 = (mod[:, i, :] for i in range(6))

        def norm_mod(es, xt, d_xt, A, sh, sspool, junk, d_junk, h, d_h):
            rs, d_rs = rms_scale(es, xt[:], d_xt, D, sspool, junk[:], d_junk)
            kb.op("dve", lambda e: e.scalar_tensor_tensor(out=h[:], in0=xt[:], scalar=rs[:, 0:1], in1=A, op0=ALU.mult, op1=ALU.mult),
                  reads=[d_xt, d_rs, d_mod], writes=[d_h])
            kb.op("pool", lambda e: e.tensor_tensor(out=h[:], in0=h[:], in1=sh, op=ALU.add), reads=[d_h, d_mod], writes=[d_h])

        def transpose_to(src, d_src, nchunk, dst_fn, d_dst, eng="act", pp=None):
            pp = pp or psum
            for c0 in range(0, nchunk, 4):
                pt, d_pt = pp.get()
                n = min(4, nchunk - c0)
                for c in range(c0, c0 + n):
                    kb.op("pe", lambda e: e.transpose(out=pt[:, (c - c0) * 128:(c - c0 + 1) * 128], in_=src[:, c * 128:(c + 1) * 128], identity=ident),
                          reads=[d_src, d_consts], writes=[d_pt])
                for c in range(c0, c0 + n):
                    if eng == "act":
                        kb.op("act", lambda e: e.copy(out=dst_fn(c), in_=pt[:, (c - c0) * 128:(c - c0 + 1) * 128]), reads=[d_pt], writes=[d_dst])
                    else:
                        kb.op("dve", lambda e: e.tensor_copy(out=dst_fn(c), in_=pt[:, (c - c0) * 128:(c - c0 + 1) * 128]), reads=[d_pt], writes=[d_dst])

        if "B" in phases:
          with ExitStack() as es:
            wi = sbt(es, "wi", [128, 8, 2560]); d_wi = [Dep() for _ in range(8)]
            for kc in range(8):
                kb.dma("sp", lambda q: q.dma_start(out=wi[:, kc, :], in_=w_in[l][kc * 128:(kc + 1) * 128, :]), writes=[d_wi[kc]])
            wsT = sbt(es, "wsT", [128, 8, 128]); d_wsT = Dep()
            kb.dma("sp", lambda q: q.dma_start(out=wsT[:], in_=gm_wsT[l]), writes=[d_wsT])
            kb.op("dve", lambda e: e.memset(wsT[64:128, :, 0:64], 0.0), writes=[d_wsT])
            bsT = sbt(es, "bsT", [128, 8]); d_bsT = Dep()
            kb.dma("sp", lambda q: q.dma_start(out=bsT[:], in_=gm_bsT[l]), writes=[d_bsT])
            vg = sbt(es, "vg", [128, 512]); d_vg = Dep()
            kb.dma("sp", lambda q: q.dma_start(out=vg[:], in_=gm_vnorm[l].partition_broadcast(128)), writes=[d_vg])
            ona = sbt(es, "ona", [128, 512]); d_ona = Dep()
            kb.dma("sp", lambda q: q.dma_start(out=ona[:], in_=out_norm_a[l].partition_broadcast(128)), writes=[d_ona])
            xpool = Pool(es, nc, "bx", [128, D], F32, 2)
            hpool = Pool(es, nc, "bh", [128, D], F32, 4)
            junk = sbt(es, "bjunk", [128, D]); d_junk = Dep()
            sspool = Pool(es, nc, "bss", [128, 8], F32, 8)
            hTpool = Pool(es, nc, "bhT", [128, 8, 512], F32, 1)
            gupool = Pool(es, nc, "bgu", [128, 512], F32, 5)
            gvpool = Pool(es, nc, "bgv", [128, 512], F32, 4)
            vapool = Pool(es, nc, "bva", [128, 512], F32, 2)
            qkpool = Pool(es, nc, "bqk", [128, 512], F32, 2)
            wkpool_b = Pool(es, nc, "bwk", [128, 512], F32, 6)
            hTs = {}

            def b_prep_norm(g):
                hs_ = []
                for j in range(4):
                    tb = g * 4 + j
                    xt, d_xt = xpool.get()
                    kb.dma("sp", lambda q: q.dma_start(out=xt[:], in_=x_src[tb * 128:(tb + 1) * 128, :]), writes=[d_xt])
                    h, d_h = hpool.get()
                    norm_mod(es, xt, d_xt, A1, sh1, sspool, junk, d_junk, h, d_h)
                    hs_.append((h, d_h))
                hTs[("n", g)] = hs_

            def b_prep_T(g):
                hT, d_hT = hTpool.get()
                for j, (h, d_h) in enumerate(hTs.pop(("n", g))):
                    transpose_to(h, d_h, 8, lambda c: hT[:, c, j * 128:(j + 1) * 128], d_hT, pp=psum8)
                hTs[g] = (hT, d_hT)

            def b_proj_tok(g, j, res):
                hT, d_hT = hTs[g]
                tb = g * 4 + j
                rows = slice(tb * 128, (tb + 1) * 128)
                for name, c0 in (("u", 0), ("v", 512), ("va", 2048)):
                    pt, d_pt = psum8.get()
                    for kc in range(8):
                        kb.op("pe", lambda e: e.matmul(pt[:], lhsT=hT[:, kc, j * 128:(j + 1) * 128], rhs=wi[:, kc, c0:c0 + 512],
                                                       start=(kc == 0), stop=(kc == 7)), reads=[d_hT, d_wi[kc]], writes=[d_pt])
                    if name == "va":
                        t, d_t = vapool.get()
                        kb.op("act", lambda e: e.copy(out=t[:], in_=pt[:]), reads=[d_pt], writes=[d_t])
                        kb.dma("sp", lambda q: q.dma_start(out=v_s[rows, :], in_=t[:]), reads=[d_t])
                    else:
                        t, d_t = (gupool if name == "u" else gvpool).get()
                        kb.op("act", lambda e: e.activation(out=t[:], in_=pt[:], func=AF.Gelu_apprx_tanh), reads=[d_pt], writes=[d_t])
                    res[(j, name)] = (t, d_t)

            def b_proj_qk(g):
                hT, d_hT = hTs[g]
                for cc in range(8):
                    col0 = 1024 + cc * 128
                    pt, d_pt = psum8.get()
                    for kc in range(8):
                        kb.op("pe", lambda e: e.matmul(pt[:], lhsT=wi[:, kc, col0:col0 + 128], rhs=hT[:, kc, :], start=(kc == 0), stop=(kc == 7)),
                              reads=[d_hT, d_wi[kc]], writes=[d_pt])
                    t, d_t = qkpool.get()
                    if cc < 4:
                        kb.op("act", lambda e: e.activation(out=t[:], in_=pt[:], func=AF.Copy, scale=0.125), reads=[d_pt], writes=[d_t])
                        dst = qT_s[cc * 128:(cc + 1) * 128, g * 512:(g + 1) * 512]
                    else:
                        kb.op("act", lambda e: e.copy(out=t[:], in_=pt[:]), reads=[d_pt], writes=[d_t])
                        dst = kT_s[(cc - 4) * 128:(cc - 3) * 128, g * 512:(g + 1) * 512]
                    kb.dma("sp", lambda q: q.dma_start(out=dst, in_=t[:]), reads=[d_t])

            def b_gmlp_pre(g, j, res):
                gv, d_gv = res[(j, "v")]
                sq, d_sq = wkpool_b.get()
                kb.op("pool", lambda e: e.tensor_tensor(out=sq[:], in0=gv[:], in1=gv[:], op=ALU.mult), reads=[d_gv], writes=[d_sq])
                rh, d_rh = sspool.get()
                kb.op("dve", lambda e: e.tensor_reduce(out=rh[:], in_=sq[:].rearrange("p (h c) -> p h c", h=8), axis=AX.X, op=ALU.add),
                      reads=[d_sq], writes=[d_rh])
                kb.op("dve", lambda e: e.tensor_scalar(out=rh[:], in0=rh[:], scalar1=1.0 / 64, scalar2=EPS, op0=ALU.mult, op1=ALU.add),
                      reads=[d_rh], writes=[d_rh])
                kb.op("act", lambda e: e.sqrt(out=rh[:], in_=rh[:]), reads=[d_rh], writes=[d_rh])
                kb.op("dve", lambda e: e.reciprocal(out=rh[:], in_=rh[:]), reads=[d_rh], writes=[d_rh])
                vh, d_vh = wkpool_b.get()
                kb.op("dve", lambda e: e.tensor_tensor(out=vh[:].rearrange("p (h c) -> p h c", h=8), in0=gv[:].rearrange("p (h c) -> p h c", h=8),
                                                       in1=rh[:].unsqueeze(2).to_broadcast([128, 8, 64]), op=ALU.mult),
                      reads=[d_gv, d_rh], writes=[d_vh])
                kb.op("pool", lambda e: e.tensor_tensor(out=vh[:], in0=vh[:], in1=vg[:], op=ALU.mult), reads=[d_vh, d_vg], writes=[d_vh])
                res[(j, "vh")] = (vh, d_vh)

            def b_gmlp_post(g, j, res):
                tb = g * 4 + j
                rows = slice(tb * 128, (tb + 1) * 128)
                gu, d_gu = res[(j, "u")]
                vh, d_vh = res[(j, "vh")]
                pz, d_pz = psum8.get()
                for hh in range(8):
                    kb.op("pe", lambda e: e.matmul(pz[:, hh * 64:(hh + 1) * 64], lhsT=wsT[:, hh, :], rhs=vh[:, hh * 64:(hh + 1) * 64],
                                                   start=True, stop=True), reads=[d_wsT, d_vh], writes=[d_pz])
                ya, d_ya = wkpool_b.get()
                kb.op("dve", lambda e: e.tensor_tensor(out=ya[:].rearrange("p (h c) -> p h c", h=8), in0=pz[:].rearrange("p (h c) -> p h c", h=8),
                                                       in1=bsT[:].unsqueeze(2).to_broadcast([128, 8, 64]), op=ALU.add),
                      reads=[d_pz, d_bsT], writes=[d_ya])
                kb.op("pool", lambda e: e.tensor_tensor(out=ya[:], in0=ya[:], in1=gu[:], op=ALU.mult), reads=[d_ya, d_gu], writes=[d_ya])
                ra, d_ra = rms_scale(es, ya[:], d_ya, 512, sspool, junk[:, 0:512], d_junk)
                yan, d_yan = wkpool_b.get()
                kb.op("dve", lambda e: e.scalar_tensor_tensor(out=yan[:], in0=ya[:], scalar=ra[:, 0:1], in1=ona[:], op0=ALU.mult, op1=ALU.mult),
                      reads=[d_ya, d_ra, d_ona], writes=[d_yan])
                kb.dma("sp", lambda q: q.dma_start(out=ya_s[rows, :], in_=yan[:]), reads=[d_yan])

            b_prep_norm(0)
            b_prep_T(0)
            prev = None
            for g in range(NG + 1):
                if g + 1 < NG:
                    b_prep_norm(g + 1)
                res = {}
                for j in range(4):
                    if prev is not None:
                        b_gmlp_pre(g - 1, j, prev)
                    if g < NG:
                        b_proj_tok(g, j, res)
                    if prev is not None:
                        b_gmlp_post(g - 1, j, prev)
                if g < NG:
                    b_proj_qk(g)
                if g + 1 < NG:
                    b_prep_T(g + 1)
                prev = res if g < NG else None
            kb.barrier()

        if "C" in phases:
          with ExitStack() as es:
            qpool = Pool(es, nc, "cq", [128, S], F32, 2)
            kpool = Pool(es, nc, "ck", [128, S], F32, 2)
            vpool = Pool(es, nc, "cv", [128, NT, 128], F32, 2)
            epool = Pool(es, nc, "ce", [128, 512], F32, 4)
            sppool = Pool(es, nc, "csp", [128, 512], F32, 8)
            apool = Pool(es, nc, "ca", [128, 512], F32, 8)
            cspool = Pool(es, nc, "ccs", [128, 512], F32, 6)
            ybpool = Pool(es, nc, "cyb", [128, 512], F32, 2)
            tiles = []
            for hp in range(4):
                for g in range(NG):
                    nkb = 4 * g + 4
                    for idx, kbk in enumerate(reversed(range(nkb))):
                        tiles.append((hp, g, idx, kbk, nkb))
            loaded = {}
            grp = {}
            st = {}

            def operands(hp):
                if hp not in loaded:
                    qT, d_q = qpool.get()
                    kT, d_k = kpool.get()
                    vv, d_v = vpool.get()
                    kb.dma("sp", lambda q: q.dma_start(out=qT[:], in_=qT_s[hp * 128:(hp + 1) * 128, :]), writes=[d_q])
                    kb.dma("sp", lambda q: q.dma_start(out=kT[:], in_=kT_s[hp * 128:(hp + 1) * 128, :]), writes=[d_k])
                    kb.dma("sp", lambda q: q.dma_start(out=vv[:], in_=v_s[:, hp * 128:(hp + 1) * 128].rearrange("(kb p) d -> p kb d", p=128)),
                           writes=[d_v])
                    loaded[hp] = (qT, d_q, kT, d_k, vv, d_v)
                return loaded[hp]

            def stage1(i):
                hp, g, idx, kbk, nkb = tiles[i]
                qT, d_q, kT, d_k, vv, d_v = operands(hp)
                if hp + 1 < 4 and g == 0 and idx == 3:
                    operands(hp + 1)
                qs = slice(g * 512, (g + 1) * 512)
                ks = slice(kbk * 128, (kbk + 1) * 128)
                jd = kbk - 4 * g
                if idx == 0:
                    grp[(hp, g)] = psum_acc.get() + cspool.get() + cspool.get()
                pzs = [psum.get() for _ in range(2)]
                for hh in range(2):
                    pr = slice(hh * 64, (hh + 1) * 64)
                    pz, d_pz = pzs[hh]
                    kb.op("pe", lambda e: e.matmul(pz[:], lhsT=kT[pr, ks], rhs=qT[pr, qs], start=True, stop=False, skip_group_check=True),
                          reads=[d_q, d_k], writes=[d_pz])
                sps = []
                for hh in range(2):
                    pz, d_pz = pzs[hh]
                    et, d_et = epool.get()
                    kb.op("act", lambda e: e.activation(out=et[:], in_=pz[:], func=AF.Exp), reads=[d_pz], writes=[d_et])
                    spt, d_spt = sppool.get()
                    kb.op("act", lambda e: e.activation(out=spt[:], in_=et[:], func=AF.Ln, bias=1.0), reads=[d_et], writes=[d_spt])
                    if jd >= 0:
                        kb.op("pool", lambda e: e.tensor_tensor(out=spt[:], in0=spt[:], in1=masks[jd], op=ALU.mult),
                              reads=[d_spt, d_consts], writes=[d_spt])
                    sps.append((spt, d_spt))
                st[i] = (pzs, sps)

            def stage2(i):
                hp, g, idx, kbk, nkb = tiles[i]
                jd = kbk - 4 * g
                pzs, sps = st[i]
                gr = grp[(hp, g)]
                css = [(gr[2], gr[3]), (gr[4], gr[5])]
                for hh in range(2):
                    pz, d_pz = pzs[hh]
                    spt, d_spt = sps[hh]
                    cs, d_cs = css[hh]
                    kb.op("pe", lambda e: e.matmul(pz[:], lhsT=negtri, rhs=spt[:], start=False, stop=(idx == 0), skip_group_check=True),
                          reads=[d_spt, d_consts], writes=[d_pz])
                    if idx > 0:
                        kb.op("pe", lambda e: e.matmul(pz[:], lhsT=negones, rhs=cs[:], start=False, stop=True, skip_group_check=True),
                              reads=[d_cs, d_consts], writes=[d_pz])
                ats = []
                for hh in range(2):
                    pz, d_pz = pzs[hh]
                    spt, d_spt = sps[hh]
                    cs, d_cs = css[hh]
                    at, d_at = apool.get()
                    kb.op("act", lambda e: e.activation(out=at[:], in_=pz[:], func=AF.Exp), reads=[d_pz], writes=[d_at])
                    if jd >= 0:
                        kb.op("pool", lambda e: e.tensor_tensor(out=at[:], in0=at[:], in1=masks[jd], op=ALU.mult),
                              reads=[d_at, d_consts], writes=[d_at])
                    if idx < nkb - 1:
                        if idx == 0:
                            kb.op("dve", lambda e: e.tensor_copy(out=cs[:], in_=spt[:]), reads=[d_spt], writes=[d_cs])
                        else:
                            kb.op("dve", lambda e: e.tensor_tensor(out=cs[:], in0=cs[:], in1=spt[:], op=ALU.add),
                                  reads=[d_spt, d_cs], writes=[d_cs])
                    ats.append((at, d_at))
                st[i] = ats

            def stage3(i):
                hp, g, idx, kbk, nkb = tiles[i]
                qT, d_q, kT, d_k, vv, d_v = operands(hp)
                qs = slice(g * 512, (g + 1) * 512)
                ats = st.pop(i)
                gr = grp[(hp, g)]
                po, d_po = gr[0], gr[1]
                for hh in range(2):
                    pr = slice(hh * 64, (hh + 1) * 64)
                    at, d_at = ats[hh]
                    kb.op("pe", lambda e: e.matmul(po[pr, :], lhsT=vv[:, kbk, pr], rhs=at[:], start=(idx == 0), stop=(idx == nkb - 1)),
                          reads=[d_v, d_at], writes=[d_po])
                if idx == nkb - 1:
                    yb, d_yb = ybpool.get()
                    kb.op("dve", lambda e: e.tensor_copy(out=yb[:], in_=po[:]), reads=[d_po], writes=[d_yb])
                    kb.dma("sp", lambda q: q.dma_start(out=ybT_s[hp * 128:(hp + 1) * 128, qs], in_=yb[:]), reads=[d_yb])
                    del grp[(hp, g)]

            nt = len(tiles)
            for step in range(nt + 2):
                if step < nt:
                    stage1(step)
                if 0 <= step - 1 < nt:
                    stage2(step - 1)
                if 0 <= step - 2 < nt:
                    stage3(step - 2)
            kb.barrier()

        if "D" in phases:
          with ExitStack() as es:
            wo = sbt(es, "wo", [128, 8, D]); d_wo = [Dep() for _ in range(8)]
            for kc in range(8):
                kb.dma("sp", lambda q: q.dma_start(out=wo[:, kc, :], in_=w_out[l][kc * 128:(kc + 1) * 128, :]), writes=[d_wo[kc]])
            gb = sbt(es, "gb", [128, 4]); d_gb = Dep()
            kb.dma("sp", lambda q: q.dma_start(out=gb[:], in_=out_norm_bT[l]), writes=[d_gb])
            xpool = Pool(es, nc, "dx", [128, D], F32, 3)
            xnpool = Pool(es, nc, "dxn", [128, D], F32, 3)
            yanpool = Pool(es, nc, "dyan", [128, 512], F32, 3)
            ybpool = Pool(es, nc, "dyb", [128, 4, 128], F32, 3)
            yaTpool = Pool(es, nc, "dyaT", [128, 512], F32, 2)
            sqbpool = Pool(es, nc, "dsqb", [128, 512], F32, 2)
            ybgpool = Pool(es, nc, "dybg", [128, 512], F32, 2)
            mainpool = Pool(es, nc, "dmain", [128, 512], F32, 4)
            sspool = Pool(es, nc, "dss", [128, 8], F32, 4)
            preps = {}

            loads = {}

            def d_load(tb):
                rows = slice(tb * 128, (tb + 1) * 128)
                xt, d_xt = xpool.get()
                kb.dma("sp", lambda q: q.dma_start(out=xt[:], in_=x_src[rows, :]), writes=[d_xt])
                yan, d_yan = yanpool.get()
                kb.dma("sp", lambda q: q.dma_start(out=yan[:], in_=ya_s[rows, :]), writes=[d_yan])
                ybT, d_ybT = ybpool.get()
                kb.dma("sp", lambda q: q.dma_start(out=ybT[:], in_=ybT_s[:, rows].rearrange("(c p) t -> p c t", p=128)), writes=[d_ybT])
                loads[tb] = (xt, d_xt, yan, d_yan, ybT, d_ybT)

            def d_prep(tb):
                xt, d_xt, yan, d_yan, ybT, d_ybT = loads.pop(tb)
                yaT, d_yaT = yaTpool.get()
                transpose_to(yan, d_yan, 4, lambda c: yaT[:, c * 128:(c + 1) * 128], d_yaT, pp=psum8)
                sqb, d_sqb = sqbpool.get()
                kb.op("act", lambda e: e.activation(out=sqb[:], in_=ybT[:].rearrange("p c t -> p (c t)"), func=AF.Square),
                      reads=[d_ybT], writes=[d_sqb])
                pq, d_pq = psum8.get()
                for c in range(4):
                    kb.op("pe", lambda e: e.matmul(pq[:, 0:1], lhsT=sqb[:, c * 128:(c + 1) * 128], rhs=ones_col, start=(c == 0), stop=(c == 3)),
                          reads=[d_sqb, d_consts], writes=[d_pq])
                rb, d_rb = sspool.get()
                kb.op("dve", lambda e: e.tensor_scalar(out=rb[:, 0:1], in0=pq[:, 0:1], scalar1=1.0 / 512, scalar2=EPS, op0=ALU.mult, op1=ALU.add),
                      reads=[d_pq], writes=[d_rb])
                kb.op("act", lambda e: e.sqrt(out=rb[:, 0:1], in_=rb[:, 0:1]), reads=[d_rb], writes=[d_rb])
                kb.op("dve", lambda e: e.reciprocal(out=rb[:, 0:1], in_=rb[:, 0:1]), reads=[d_rb], writes=[d_rb])
                ybg, d_ybg = ybgpool.get()
                for c in range(4):
                    kb.op("act", lambda e: e.activation(out=ybg[:, c * 128:(c + 1) * 128], in_=ybT[:, c, :], func=AF.Copy, scale=gb[:, c:c + 1]),
                          reads=[d_ybT, d_gb], writes=[d_ybg])
                preps[tb] = (xt, d_xt, yaT, d_yaT, ybg, d_ybg, rb, d_rb)

            def d_main(tb):
                rows = slice(tb * 128, (tb + 1) * 128)
                xt, d_xt, yaT, d_yaT, ybg, d_ybg, rb, d_rb = preps.pop(tb)
                xn, d_xn = xnpool.get()
                for half in range(2):
                    hs = slice(half * 512, (half + 1) * 512)
                    pA, d_pA = psum8.get()
                    for c in range(4):
                        kb.op("pe", lambda e: e.matmul(pA[:], lhsT=yaT[:, c * 128:(c + 1) * 128], rhs=wo[:, c, hs], start=(c == 0), stop=(c == 3)),
                              reads=[d_yaT, d_wo[c]], writes=[d_pA])
                    pB, d_pB = psum8.get()
                    for c in range(4):
                        kb.op("pe", lambda e: e.matmul(pB[:], lhsT=ybg[:, c * 128:(c + 1) * 128], rhs=wo[:, 4 + c, hs], start=(c == 0), stop=(c == 3)),
                              reads=[d_ybg, d_wo[4 + c]], writes=[d_pB])
                    pas, d_pas = mainpool.get()
                    kb.op("act", lambda e: e.copy(out=pas[:], in_=pA[:]), reads=[d_pA], writes=[d_pas])
                    mix, d_mix = mainpool.get()
                    kb.op("dve", lambda e: e.scalar_tensor_tensor(out=mix[:], in0=pB[:], scalar=rb[:, 0:1], in1=pas[:], op0=ALU.mult, op1=ALU.add),
                          reads=[d_pB, d_rb, d_pas], writes=[d_mix])
                    kb.op("dve", lambda e: e.tensor_tensor(out=mix[:], in0=mix[:], in1=g1[:, hs], op=ALU.mult), reads=[d_mix, d_mod], writes=[d_mix])
                    kb.op("dve", lambda e: e.tensor_tensor(out=xn[:, hs], in0=xt[:, hs], in1=mix[:], op=ALU.add), reads=[d_mix, d_xt], writes=[d_xn])
                kb.dma("pool", lambda q: q.dma_start(out=xs[rows, :], in_=xn[:]), reads=[d_xn])

            d_load(0)
            if NT > 1:
                d_load(1)
            d_prep(0)
            for tb in range(NT):
                if tb + 2 < NT:
                    d_load(tb + 2)
                d_main(tb)
                if tb + 1 < NT:
                    d_prep(tb + 1)
            kb.barrier()

        if "E" in phases:
          with ExitStack() as es:
            NB, CH, NDVE = 8, 4, 0
            NCH = 128 // CH
            kT = sbt(es, "pk", [128, 2, 128]); d_kT = Dep()
            kb.dma("sp", lambda q: q.dma_start(out=kT[:, 0, :], in_=peer_k1T[l]), writes=[d_kT])
            kb.dma("sp", lambda q: q.dma_start(out=kT[:, 1, :], in_=peer_k2T[l]), writes=[d_kT])
            fn_bc = None
            if l == L - 1:
                fn_bc = sbt(es, "fnbc", [128, D]); d_fn = Dep()
                kb.dma("sp", lambda q: q.dma_start(out=fn_bc[:], in_=final_norm.partition_broadcast(128)), writes=[d_fn])
            xpool = Pool(es, nc, "ex", [128, D], F32, 2)
            hpool = Pool(es, nc, "eh", [128, D], F32, 2)
            junk = sbt(es, "ejunk", [128, D]); d_junk = Dep()
            sspool = Pool(es, nc, "ess", [128, 8], F32, 6)
            hTpool = Pool(es, nc, "ehT", [128, 8, 128], F32, 1)
            wqpool = Pool(es, nc, "ewq", [128, 8, 256], F32, 2)
            qTpool = Pool(es, nc, "eqT", [128, 4, 128], F32, 2)
            scpool = Pool(es, nc, "esc", [128, 4, 128], F32, 2)
            wkpool = Pool(es, nc, "ewk", [128, 128], F32, 2)
            v16pool = Pool(es, nc, "ev16", [128, 16, 16], F32, 2)
            i16pool = Pool(es, nc, "ei16", [128, 16, 16], U32, 2)
            i16fpool = Pool(es, nc, "ei16f", [128, 16, 16], F32, 1)
            candpool = Pool(es, nc, "ecand", [128, 16, 16], F32, 2)
            cidpool = Pool(es, nc, "ecid", [128, 16, 16], F32, 2)
            wk2pool = Pool(es, nc, "ewk2", [128, 256], F32, 2)
            j2pool = Pool(es, nc, "ej2", [128, 256], F32, 1)
            tspool = Pool(es, nc, "ets", [128, 8, 16], F32, 2)
            eidfpool = Pool(es, nc, "eeidf", [128, 128], F32, 2)
            eidipool = Pool(es, nc, "eeidi", [128, 128], I32, 2)
            gpool = Pool(es, nc, "eg", [128, 8, 16], F32, 2)
            uvpool = Pool(es, nc, "fuv", [128, 2 * D], F32, NB)
            tmppool = Pool(es, nc, "ftmp", [128, D], F32, 3)
            actpool = Pool(es, nc, "fact", [128, 128], F32, 2)
            coefpool = Pool(es, nc, "fcoef", [128, 128], F32, 2)
            accpool = Pool(es, nc, "facc", [128, D], F32, 2)
            blocks = {}
            chunkdeps = {}

            def p1_gen(tb):
                rows = slice(tb * 128, (tb + 1) * 128)
                xt, d_xt = xpool.get()
                kb.dma("sp", lambda q: q.dma_start(out=xt[:], in_=xs[rows, :]), writes=[d_xt])
                h, d_h = hpool.get()
                norm_mod(es, xt, d_xt, A2, sh2, sspool, junk, d_junk, h, d_h)
                hT, d_hT = hTpool.get()
                transpose_to(h, d_h, 8, lambda c: hT[:, c, :], d_hT)
                v16, d_v16 = v16pool.get()
                i16, d_i16 = i16pool.get()
                yield
                for j0 in range(0, 16, 4):
                    pt, d_pt = psum.get()
                    for jp in range(j0, j0 + 4, 2):
                        wqt, d_wqt = wqpool.get()
                        kb.dma("sp", lambda q: q.dma_start(out=wqt[:], in_=peer_wq[l][:, jp * 128:(jp + 2) * 128].rearrange("(kc p) n -> p kc n", p=128)),
                               writes=[d_wqt])
                        for j in (jp, jp + 1):
                            for kc in range(8):
                                kb.op("pe", lambda e: e.matmul(pt[:, (j - j0) * 128:(j - j0 + 1) * 128], lhsT=wqt[:, kc, (j - jp) * 128:(j - jp + 1) * 128],
                                                               rhs=hT[:, kc, :], start=(kc == 0), stop=(kc == 7)), reads=[d_hT, d_wqt], writes=[d_pt])
                    qT4, d_qT4 = qTpool.get()
                    kb.op("act", lambda e: e.copy(out=qT4[:].rearrange("p a b -> p (a b)"), in_=pt[:]), reads=[d_pt], writes=[d_qT4])
                    psc, d_psc = psum.get()
                    for j in range(j0, j0 + 4):
                        kb.op("pe", lambda e: e.matmul(psc[:, (j - j0) * 128:(j - j0 + 1) * 128], lhsT=qT4[:, j - j0, :], rhs=kT[:, j % 2, :], start=True, stop=True),
                              reads=[d_qT4, d_kT], writes=[d_psc])
                    sc4, d_sc4 = scpool.get()
                    kb.op("act", lambda e: e.copy(out=sc4[:].rearrange("p a b -> p (a b)"), in_=psc[:]), reads=[d_psc], writes=[d_sc4])
                    yield
                    for j in range(j0, j0 + 4):
                        scj = sc4[:, j - j0, :]
                        wk, d_wk = wkpool.get()
                        kb.op("dve", lambda e: e.max(out=v16[:, j, 0:8], in_=scj), reads=[d_sc4], writes=[d_v16])
                        kb.op("dve", lambda e: e.max_index(out=i16[:, j, 0:8], in_max=v16[:, j, 0:8], in_values=scj), reads=[d_sc4, d_v16], writes=[d_i16])
                        kb.op("dve", lambda e: e.match_replace(out=wk[:], in_to_replace=v16[:, j, 0:8], in_values=scj, imm_value=-1e30),
                              reads=[d_sc4, d_v16], writes=[d_wk])
                        kb.op("dve", lambda e: e.max(out=v16[:, j, 8:16], in_=wk[:]), reads=[d_wk], writes=[d_v16])
                        kb.op("dve", lambda e: e.max_index(out=i16[:, j, 8:16], in_max=v16[:, j, 8:16], in_values=wk[:]), reads=[d_wk, d_v16], writes=[d_i16])
                    yield
                i16f, d_i16f = i16fpool.get()
                kb.op("dve", lambda e: e.tensor_copy(out=i16f[:], in_=i16[:]), reads=[d_i16], writes=[d_i16f])
                ts, d_ts = tspool.get()
                eidf, d_eidf = eidfpool.get()
                kb.op("dve", lambda e: e.memset(eidf[:], 0.0), writes=[d_eidf])
                for hd in range(8):
                    j1, j2 = 2 * hd, 2 * hd + 1
                    cand, d_cand = candpool.get()
                    cid, d_cid = cidpool.get()
                    kb.op("dve", lambda e: e.tensor_tensor(out=cand[:], in0=v16[:, j1, :].unsqueeze(2).to_broadcast([128, 16, 16]),
                                                           in1=v16[:, j2:j2 + 1, :].to_broadcast([128, 16, 16]), op=ALU.add),
                          reads=[d_v16], writes=[d_cand])
                    kb.op("dve", lambda e: e.scalar_tensor_tensor(out=cid[:], in0=i16f[:, j1, :].unsqueeze(2).to_broadcast([128, 16, 16]), scalar=128.0,
                                                                  in1=i16f[:, j2:j2 + 1, :].to_broadcast([128, 16, 16]), op0=ALU.mult, op1=ALU.add),
                          reads=[d_i16f], writes=[d_cid])
                    candf = cand[:].rearrange("p a b -> p (a b)")
                    cidf = cid[:].rearrange("p a b -> p (a b)")
                    wk2, d_wk2 = wk2pool.get()
                    kb.op("dve", lambda e: e.max(out=ts[:, hd, 0:8], in_=candf), reads=[d_cand], writes=[d_ts])
                    kb.op("dve", lambda e: e.match_replace(out=wk2[:], in_to_replace=ts[:, hd, 0:8], in_values=candf, imm_value=-1e30),
                          reads=[d_cand, d_ts], writes=[d_wk2])
                    kb.op("dve", lambda e: e.max(out=ts[:, hd, 8:16], in_=wk2[:]), reads=[d_wk2], writes=[d_ts])
                    j2t, d_j2t = j2pool.get()
                    for sl in range(16):
                        kb.op("dve", lambda e: e.scalar_tensor_tensor(out=j2t[:], in0=candf, scalar=ts[:, hd, sl:sl + 1], in1=cidf, op0=ALU.is_equal, op1=ALU.mult,
                                                                      accum_out=eidf[:, hd * 16 + sl:hd * 16 + sl + 1]),
                              reads=[d_cand, d_cid, d_ts], writes=[d_j2t, d_eidf])
                        if sl % 8 == 7:
                            yield
                eidi, d_eidi = eidipool.get()
                kb.op("dve", lambda e: e.tensor_scalar(out=eidf[:], in0=eidf[:], scalar1=float(NEXP - 1), scalar2=float(l * NEXP), op0=ALU.min, op1=ALU.add),
                      reads=[d_eidf], writes=[d_eidf])
                kb.op("dve", lambda e: e.tensor_copy(out=eidi[:], in_=eidf[:]), reads=[d_eidf], writes=[d_eidi])
                gt, d_gt = gpool.get()
                kb.op("dve", lambda e: e.tensor_tensor(out=gt[:], in0=ts[:], in1=ts[:, :, 0:1].to_broadcast([128, 8, 16]), op=ALU.subtract),
                      reads=[d_ts], writes=[d_gt])
                kb.op("act", lambda e: e.activation(out=gt[:], in_=gt[:], func=AF.Exp), reads=[d_gt], writes=[d_gt])
                sm, d_sm = sspool.get()
                kb.op("dve", lambda e: e.tensor_reduce(out=sm[:], in_=gt[:], axis=AX.X, op=ALU.add), reads=[d_gt], writes=[d_sm])
                kb.op("dve", lambda e: e.reciprocal(out=sm[:], in_=sm[:]), reads=[d_sm], writes=[d_sm])
                kb.op("dve", lambda e: e.tensor_tensor(out=gt[:], in0=gt[:], in1=sm[:].unsqueeze(2).to_broadcast([128, 8, 16]), op=ALU.mult),
                      reads=[d_gt, d_sm], writes=[d_gt])
                blocks[tb] = dict(xt=xt, d_xt=d_xt, h=h, d_h=d_h, eid=eidi, d_eid=d_eidi, gt=gt[:].rearrange("p a b -> p (a b)"), d_gt=d_gt, bufs={})

            def gathers(bk, c):
                for j in range(c * CH, (c + 1) * CH):
                    uv, d_uv = uvpool.get()
                    kb.dma("pool", lambda q: q.indirect_dma_start(out=uv[:], out_offset=None, in_=peer_uv,
                                                                  in_offset=bass.IndirectOffsetOnAxis(ap=bk["eid"][:, j:j + 1], axis=0)),
                           reads=[bk["d_eid"]], writes=[d_uv])
                    bk["bufs"][j] = (uv, d_uv)

            for _ in p1_gen(0):
                pass
            gathers(blocks[0], 0)
            for tb in range(NT):
                rows = slice(tb * 128, (tb + 1) * 128)
                cur = blocks.pop(tb)
                gen = p1_gen(tb + 1) if tb + 1 < NT else iter(())
                xt, d_xt, gt, d_gt, h, d_h = cur["xt"], cur["d_xt"], cur["gt"], cur["d_gt"], cur["h"], cur["d_h"]
                act, _ = actpool.get()
                coef, _ = coefpool.get()
                d_actc = chunkdeps.setdefault(id(act), [Dep() for _ in range(NCH)])
                d_coefc = chunkdeps.setdefault(id(coef), [Dep() for _ in range(NCH)])
                acc, d_acc = accpool.get()
                pa0, d_pa0 = psum_acc.get()
                pa1, d_pa1 = psum_acc.get()
                state = {"pe": 0, "dve": 0}

                def dots(c):
                    cs_ = slice(c * CH, (c + 1) * CH)
                    kb.op("dve", lambda e: e.memset(act[:, cs_], 0.0), reads=[], writes=[d_actc[c]])
                    for j in range(c * CH, (c + 1) * CH):
                        uv, d_uv = cur["bufs"][j]
                        kb.op("dve", lambda e: e.scalar_tensor_tensor(out=junk[:], in0=uv[:, 0:D], scalar=1.0, in1=h[:], op0=ALU.mult, op1=ALU.mult,
                                                                      accum_out=act[:, j:j + 1]), reads=[d_uv, d_h], writes=[d_junk, d_actc[c]])
                    kb.op("act", lambda e: e.activation(out=coef[:, cs_], in_=act[:, cs_], func=AF.Gelu_apprx_tanh), reads=[d_actc[c]], writes=[d_coefc[c]])
                    kb.op("dve", lambda e: e.tensor_tensor(out=coef[:, cs_], in0=coef[:, cs_], in1=gt[:, cs_], op=ALU.mult),
                          reads=[d_coefc[c], d_gt], writes=[d_coefc[c]])

                def vside(c):
                    for jj, j in enumerate(range(c * CH, (c + 1) * CH)):
                        uv, d_uv = cur["bufs"].pop(j)
                        if jj < NDVE:
                            if state["dve"] == 0:
                                kb.op("dve", lambda e: e.tensor_scalar(out=acc[:], in0=uv[:, D:2 * D], scalar1=coef[:, j:j + 1], scalar2=None, op0=ALU.mult),
                                      reads=[d_uv, d_coefc[c]], writes=[d_acc])
                            else:
                                kb.op("dve", lambda e: e.scalar_tensor_tensor(out=acc[:], in0=uv[:, D:2 * D], scalar=coef[:, j:j + 1], in1=acc[:],
                                                                              op0=ALU.mult, op1=ALU.add), reads=[d_uv, d_coefc[c], d_acc], writes=[d_acc])
                            state["dve"] += 1
                        else:
                            tmp, d_tmp = tmppool.get()
                            kb.op("act", lambda e: e.activation(out=tmp[:], in_=uv[:, D:2 * D], func=AF.Copy, scale=coef[:, j:j + 1]),
                                  reads=[d_uv, d_coefc[c]], writes=[d_tmp])
                            first = state["pe"] == 0
                            last = (j == 127)
                            kb.op("pe", lambda e: e.matmul(pa0[:], lhsT=ident, rhs=tmp[:, 0:512], start=first, stop=last), reads=[d_tmp, d_consts], writes=[d_pa0])
                            kb.op("pe", lambda e: e.matmul(pa1[:], lhsT=ident, rhs=tmp[:, 512:1024], start=first, stop=last), reads=[d_tmp, d_consts], writes=[d_pa1])
                            state["pe"] += 1

                for c in range(NCH):
                    if c + 1 < NCH:
                        gathers(cur, c + 1)
                    else:
                        for _ in gen:
                            pass
                        if tb + 1 < NT:
                            gathers(blocks[tb + 1], 0)
                    dots(c)
                    next(gen, None)
                    vside(c)
                if NDVE > 0:
                    kb.op("dve", lambda e: e.tensor_tensor(out=acc[:, 0:512], in0=pa0[:], in1=acc[:, 0:512], op=ALU.add), reads=[d_pa0, d_acc], writes=[d_acc])
                    kb.op("dve", lambda e: e.tensor_tensor(out=acc[:, 512:1024], in0=pa1[:], in1=acc[:, 512:1024], op=ALU.add), reads=[d_pa1, d_acc], writes=[d_acc])
                    kb.op("dve", lambda e: e.tensor_tensor(out=acc[:], in0=acc[:], in1=g2, op=ALU.mult), reads=[d_acc, d_mod], writes=[d_acc])
                else:
                    kb.op("dve", lambda e: e.tensor_tensor(out=acc[:, 0:512], in0=pa0[:], in1=g2[:, 0:512], op=ALU.mult), reads=[d_pa0, d_mod], writes=[d_acc])
                    kb.op("dve", lambda e: e.tensor_tensor(out=acc[:, 512:1024], in0=pa1[:], in1=g2[:, 512:1024], op=ALU.mult), reads=[d_pa1, d_mod], writes=[d_acc])
                kb.op("dve", lambda e: e.tensor_tensor(out=acc[:], in0=acc[:], in1=xt[:], op=ALU.add), reads=[d_acc, d_xt], writes=[d_acc])
                if l == L - 1:
                    rs, d_rs = rms_scale(es, acc[:], d_acc, D, sspool, junk[:], d_junk)
                    kb.op("dve", lambda e: e.scalar_tensor_tensor(out=acc[:], in0=acc[:], scalar=rs[:, 0:1], in1=fn_bc[:], op0=ALU.mult, op1=ALU.mult),
                          reads=[d_acc, d_rs, d_fn], writes=[d_acc])
                    kb.dma("sp", lambda q: q.dma_start(out=out[rows, :], in_=acc[:]), reads=[d_acc])
                else:
                    kb.dma("sp", lambda q: q.dma_start(out=xs[rows, :], in_=acc[:]), reads=[d_acc])
            kb.barrier()

    if dbg:
        for name, src, shape, dt in (("d_xs", xs, [S, D], F32), ("d_qT", qT_s, [512, S], F32), ("d_kT", kT_s, [512, S], F32),
                                     ("d_v", v_s, [S, 512], F32), ("d_ya", ya_s, [S, 512], F32), ("d_ybT", ybT_s, [512, S], F32),
                                     ):
            o = nc.dram_tensor(name, shape, dt, kind="ExternalOutput").ap()
            kb.dma("sp", lambda q: q.dma_start(out=o, in_=src))
        kb.barrier()
    print("instructions", kb.nins, "waits", kb.nwait)
    return nc


def prep_inputs(inp, b, L):
    f = lambda a: np.ascontiguousarray(a, dtype=np.float32)
    m = {
        "x": f(inp["x"][b]),
        "c": f(inp["c"][b].reshape(8, 128).T),
        "ada_w": f(inp["ada_w"][:L]),
        "ada_b": f(inp["ada_b"][:L]),
        "norm_mix": f(inp["norm_mix"][:L]),
        "norm_ffn": f(inp["norm_ffn"][:L]),
        "w_in": f(inp["w_in"][:L]),
        "gm_wsT": f(np.transpose(inp["gm_ws"][:L], (0, 3, 1, 2))),
        "gm_bsT": f(np.transpose(inp["gm_bs"][:L], (0, 2, 1))),
        "gm_vnorm": f(inp["gm_vnorm"][:L]),
        "out_norm_a": f(inp["out_norm_a"][:L]),
        "out_norm_bT": f(np.transpose(inp["out_norm_b"][:L].reshape(L, 4, 128), (0, 2, 1))),
        "w_out": f(inp["w_out"][:L]),
        "peer_wq": f(inp["peer_wq"][:L]),
        "peer_k1T": f(np.transpose(inp["peer_k1"][:L], (0, 2, 1))),
        "peer_k2T": f(np.transpose(inp["peer_k2"][:L], (0, 2, 1))),
        "peer_uv": np.concatenate([f(inp["peer_u"][:L]).reshape(L * NEXP, D), f(inp["peer_v"][:L]).reshape(L * NEXP, D)], axis=1),
        "final_norm": f(inp["final_norm"]),
        "consts": make_consts(),
    }
    return m


def kernel(**inputs):
    inp = {k: np.asarray(v) for k, v in inputs.items()}
    B, S, _ = inp["x"].shape
    L = inp["ada_w"].shape[0]
    nc = build(L, S)
    shared = prep_inputs(inp, 0, L)
    in_maps = []
    for b in range(B):
        m = dict(shared)
        m["x"] = np.ascontiguousarray(inp["x"][b], dtype=np.float32)
        m["c"] = np.ascontiguousarray(inp["c"][b].reshape(8, 128).T, dtype=np.float32)
        in_maps.append(m)
    res = run_bass_kernel_spmd(nc, in_maps, core_ids=list(range(B)))
    return np.stack([r["out"] for r in res.results], axis=0).astype(np.float32)
```

```python
import numpy as np
from contextlib import ExitStack
import concourse.bass as bass
import concourse.mybir as mybir
from concourse.bass_utils import run_bass_kernel_spmd

F32 = mybir.dt.float32
I32 = mybir.dt.int32
U32 = mybir.dt.uint32
AF = mybir.ActivationFunctionType
ALU = mybir.AluOpType
AX = mybir.AxisListType

D = 1024
NEXP = 16384
EPS = 1e-6
NDS = 40
NSW = 16
SAME_ENG_SYNC = True


class Dep:
    __slots__ = ("w", "r")

    def __init__(self):
        self.w = None
        self.r = {}


class KB:
    def __init__(self, nc):
        self.nc = nc
        self.es = ExitStack()
        self.eng = {"pe": nc.tensor, "act": nc.scalar, "dve": nc.vector, "pool": nc.gpsimd, "sp": nc.sync}
        self.esem = {e: self.es.enter_context(nc.semaphore("es_" + e)) for e in self.eng}
        self.ecount = {e: 0 for e in self.eng}
        self.known = {e: {} for e in self.eng}
        self.dsem = [self.es.enter_context(nc.semaphore("ds%d" % i)) for i in range(NDS + NSW)]
        self.dcount = [0] * (NDS + NSW)
        self.dnext = 0
        self.swnext = 0
        self.nwait = 0
        self.nins = 0

    def _sem(self, key):
        return self.esem[key[1]] if key[0] == "e" else self.dsem[key[1]]

    def _need(self, e, deps):
        need = {}
        for key, val in deps:
            if key[0] == "e" and key[1] == e:
                if e == "pe" or not SAME_ENG_SYNC:
                    continue
            if self.known[e].get(key, 0) >= val:
                continue
            if need.get(key, 0) < val:
                need[key] = val
        return list(need.items())

    def _wait(self, e, deps, keep_one=False):
        need = self._need(e, deps)
        emb = None
        if keep_one and need:
            emb = need.pop()
        for key, val in need:
            self.eng[e].wait_ge(self._sem(key), val)
            self.known[e][key] = val
            self.nwait += 1
        return emb

    def _embed(self, e, ins, emb):
        if emb is not None:
            key, val = emb
            ins._wait_ge(self._sem(key), val)
            self.known[e][key] = val

    @staticmethod
    def _collect(reads, writes):
        deps = []
        for d in reads:
            if d.w is not None:
                deps.append(d.w)
        for d in writes:
            if d.w is not None:
                deps.append(d.w)
            deps.extend(d.r.items())
        return deps

    @staticmethod
    def _record(ev, reads, writes):
        key, val = ev
        for d in reads:
            if d.r.get(key, 0) < val:
                d.r[key] = val
        for d in writes:
            d.w = ev
            d.r = {}

    def op(self, e, fn, reads=(), writes=()):
        emb = self._wait(e, self._collect(reads, writes), keep_one=True)
        ins = fn(self.eng[e])
        self._embed(e, ins, emb)
        self.ecount[e] += 1
        ins.then_inc(self.esem[e], 1)
        self._record((("e", e), self.ecount[e]), reads, writes)
        self.nins += 1

    def dma(self, q, fn, reads=(), writes=()):
        deps = self._collect(reads, writes)
        if q == "pool":
            k = NDS + self.swnext
            self.swnext = (self.swnext + 1) % NSW
        else:
            k = self.dnext
            self.dnext = (k + 1) % NDS
        if self.dcount[k] > 0:
            deps.append((("d", k), self.dcount[k]))
        emb = self._wait(q, deps, keep_one=True)
        ins = fn(self.eng[q])
        self._embed(q, ins, emb)
        self.dcount[k] += 16
        ins.then_inc(self.dsem[k], 16)
        self._record((("d", k), self.dcount[k]), reads, writes)
        self.nins += 1

    def barrier(self):
        allev = [(("e", e), c) for e, c in self.ecount.items() if c > 0]
        allev += [(("d", k), c) for k, c in enumerate(self.dcount) if c > 0]
        for e in self.eng:
            for key, val in allev:
                if self.known[e].get(key, 0) < val:
                    self.eng[e].wait_ge(self._sem(key), val)
                    self.known[e][key] = val
                    self.nwait += 1


class Pool:
    uid = 0

    def __init__(self, es, nc, name, shape, dt, n, psum=False):
        self.t = []
        for i in range(n):
            mk = nc.psum_tensor if psum else nc.sbuf_tensor
            Pool.uid += 1
            self.t.append((es.enter_context(mk("%s_%d_%d" % (name, i, Pool.uid), shape, dt)), Dep()))
        self.i = 0

    def get(self):
        r = self.t[self.i]
        self.i = (self.i + 1) % len(self.t)
        return r

    @classmethod
    def join(cls, *pools):
        p = cls.__new__(cls)
        p.t = [x for q in pools for x in q.t]
        p.i = 0
        return p


def make_consts():
    c = np.zeros((128, 128 * 3 + 4 * 512 + 1), np.float32)
    c[:, 0:128] = np.eye(128, dtype=np.float32)
    j = np.arange(128)[:, None]
    s = np.arange(128)[None, :]
    c[:, 128:256] = -(j >= s).astype(np.float32)
    c[:, 256:384] = -1.0
    t = np.arange(512)[None, :]
    for jd in range(4):
        c[:, 384 + jd * 512:384 + (jd + 1) * 512] = ((128 * jd + j) < t).astype(np.float32)
    c[:, 384 + 2048] = 1.0
    return c


def build(L, S, dbg=False, phases="ABCDEF"):
    nc = bass.Bass("TRN2", target_bir_lowering=False)
    NT = S // 128
    NG = S // 512
    assert S % 512 == 0

    def din(name, shape, dt=F32):
        return nc.dram_tensor(name, shape, dt, kind="ExternalInput").ap()

    def dscr(name, shape, dt=F32):
        return nc.dram_tensor(name, shape, dt, kind="Internal").ap()

    x_in = din("x", [S, D])
    c_in = din("c", [128, 8])
    ada_w = din("ada_w", [L, D, 6 * D])
    ada_b = din("ada_b", [L, 6 * D])
    norm_mix = din("norm_mix", [L, D])
    norm_ffn = din("norm_ffn", [L, D])
    w_in = din("w_in", [L, D, 2560])
    gm_wsT = din("gm_wsT", [L, 128, 8, 128])
    gm_bsT = din("gm_bsT", [L, 128, 8])
    gm_vnorm = din("gm_vnorm", [L, 512])
    out_norm_a = din("out_norm_a", [L, 512])
    out_norm_bT = din("out_norm_bT", [L, 128, 4])
    w_out = din("w_out", [L, D, D])
    peer_wq = din("peer_wq", [L, D, 2048])
    peer_k1T = din("peer_k1T", [L, 128, 128])
    peer_k2T = din("peer_k2T", [L, 128, 128])
    peer_uv = din("peer_uv", [L * NEXP, 2 * D])
    final_norm = din("final_norm", [D])
    consts_in = din("consts", [128, 2433])
    out = nc.dram_tensor("out", [S, D], F32, kind="ExternalOutput").ap()

    xs = dscr("xs", [S, D])
    qT_s = dscr("qT_s", [512, S])
    kT_s = dscr("kT_s", [512, S])
    v_s = dscr("v_s", [S, 512])
    ya_s = dscr("ya_s", [S, 512])
    ybT_s = dscr("ybT_s", [512, S])

    kb = KB(nc)
    top = kb.es

    def sbt(es, name, shape, dt=F32):
        Pool.uid += 1
        return es.enter_context(nc.sbuf_tensor("%s_t%d" % (name, Pool.uid), shape, dt))

    consts = sbt(top, "consts", [128, 2433]); d_consts = Dep()
    ident = consts[:, 0:128]
    negtri = consts[:, 128:256]
    negones = consts[:, 256:384]
    masks = [consts[:, 384 + jd * 512:384 + (jd + 1) * 512] for jd in range(4)]
    ones_col = consts[:, 2432:2433]
    mod = sbt(top, "mod", [128, 6, D]); d_mod = Dep()
    c_act = sbt(top, "c_act", [128, 8]); d_cact = Dep()
    psum = Pool(top, nc, "ps", [128, 512], F32, 6, psum=True)
    psum_acc = Pool(top, nc, "psacc", [128, 512], F32, 2, psum=True)
    psum8 = Pool.join(psum, psum_acc)

    kb.dma("sp", lambda q: q.dma_start(out=consts[:], in_=consts_in), writes=[d_consts])
    kb.dma("sp", lambda q: q.dma_start(out=c_act[:], in_=c_in), writes=[d_cact])
    with ExitStack() as es:
        sg = sbt(es, "sg", [128, 8]); d_sg = Dep()
        kb.op("act", lambda e: e.activation(out=sg[:], in_=c_act[:], func=AF.Sigmoid), reads=[d_cact], writes=[d_sg])
        kb.op("dve", lambda e: e.tensor_tensor(out=c_act[:], in0=c_act[:], in1=sg[:], op=ALU.mult), reads=[d_sg, d_cact], writes=[d_cact])
        kb.barrier()

    def rms_scale(es_pool, src, d_src, nfeat, ss_pool, junk, d_junk):
        ss, d_ss = ss_pool.get()
        s1 = ss[:, 0:1]
        kb.op("dve", lambda e: e.memset(s1, 0.0), writes=[d_ss])
        kb.op("dve", lambda e: e.scalar_tensor_tensor(out=junk, in0=src, scalar=1.0, in1=src, op0=ALU.mult, op1=ALU.mult,
                                                      accum_out=s1), reads=[d_src], writes=[d_junk, d_ss])
        kb.op("dve", lambda e: e.tensor_scalar(out=s1, in0=s1, scalar1=1.0 / nfeat, scalar2=EPS, op0=ALU.mult, op1=ALU.add),
              reads=[d_ss], writes=[d_ss])
        kb.op("act", lambda e: e.sqrt(out=s1, in_=s1), reads=[d_ss], writes=[d_ss])
        kb.op("dve", lambda e: e.reciprocal(out=s1, in_=s1), reads=[d_ss], writes=[d_ss])
        return ss, d_ss

    for l in range(L):
        x_src = x_in if l == 0 else xs
        with ExitStack() as es:
            c_rep = sbt(es, "c_rep", [128, 8, 128]); d_crep = Dep()
            kb.op("dve", lambda e: e.tensor_copy(out=c_rep[:], in_=c_act[:].unsqueeze(2).to_broadcast([128, 8, 128])),
                  reads=[d_cact], writes=[d_crep])
            wpool = Pool(es, nc, "adaw", [128, 8, 512], F32, 4)
            bpool = Pool(es, nc, "adab", [128, 512], F32, 4)
            nm = sbt(es, "nm", [128, 2, D]); d_nm = Dep()
            kb.dma("sp", lambda q: q.dma_start(out=nm[:, 0, :], in_=norm_mix[l].partition_broadcast(128)), writes=[d_nm])
            kb.dma("sp", lambda q: q.dma_start(out=nm[:, 1, :], in_=norm_ffn[l].partition_broadcast(128)), writes=[d_nm])
            for n in range(12):
                wt, d_wt = wpool.get()
                bt, d_bt = bpool.get()
                kb.dma("sp", lambda q: q.dma_start(out=wt[:], in_=ada_w[l][:, n * 512:(n + 1) * 512].rearrange("(kc p) n -> p kc n", p=128)),
                       writes=[d_wt])
                kb.dma("sp", lambda q: q.dma_start(out=bt[:], in_=ada_b[l, n * 512:(n + 1) * 512].partition_broadcast(128)), writes=[d_bt])
                pt, d_pt = psum.get()
                for kc in range(8):
                    kb.op("pe", lambda e: e.matmul(pt[:], lhsT=c_rep[:, kc, :], rhs=wt[:, kc, :], start=(kc == 0), stop=(kc == 7)),
                          reads=[d_crep, d_wt], writes=[d_pt])
                dst = mod[:, n // 2, (n % 2) * 512:(n % 2 + 1) * 512]
                kb.op("dve", lambda e: e.tensor_tensor(out=dst, in0=pt[:], in1=bt[:], op=ALU.add), reads=[d_pt, d_bt], writes=[d_mod])
            kb.op("dve", lambda e: e.scalar_tensor_tensor(out=mod[:, 1, :], in0=mod[:, 1, :], scalar=1.0, in1=nm[:, 0, :], op0=ALU.add, op1=ALU.mult),
                  reads=[d_nm, d_mod], writes=[d_mod])
            kb.op("dve", lambda e: e.scalar_tensor_tensor(out=mod[:, 4, :], in0=mod[:, 4, :], scalar=1.0, in1=nm[:, 1, :], op0=ALU.add, op1=ALU.mult),
                  reads=[d_nm, d_mod], writes=[d_mod])
            kb.barrier()
        sh1, A1, g1, sh2, A2, g2 = (mod[:, i, :] for i in range(6))

        def norm_mod(es, xt, d_xt, A, sh, sspool, junk, d_junk, h, d_h):
            rs, d_rs = rms_scale(es, xt[:], d_xt, D, sspool, junk[:], d_junk)
            kb.op("dve", lambda e: e.scalar_tensor_tensor(out=h[:], in0=xt[:], scalar=rs[:, 0:1], in1=A, op0=ALU.mult, op1=ALU.mult),
                  reads=[d_xt, d_rs, d_mod], writes=[d_h])
            kb.op("pool", lambda e: e.tensor_tensor(out=h[:], in0=h[:], in1=sh, op=ALU.add), reads=[d_h, d_mod], writes=[d_h])

        def transpose_to(src, d_src, nchunk, dst_fn, d_dst, eng="act", pp=None):
            pp = pp or psum
            for c0 in range(0, nchunk, 4):
                pt, d_pt = pp.get()
                n = min(4, nchunk - c0)
                for c in range(c0, c0 + n):
                    kb.op("pe", lambda e: e.transpose(out=pt[:, (c - c0) * 128:(c - c0 + 1) * 128], in_=src[:, c * 128:(c + 1) * 128], identity=ident),
                          reads=[d_src, d_consts], writes=[d_pt])
                for c in range(c0, c0 + n):
                    if eng == "act":
                        kb.op("act", lambda e: e.copy(out=dst_fn(c), in_=pt[:, (c - c0) * 128:(c - c0 + 1) * 128]), reads=[d_pt], writes=[d_dst])
                    else:
                        kb.op("dve", lambda e: e.tensor_copy(out=dst_fn(c), in_=pt[:, (c - c0) * 128:(c - c0 + 1) * 128]), reads=[d_pt], writes=[d_dst])

        if "B" in phases:
          with ExitStack() as es:
            wi = sbt(es, "wi", [128, 8, 2560]); d_wi = [Dep() for _ in range(8)]
            for kc in range(8):
                kb.dma("sp", lambda q: q.dma_start(out=wi[:, kc, :], in_=w_in[l][kc * 128:(kc + 1) * 128, :]), writes=[d_wi[kc]])
            wsT = sbt(es, "wsT", [128, 8, 128]); d_wsT = Dep()
            kb.dma("sp", lambda q: q.dma_start(out=wsT[:], in_=gm_wsT[l]), writes=[d_wsT])
            kb.op("dve", lambda e: e.memset(wsT[64:128, :, 0:64], 0.0), writes=[d_wsT])
            bsT = sbt(es, "bsT", [128, 8]); d_bsT = Dep()
            kb.dma("sp", lambda q: q.dma_start(out=bsT[:], in_=gm_bsT[l]), writes=[d_bsT])
            vg = sbt(es, "vg", [128, 512]); d_vg = Dep()
            kb.dma("sp", lambda q: q.dma_start(out=vg[:], in_=gm_vnorm[l].partition_broadcast(128)), writes=[d_vg])
            ona = sbt(es, "ona", [128, 512]); d_ona = Dep()
            kb.dma("sp", lambda q: q.dma_start(out=ona[:], in_=out_norm_a[l].partition_broadcast(128)), writes=[d_ona])
            xpool = Pool(es, nc, "bx", [128, D], F32, 2)
            hpool = Pool(es, nc, "bh", [128, D], F32, 4)
            junk = sbt(es, "bjunk", [128, D]); d_junk = Dep()
            sspool = Pool(es, nc, "bss", [128, 8], F32, 8)
            hTpool = Pool(es, nc, "bhT", [128, 8, 512], F32, 1)
            gupool = Pool(es, nc, "bgu", [128, 512], F32, 5)
            gvpool = Pool(es, nc, "bgv", [128, 512], F32, 4)
            vapool = Pool(es, nc, "bva", [128, 512], F32, 2)
            qkpool = Pool(es, nc, "bqk", [128, 512], F32, 2)
            wkpool_b = Pool(es, nc, "bwk", [128, 512], F32, 6)
            hTs = {}

            def b_prep_norm(g):
                hs_ = []
                for j in range(4):
                    tb = g * 4 + j
                    xt, d_xt = xpool.get()
                    kb.dma("sp", lambda q: q.dma_start(out=xt[:], in_=x_src[tb * 128:(tb + 1) * 128, :]), writes=[d_xt])
                    h, d_h = hpool.get()
                    norm_mod(es, xt, d_xt, A1, sh1, sspool, junk, d_junk, h, d_h)
                    hs_.append((h, d_h))
                hTs[("n", g)] = hs_

            def b_prep_T(g):
                hT, d_hT = hTpool.get()
                for j, (h, d_h) in enumerate(hTs.pop(("n", g))):
                    transpose_to(h, d_h, 8, lambda c: hT[:, c, j * 128:(j + 1) * 128], d_hT, pp=psum8)
                hTs[g] = (hT, d_hT)

            def b_proj_tok(g, j, res):
                hT, d_hT = hTs[g]
                tb = g * 4 + j
                rows = slice(tb * 128, (tb + 1) * 128)
                for name, c0 in (("u", 0), ("v", 512), ("va", 2048)):
                    pt, d_pt = psum8.get()
                    for kc in range(8):
                        kb.op("pe", lambda e: e.matmul(pt[:], lhsT=hT[:, kc, j * 128:(j + 1) * 128], rhs=wi[:, kc, c0:c0 + 512],
                                                       start=(kc == 0), stop=(kc == 7)), reads=[d_hT, d_wi[kc]], writes=[d_pt])
                    if name == "va":
                        t, d_t = vapool.get()
                        kb.op("act", lambda e: e.copy(out=t[:], in_=pt[:]), reads=[d_pt], writes=[d_t])
                        kb.dma("sp", lambda q: q.dma_start(out=v_s[rows, :], in_=t[:]), reads=[d_t])
                    else:
                        t, d_t = (gupool if name == "u" else gvpool).get()
                        kb.op("act", lambda e: e.activation(out=t[:], in_=pt[:], func=AF.Gelu_apprx_tanh), reads=[d_pt], writes=[d_t])
                    res[(j, name)] = (t, d_t)

            def b_proj_qk(g):
                hT, d_hT = hTs[g]
                for cc in range(8):
                    col0 = 1024 + cc * 128
                    pt, d_pt = psum8.get()
                    for kc in range(8):
                        kb.op("pe", lambda e: e.matmul(pt[:], lhsT=wi[:, kc, col0:col0 + 128], rhs=hT[:, kc, :], start=(kc == 0), stop=(kc == 7)),
                              reads=[d_hT, d_wi[kc]], writes=[d_pt])
                    t, d_t = qkpool.get()
                    if cc < 4:
                        kb.op("act", lambda e: e.activation(out=t[:], in_=pt[:], func=AF.Copy, scale=0.125), reads=[d_pt], writes=[d_t])
                        dst = qT_s[cc * 128:(cc + 1) * 128, g * 512:(g + 1) * 512]
                    else:
                        kb.op("act", lambda e: e.copy(out=t[:], in_=pt[:]), reads=[d_pt], writes=[d_t])
                        dst = kT_s[(cc - 4) * 128:(cc - 3) * 128, g * 512:(g + 1) * 512]
                    kb.dma("sp", lambda q: q.dma_start(out=dst, in_=t[:]), reads=[d_t])

            def b_gmlp_pre(g, j, res):
                gv, d_gv = res[(j, "v")]
                sq, d_sq = wkpool_b.get()
                kb.op("pool", lambda e: e.tensor_tensor(out=sq[:], in0=gv[:], in1=gv[:], op=ALU.mult), reads=[d_gv], writes=[d_sq])
                rh, d_rh = sspool.get()
                kb.op("dve", lambda e: e.tensor_reduce(out=rh[:], in_=sq[:].rearrange("p (h c) -> p h c", h=8), axis=AX.X, op=ALU.add),
                      reads=[d_sq], writes=[d_rh])
                kb.op("dve", lambda e: e.tensor_scalar(out=rh[:], in0=rh[:], scalar1=1.0 / 64, scalar2=EPS, op0=ALU.mult, op1=ALU.add),
                      reads=[d_rh], writes=[d_rh])
                kb.op("act", lambda e: e.sqrt(out=rh[:], in_=rh[:]), reads=[d_rh], writes=[d_rh])
                kb.op("dve", lambda e: e.reciprocal(out=rh[:], in_=rh[:]), reads=[d_rh], writes=[d_rh])
                vh, d_vh = wkpool_b.get()
                kb.op("dve", lambda e: e.tensor_tensor(out=vh[:].rearrange("p (h c) -> p h c", h=8), in0=gv[:].rearrange("p (h c) -> p h c", h=8),
                                                       in1=rh[:].unsqueeze(2).to_broadcast([128, 8, 64]), op=ALU.mult),
                      reads=[d_gv, d_rh], writes=[d_vh])
                kb.op("pool", lambda e: e.tensor_tensor(out=vh[:], in0=vh[:], in1=vg[:], op=ALU.mult), reads=[d_vh, d_vg], writes=[d_vh])
                res[(j, "vh")] = (vh, d_vh)

            def b_gmlp_post(g, j, res):
                tb = g * 4 + j
                rows = slice(tb * 128, (tb + 1) * 128)
                gu, d_gu = res[(j, "u")]
                vh, d_vh = res[(j, "vh")]
                pz, d_pz = psum8.get()
                for hh in range(8):
                    kb.op("pe", lambda e: e.matmul(pz[:, hh * 64:(hh + 1) * 64], lhsT=wsT[:, hh, :], rhs=vh[:, hh * 64:(hh + 1) * 64],
                                                   start=True, stop=True), reads=[d_wsT, d_vh], writes=[d_pz])
                ya, d_ya = wkpool_b.get()
                kb.op("dve", lambda e: e.tensor_tensor(out=ya[:].rearrange("p (h c) -> p h c", h=8), in0=pz[:].rearrange("p (h c) -> p h c", h=8),
                                                       in1=bsT[:].unsqueeze(2).to_broadcast([128, 8, 64]), op=ALU.add),
                      reads=[d_pz, d_bsT], writes=[d_ya])
                kb.op("pool", lambda e: e.tensor_tensor(out=ya[:], in0=ya[:], in1=gu[:], op=ALU.mult), reads=[d_ya, d_gu], writes=[d_ya])
                ra, d_ra = rms_scale(es, ya[:], d_ya, 512, sspool, junk[:, 0:512], d_junk)
                yan, d_yan = wkpool_b.get()
                kb.op("dve", lambda e: e.scalar_tensor_tensor(out=yan[:], in0=ya[:], scalar=ra[:, 0:1], in1=ona[:], op0=ALU.mult, op1=ALU.mult),
                      reads=[d_ya, d_ra, d_ona], writes=[d_yan])
                kb.dma("sp", lambda q: q.dma_start(out=ya_s[rows, :], in_=yan[:]), reads=[d_yan])

            b_prep_norm(0)
            b_prep_T(0)
            prev = None
            for g in range(NG + 1):
                if g + 1 < NG:
                    b_prep_norm(g + 1)
                res = {}
                for j in range(4):
                    if prev is not None:
                        b_gmlp_pre(g - 1, j, prev)
                    if g < NG:
                        b_proj_tok(g, j, res)
                    if prev is not None:
                        b_gmlp_post(g - 1, j, prev)
                if g < NG:
                    b_proj_qk(g)
                if g + 1 < NG:
                    b_prep_T(g + 1)
                prev = res if g < NG else None
            kb.barrier()

        if "C" in phases:
          with ExitStack() as es:
            qpool = Pool(es, nc, "cq", [128, S], F32, 2)
            kpool = Pool(es, nc, "ck", [128, S], F32, 2)
            vpool = Pool(es, nc, "cv", [128, NT, 128], F32, 2)
            epool = Pool(es, nc, "ce", [128, 512], F32, 4)
            sppool = Pool(es, nc, "csp", [128, 512], F32, 8)
            apool = Pool(es, nc, "ca", [128, 512], F32, 8)
            cspool = Pool(es, nc, "ccs", [128, 512], F32, 6)
            ybpool = Pool(es, nc, "cyb", [128, 512], F32, 2)
            tiles = []
            for hp in range(4):
                for g in range(NG):
                    nkb = 4 * g + 4
                    for idx, kbk in enumerate(reversed(range(nkb))):
                        tiles.append((hp, g, idx, kbk, nkb))
            loaded = {}
            grp = {}
            st = {}

            def operands(hp):
                if hp not in loaded:
                    qT, d_q = qpool.get()
                    kT, d_k = kpool.get()
                    vv, d_v = vpool.get()
                    kb.dma("sp", lambda q: q.dma_start(out=qT[:], in_=qT_s[hp * 128:(hp + 1) * 128, :]), writes=[d_q])
                    kb.dma("sp", lambda q: q.dma_start(out=kT[:], in_=kT_s[hp * 128:(hp + 1) * 128, :]), writes=[d_k])
                    kb.dma("sp", lambda q: q.dma_start(out=vv[:], in_=v_s[:, hp * 128:(hp + 1) * 128].rearrange("(kb p) d -> p kb d", p=128)),
                           writes=[d_v])
                    loaded[hp] = (qT, d_q, kT, d_k, vv, d_v)
                return loaded[hp]

            def stage1(i):
                hp, g, idx, kbk, nkb = tiles[i]
                qT, d_q, kT, d_k, vv, d_v = operands(hp)
                if hp + 1 < 4 and g == 0 and idx == 3:
                    operands(hp + 1)
                qs = slice(g * 512, (g + 1) * 512)
                ks = slice(kbk * 128, (kbk + 1) * 128)
                jd = kbk - 4 * g
                if idx == 0:
                    grp[(hp, g)] = psum_acc.get() + cspool.get() + cspool.get()
                pzs = [psum.get() for _ in range(2)]
                for hh in range(2):
                    pr = slice(hh * 64, (hh + 1) * 64)
                    pz, d_pz = pzs[hh]
                    kb.op("pe", lambda e: e.matmul(pz[:], lhsT=kT[pr, ks], rhs=qT[pr, qs], start=True, stop=False, skip_group_check=True),
                          reads=[d_q, d_k], writes=[d_pz])
                sps = []
                for hh in range(2):
                    pz, d_pz = pzs[hh]
                    et, d_et = epool.get()
                    kb.op("act", lambda e: e.activation(out=et[:], in_=pz[:], func=AF.Exp), reads=[d_pz], writes=[d_et])
                    spt, d_spt = sppool.get()
                    kb.op("act", lambda e: e.activation(out=spt[:], in_=et[:], func=AF.Ln, bias=1.0), reads=[d_et], writes=[d_spt])
                    if jd >= 0:
                        kb.op("pool", lambda e: e.tensor_tensor(out=spt[:], in0=spt[:], in1=masks[jd], op=ALU.mult),
                              reads=[d_spt, d_consts], writes=[d_spt])
                    sps.append((spt, d_spt))
                st[i] = (pzs, sps)

            def stage2(i):
                hp, g, idx, kbk, nkb = tiles[i]
                jd = kbk - 4 * g
                pzs, sps = st[i]
                gr = grp[(hp, g)]
                css = [(gr[2], gr[3]), (gr[4], gr[5])]
                for hh in range(2):
                    pz, d_pz = pzs[hh]
                    spt, d_spt = sps[hh]
                    cs, d_cs = css[hh]
                    kb.op("pe", lambda e: e.matmul(pz[:], lhsT=negtri, rhs=spt[:], start=False, stop=(idx == 0), skip_group_check=True),
                          reads=[d_spt, d_consts], writes=[d_pz])
                    if idx > 0:
                        kb.op("pe", lambda e: e.matmul(pz[:], lhsT=negones, rhs=cs[:], start=False, stop=True, skip_group_check=True),
                              reads=[d_cs, d_consts], writes=[d_pz])
                ats = []
                for hh in range(2):
                    pz, d_pz = pzs[hh]
                    spt, d_spt = sps[hh]
                    cs, d_cs = css[hh]
                    at, d_at = apool.get()
                    kb.op("act", lambda e: e.activation(out=at[:], in_=pz[:], func=AF.Exp), reads=[d_pz], writes=[d_at])
                    if jd >= 0:
                        kb.op("pool", lambda e: e.tensor_tensor(out=at[:], in0=at[:], in1=masks[jd], op=ALU.mult),
                              reads=[d_at, d_consts], writes=[d_at])
                    if idx < nkb - 1:
                        if idx == 0:
                            kb.op("dve", lambda e: e.tensor_copy(out=cs[:], in_=spt[:]), reads=[d_spt], writes=[d_cs])
                        else:
                            kb.op("dve", lambda e: e.tensor_tensor(out=cs[:], in0=cs[:], in1=spt[:], op=ALU.add),
                                  reads=[d_spt, d_cs], writes=[d_cs])
                    ats.append((at, d_at))
                st[i] = ats

            def stage3(i):
                hp, g, idx, kbk, nkb = tiles[i]
                qT, d_q, kT, d_k, vv, d_v = operands(hp)
                qs = slice(g * 512, (g + 1) * 512)
                ats = st.pop(i)
                gr = grp[(hp, g)]
                po, d_po = gr[0], gr[1]
                for hh in range(2):
                    pr = slice(hh * 64, (hh + 1) * 64)
                    at, d_at = ats[hh]
                    kb.op("pe", lambda e: e.matmul(po[pr, :], lhsT=vv[:, kbk, pr], rhs=at[:], start=(idx == 0), stop=(idx == nkb - 1)),
                          reads=[d_v, d_at], writes=[d_po])
                if idx == nkb - 1:
                    yb, d_yb = ybpool.get()
                    kb.op("dve", lambda e: e.tensor_copy(out=yb[:], in_=po[:]), reads=[d_po], writes=[d_yb])
                    kb.dma("sp", lambda q: q.dma_start(out=ybT_s[hp * 128:(hp + 1) * 128, qs], in_=yb[:]), reads=[d_yb])
                    del grp[(hp, g)]

            nt = len(tiles)
            for step in range(nt + 2):
                if step < nt:
                    stage1(step)
                if 0 <= step - 1 < nt:
                    stage2(step - 1)
                if 0 <= step - 2 < nt:
                    stage3(step - 2)
            kb.barrier()

        if "D" in phases:
          with ExitStack() as es:
            wo = sbt(es, "wo", [128, 8, D]); d_wo = [Dep() for _ in range(8)]
            for kc in range(8):
                kb.dma("sp", lambda q: q.dma_start(out=wo[:, kc, :], in_=w_out[l][kc * 128:(kc + 1) * 128, :]), writes=[d_wo[kc]])
            gb = sbt(es, "gb", [128, 4]); d_gb = Dep()
            kb.dma("sp", lambda q: q.dma_start(out=gb[:], in_=out_norm_bT[l]), writes=[d_gb])
            xpool = Pool(es, nc, "dx", [128, D], F32, 3)
            xnpool = Pool(es, nc, "dxn", [128, D], F32, 3)
            yanpool = Pool(es, nc, "dyan", [128, 512], F32, 3)
            ybpool = Pool(es, nc, "dyb", [128, 4, 128], F32, 3)
            yaTpool = Pool(es, nc, "dyaT", [128, 512], F32, 2)
            sqbpool = Pool(es, nc, "dsqb", [128, 512], F32, 2)
            ybgpool = Pool(es, nc, "dybg", [128, 512], F32, 2)
            mainpool = Pool(es, nc, "dmain", [128, 512], F32, 4)
            sspool = Pool(es, nc, "dss", [128, 8], F32, 4)
            preps = {}

            loads = {}

            def d_load(tb):
                rows = slice(tb * 128, (tb + 1) * 128)
                xt, d_xt = xpool.get()
                kb.dma("sp", lambda q: q.dma_start(out=xt[:], in_=x_src[rows, :]), writes=[d_xt])
                yan, d_yan = yanpool.get()
                kb.dma("sp", lambda q: q.dma_start(out=yan[:], in_=ya_s[rows, :]), writes=[d_yan])
                ybT, d_ybT = ybpool.get()
                kb.dma("sp", lambda q: q.dma_start(out=ybT[:], in_=ybT_s[:, rows].rearrange("(c p) t -> p c t", p=128)), writes=[d_ybT])
                loads[tb] = (xt, d_xt, yan, d_yan, ybT, d_ybT)

            def d_prep(tb):
                xt, d_xt, yan, d_yan, ybT, d_ybT = loads.pop(tb)
                yaT, d_yaT = yaTpool.get()
                transpose_to(yan, d_yan, 4, lambda c: yaT[:, c * 128:(c + 1) * 128], d_yaT, pp=psum8)
                sqb, d_sqb = sqbpool.get()
                kb.op("act", lambda e: e.activation(out=sqb[:], in_=ybT[:].rearrange("p c t -> p (c t)"), func=AF.Square),
                      reads=[d_ybT], writes=[d_sqb])
                pq, d_pq = psum8.get()
                for c in range(4):
                    kb.op("pe", lambda e: e.matmul(pq[:, 0:1], lhsT=sqb[:, c * 128:(c + 1) * 128], rhs=ones_col, start=(c == 0), stop=(c == 3)),
                          reads=[d_sqb, d_consts], writes=[d_pq])
                rb, d_rb = sspool.get()
                kb.op("dve", lambda e: e.tensor_scalar(out=rb[:, 0:1], in0=pq[:, 0:1], scalar1=1.0 / 512, scalar2=EPS, op0=ALU.mult, op1=ALU.add),
                      reads=[d_pq], writes=[d_rb])
                kb.op("act", lambda e: e.sqrt(out=rb[:, 0:1], in_=rb[:, 0:1]), reads=[d_rb], writes=[d_rb])
                kb.op("dve", lambda e: e.reciprocal(out=rb[:, 0:1], in_=rb[:, 0:1]), reads=[d_rb], writes=[d_rb])
                ybg, d_ybg = ybgpool.get()
                for c in range(4):
                    kb.op("act", lambda e: e.activation(out=ybg[:, c * 128:(c + 1) * 128], in_=ybT[:, c, :], func=AF.Copy, scale=gb[:, c:c + 1]),
                          reads=[d_ybT, d_gb], writes=[d_ybg])
                preps[tb] = (xt, d_xt, yaT, d_yaT, ybg, d_ybg, rb, d_rb)

            def d_main(tb):
                rows = slice(tb * 128, (tb + 1) * 128)
                xt, d_xt, yaT, d_yaT, ybg, d_ybg, rb, d_rb = preps.pop(tb)
                xn, d_xn = xnpool.get()
                for half in range(2):
                    hs = slice(half * 512, (half + 1) * 512)
                    pA, d_pA = psum8.get()
                    for c in range(4):
                        kb.op("pe", lambda e: e.matmul(pA[:], lhsT=yaT[:, c * 128:(c + 1) * 128], rhs=wo[:, c, hs], start=(c == 0), stop=(c == 3)),
                              reads=[d_yaT, d_wo[c]], writes=[d_pA])
                    pB, d_pB = psum8.get()
                    for c in range(4):
                        kb.op("pe", lambda e: e.matmul(pB[:], lhsT=ybg[:, c * 128:(c + 1) * 128], rhs=wo[:, 4 + c, hs], start=(c == 0), stop=(c == 3)),
                              reads=[d_ybg, d_wo[4 + c]], writes=[d_pB])
                    pas, d_pas = mainpool.get()
                    kb.op("act", lambda e: e.copy(out=pas[:], in_=pA[:]), reads=[d_pA], writes=[d_pas])
                    mix, d_mix = mainpool.get()
                    kb.op("dve", lambda e: e.scalar_tensor_tensor(out=mix[:], in0=pB[:], scalar=rb[:, 0:1], in1=pas[:], op0=ALU.mult, op1=ALU.add),
                          reads=[d_pB, d_rb, d_pas], writes=[d_mix])
                    kb.op("dve", lambda e: e.tensor_tensor(out=mix[:], in0=mix[:], in1=g1[:, hs], op=ALU.mult), reads=[d_mix, d_mod], writes=[d_mix])
                    kb.op("dve", lambda e: e.tensor_tensor(out=xn[:, hs], in0=xt[:, hs], in1=mix[:], op=ALU.add), reads=[d_mix, d_xt], writes=[d_xn])
                kb.dma("pool", lambda q: q.dma_start(out=xs[rows, :], in_=xn[:]), reads=[d_xn])

            d_load(0)
            if NT > 1:
                d_load(1)
            d_prep(0)
            for tb in range(NT):
                if tb + 2 < NT:
                    d_load(tb + 2)
                d_main(tb)
                if tb + 1 < NT:
                    d_prep(tb + 1)
            kb.barrier()

        if "E" in phases:
          with ExitStack() as es:
            NB, CH, NDVE = 8, 4, 0
            NCH = 128 // CH
            kT = sbt(es, "pk", [128, 2, 128]); d_kT = Dep()
            kb.dma("sp", lambda q: q.dma_start(out=kT[:, 0, :], in_=peer_k1T[l]), writes=[d_kT])
            kb.dma("sp", lambda q: q.dma_start(out=kT[:, 1, :], in_=peer_k2T[l]), writes=[d_kT])
            fn_bc = None
            if l == L - 1:
                fn_bc = sbt(es, "fnbc", [128, D]); d_fn = Dep()
                kb.dma("sp", lambda q: q.dma_start(out=fn_bc[:], in_=final_norm.partition_broadcast(128)), writes=[d_fn])
            xpool = Pool(es, nc, "ex", [128, D], F32, 2)
            hpool = Pool(es, nc, "eh", [128, D], F32, 2)
            junk = sbt(es, "ejunk", [128, D]); d_junk = Dep()
            sspool = Pool(es, nc, "ess", [128, 8], F32, 6)
            hTpool = Pool(es, nc, "ehT", [128, 8, 128], F32, 1)
            wqpool = Pool(es, nc, "ewq", [128, 8, 256], F32, 2)
            qTpool = Pool(es, nc, "eqT", [128, 4, 128], F32, 2)
            scpool = Pool(es, nc, "esc", [128, 4, 128], F32, 2)
            wkpool = Pool(es, nc, "ewk", [128, 128], F32, 2)
            v16pool = Pool(es, nc, "ev16", [128, 16, 16], F32, 2)
            i16pool = Pool(es, nc, "ei16", [128, 16, 16], U32, 2)
            i16fpool = Pool(es, nc, "ei16f", [128, 16, 16], F32, 1)
            candpool = Pool(es, nc, "ecand", [128, 16, 16], F32, 2)
            cidpool = Pool(es, nc, "ecid", [128, 16, 16], F32, 2)
            wk2pool = Pool(es, nc, "ewk2", [128, 256], F32, 2)
            j2pool = Pool(es, nc, "ej2", [128, 256], F32, 1)
            tspool = Pool(es, nc, "ets", [128, 8, 16], F32, 2)
            eidfpool = Pool(es, nc, "eeidf", [128, 128], F32, 2)
            eidipool = Pool(es, nc, "eeidi", [128, 128], I32, 2)
            gpool = Pool(es, nc, "eg", [128, 8, 16], F32, 2)
            uvpool = Pool(es, nc, "fuv", [128, 2 * D], F32, NB)
            tmppool = Pool(es, nc, "ftmp", [128, D], F32, 3)
            actpool = Pool(es, nc, "fact", [128, 128], F32, 2)
            coefpool = Pool(es, nc, "fcoef", [128, 128], F32, 2)
            accpool = Pool(es, nc, "facc", [128, D], F32, 2)
            blocks = {}
            chunkdeps = {}

            def p1_gen(tb):
                rows = slice(tb * 128, (tb + 1) * 128)
                xt, d_xt = xpool.get()
                kb.dma("sp", lambda q: q.dma_start(out=xt[:], in_=xs[rows, :]), writes=[d_xt])
                h, d_h = hpool.get()
                norm_mod(es, xt, d_xt, A2, sh2, sspool, junk, d_junk, h, d_h)
                hT, d_hT = hTpool.get()
                transpose_to(h, d_h, 8, lambda c: hT[:, c, :], d_hT)
                v16, d_v16 = v16pool.get()
                i16, d_i16 = i16pool.get()
                yield
                for j0 in range(0, 16, 4):
                    pt, d_pt = psum.get()
                    for jp in range(j0, j0 + 4, 2):
                        wqt, d_wqt = wqpool.get()
                        kb.dma("sp", lambda q: q.dma_start(out=wqt[:], in_=peer_wq[l][:, jp * 128:(jp + 2) * 128].rearrange("(kc p) n -> p kc n", p=128)),
                               writes=[d_wqt])
                        for j in (jp, jp + 1):
                            for kc in range(8):
                                kb.op("pe", lambda e: e.matmul(pt[:, (j - j0) * 128:(j - j0 + 1) * 128], lhsT=wqt[:, kc, (j - jp) * 128:(j - jp + 1) * 128],
                                                               rhs=hT[:, kc, :], start=(kc == 0), stop=(kc == 7)), reads=[d_hT, d_wqt], writes=[d_pt])
                    qT4, d_qT4 = qTpool.get()
                    kb.op("act", lambda e: e.copy(out=qT4[:].rearrange("p a b -> p (a b)"), in_=pt[:]), reads=[d_pt], writes=[d_qT4])
                    psc, d_psc = psum.get()
                    for j in range(j0, j0 + 4):
                        kb.op("pe", lambda e: e.matmul(psc[:, (j - j0) * 128:(j - j0 + 1) * 128], lhsT=qT4[:, j - j0, :], rhs=kT[:, j % 2, :], start=True, stop=True),
                              reads=[d_qT4, d_kT], writes=[d_psc])
                    sc4, d_sc4 = scpool.get()
                    kb.op("act", lambda e: e.copy(out=sc4[:].rearrange("p a b -> p (a b)"), in_=psc[:]), reads=[d_psc], writes=[d_sc4])
                    yield
                    for j in range(j0, j0 + 4):
                        scj = sc4[:, j - j0, :]
                        wk, d_wk = wkpool.get()
                        kb.op("dve", lambda e: e.max(out=v16[:, j, 0:8], in_=scj), reads=[d_sc4], writes=[d_v16])
                        kb.op("dve", lambda e: e.max_index(out=i16[:, j, 0:8], in_max=v16[:, j, 0:8], in_values=scj), reads=[d_sc4, d_v16], writes=[d_i16])
                        kb.op("dve", lambda e: e.match_replace(out=wk[:], in_to_replace=v16[:, j, 0:8], in_values=scj, imm_value=-1e30),
                              reads=[d_sc4, d_v16], writes=[d_wk])
                        kb.op("dve", lambda e: e.max(out=v16[:, j, 8:16], in_=wk[:]), reads=[d_wk], writes=[d_v16])
                        kb.op("dve", lambda e: e.max_index(out=i16[:, j, 8:16], in_max=v16[:, j, 8:16], in_values=wk[:]), reads=[d_wk, d_v16], writes=[d_i16])
                    yield
                i16f, d_i16f = i16fpool.get()
                kb.op("dve", lambda e: e.tensor_copy(out=i16f[:], in_=i16[:]), reads=[d_i16], writes=[d_i16f])
                ts, d_ts = tspool.get()
                eidf, d_eidf = eidfpool.get()
                kb.op("dve", lambda e: e.memset(eidf[:], 0.0), writes=[d_eidf])
                for hd in range(8):
                    j1, j2 = 2 * hd, 2 * hd + 1
                    cand, d_cand = candpool.get()
                    cid, d_cid = cidpool.get()
                    kb.op("dve", lambda e: e.tensor_tensor(out=cand[:], in0=v16[:, j1, :].unsqueeze(2).to_broadcast([128, 16, 16]),
                                                           in1=v16[:, j2:j2 + 1, :].to_broadcast([128, 16, 16]), op=ALU.add),
                          reads=[d_v16], writes=[d_cand])
                    kb.op("dve", lambda e: e.scalar_tensor_tensor(out=cid[:], in0=i16f[:, j1, :].unsqueeze(2).to_broadcast([128, 16, 16]), scalar=128.0,
                                                                  in1=i16f[:, j2:j2 + 1, :].to_broadcast([128, 16, 16]), op0=ALU.mult, op1=ALU.add),
                          reads=[d_i16f], writes=[d_cid])
                    candf = cand[:].rearrange("p a b -> p (a b)")
                    cidf = cid[:].rearrange("p a b -> p (a b)")
                    wk2, d_wk2 = wk2pool.get()
                    kb.op("dve", lambda e: e.max(out=ts[:, hd, 0:8], in_=candf), reads=[d_cand], writes=[d_ts])
                    kb.op("dve", lambda e: e.match_replace(out=wk2[:], in_to_replace=ts[:, hd, 0:8], in_values=candf, imm_value=-1e30),
                          reads=[d_cand, d_ts], writes=[d_wk2])
                    kb.op("dve", lambda e: e.max(out=ts[:, hd, 8:16], in_=wk2[:]), reads=[d_wk2], writes=[d_ts])
                    j2t, d_j2t = j2pool.get()
                    for sl in range(16):
                        kb.op("dve", lambda e: e.scalar_tensor_tensor(out=j2t[:], in0=candf, scalar=ts[:, hd, sl:sl + 1], in1=cidf, op0=ALU.is_equal, op1=ALU.mult,
                                                                      accum_out=eidf[:, hd * 16 + sl:hd * 16 + sl + 1]),
                              reads=[d_cand, d_cid, d_ts], writes=[d_j2t, d_eidf])
                        if sl % 8 == 7:
                            yield
                eidi, d_eidi = eidipool.get()
                kb.op("dve", lambda e: e.tensor_scalar(out=eidf[:], in0=eidf[:], scalar1=float(NEXP - 1), scalar2=float(l * NEXP), op0=ALU.min, op1=ALU.add),
                      reads=[d_eidf], writes=[d_eidf])
                kb.op("dve", lambda e: e.tensor_copy(out=eidi[:], in_=eidf[:]), reads=[d_eidf], writes=[d_eidi])
                gt, d_gt = gpool.get()
                kb.op("dve", lambda e: e.tensor_tensor(out=gt[:], in0=ts[:], in1=ts[:, :, 0:1].to_broadcast([128, 8, 16]), op=ALU.subtract),
                      reads=[d_ts], writes=[d_gt])
                kb.op("act", lambda e: e.activation(out=gt[:], in_=gt[:], func=AF.Exp), reads=[d_gt], writes=[d_gt])
                sm, d_sm = sspool.get()
                kb.op("dve", lambda e: e.tensor_reduce(out=sm[:], in_=gt[:], axis=AX.X, op=ALU.add), reads=[d_gt], writes=[d_sm])
                kb.op("dve", lambda e: e.reciprocal(out=sm[:], in_=sm[:]), reads=[d_sm], writes=[d_sm])
                kb.op("dve", lambda e: e.tensor_tensor(out=gt[:], in0=gt[:], in1=sm[:].unsqueeze(2).to_broadcast([128, 8, 16]), op=ALU.mult),
                      reads=[d_gt, d_sm], writes=[d_gt])
                blocks[tb] = dict(xt=xt, d_xt=d_xt, h=h, d_h=d_h, eid=eidi, d_eid=d_eidi, gt=gt[:].rearrange("p a b -> p (a b)"), d_gt=d_gt, bufs={})

            def gathers(bk, c):
                for j in range(c * CH, (c + 1) * CH):
                    uv, d_uv = uvpool.get()
                    kb.dma("pool", lambda q: q.indirect_dma_start(out=uv[:], out_offset=None, in_=peer_uv,
                                                                  in_offset=bass.IndirectOffsetOnAxis(ap=bk["eid"][:, j:j + 1], axis=0)),
                           reads=[bk["d_eid"]], writes=[d_uv])
                    bk["bufs"][j] = (uv, d_uv)

            for _ in p1_gen(0):
                pass
            gathers(blocks[0], 0)
            for tb in range(NT):
                rows = slice(tb * 128, (tb + 1) * 128)
                cur = blocks.pop(tb)
                gen = p1_gen(tb + 1) if tb + 1 < NT else iter(())
                xt, d_xt, gt, d_gt, h, d_h = cur["xt"], cur["d_xt"], cur["gt"], cur["d_gt"], cur["h"], cur["d_h"]
                act, _ = actpool.get()
                coef, _ = coefpool.get()
                d_actc = chunkdeps.setdefault(id(act), [Dep() for _ in range(NCH)])
                d_coefc = chunkdeps.setdefault(id(coef), [Dep() for _ in range(NCH)])
                acc, d_acc = accpool.get()
                pa0, d_pa0 = psum_acc.get()
                pa1, d_pa1 = psum_acc.get()
                state = {"pe": 0, "dve": 0}

                def dots(c):
                    cs_ = slice(c * CH, (c + 1) * CH)
                    kb.op("dve", lambda e: e.memset(act[:, cs_], 0.0), reads=[], writes=[d_actc[c]])
                    for j in range(c * CH, (c + 1) * CH):
                        uv, d_uv = cur["bufs"][j]
                        kb.op("dve", lambda e: e.scalar_tensor_tensor(out=junk[:], in0=uv[:, 0:D], scalar=1.0, in1=h[:], op0=ALU.mult, op1=ALU.mult,
                                                                      accum_out=act[:, j:j + 1]), reads=[d_uv, d_h], writes=[d_junk, d_actc[c]])
                    kb.op("act", lambda e: e.activation(out=coef[:, cs_], in_=act[:, cs_], func=AF.Gelu_apprx_tanh), reads=[d_actc[c]], writes=[d_coefc[c]])
                    kb.op("dve", lambda e: e.tensor_tensor(out=coef[:, cs_], in0=coef[:, cs_], in1=gt[:, cs_], op=ALU.mult),
                          reads=[d_coefc[c], d_gt], writes=[d_coefc[c]])

                def vside(c):
                    for jj, j in enumerate(range(c * CH, (c + 1) * CH)):
                        uv, d_uv = cur["bufs"].pop(j)
                        if jj < NDVE:
                            if state["dve"] == 0:
                                kb.op("dve", lambda e: e.tensor_scalar(out=acc[:], in0=uv[:, D:2 * D], scalar1=coef[:, j:j + 1], scalar2=None, op0=ALU.mult),
                                      reads=[d_uv, d_coefc[c]], writes=[d_acc])
                            else:
                                kb.op("dve", lambda e: e.scalar_tensor_tensor(out=acc[:], in0=uv[:, D:2 * D], scalar=coef[:, j:j + 1], in1=acc[:],
                                                                              op0=ALU.mult, op1=ALU.add), reads=[d_uv, d_coefc[c], d_acc], writes=[d_acc])
                            state["dve"] += 1
                        else:
                            tmp, d_tmp = tmppool.get()
                            kb.op("act", lambda e: e.activation(out=tmp[:], in_=uv[:, D:2 * D], func=AF.Copy, scale=coef[:, j:j + 1]),
                                  reads=[d_uv, d_coefc[c]], writes=[d_tmp])
                            first = state["pe"] == 0
                            last = (j == 127)
                            kb.op("pe", lambda e: e.matmul(pa0[:], lhsT=ident, rhs=tmp[:, 0:512], start=first, stop=last), reads=[d_tmp, d_consts], writes=[d_pa0])
                            kb.op("pe", lambda e: e.matmul(pa1[:], lhsT=ident, rhs=tmp[:, 512:1024], start=first, stop=last), reads=[d_tmp, d_consts], writes=[d_pa1])
                            state["pe"] += 1

                for c in range(NCH):
                    if c + 1 < NCH:
                        gathers(cur, c + 1)
                    else:
                        for _ in gen:
                            pass
                        if tb + 1 < NT:
                            gathers(blocks[tb + 1], 0)
                    dots(c)
                    next(gen, None)
                    vside(c)
                if NDVE > 0:
                    kb.op("dve", lambda e: e.tensor_tensor(out=acc[:, 0:512], in0=pa0[:], in1=acc[:, 0:512], op=ALU.add), reads=[d_pa0, d_acc], writes=[d_acc])
                    kb.op("dve", lambda e: e.tensor_tensor(out=acc[:, 512:1024], in0=pa1[:], in1=acc[:, 512:1024], op=ALU.add), reads=[d_pa1, d_acc], writes=[d_acc])
                    kb.op("dve", lambda e: e.tensor_tensor(out=acc[:], in0=acc[:], in1=g2, op=ALU.mult), reads=[d_acc, d_mod], writes=[d_acc])
                else:
                    kb.op("dve", lambda e: e.tensor_tensor(out=acc[:, 0:512], in0=pa0[:], in1=g2[:, 0:512], op=ALU.mult), reads=[d_pa0, d_mod], writes=[d_acc])
                    kb.op("dve", lambda e: e.tensor_tensor(out=acc[:, 512:1024], in0=pa1[:], in1=g2[:, 512:1024], op=ALU.mult), reads=[d_pa1, d_mod], writes=[d_acc])
                kb.op("dve", lambda e: e.tensor_tensor(out=acc[:], in0=acc[:], in1=xt[:], op=ALU.add), reads=[d_acc, d_xt], writes=[d_acc])
                if l == L - 1:
                    rs, d_rs = rms_scale(es, acc[:], d_acc, D, sspool, junk[:], d_junk)
                    kb.op("dve", lambda e: e.scalar_tensor_tensor(out=acc[:], in0=acc[:], scalar=rs[:, 0:1], in1=fn_bc[:], op0=ALU.mult, op1=ALU.mult),
                          reads=[d_acc, d_rs, d_fn], writes=[d_acc])
                    kb.dma("sp", lambda q: q.dma_start(out=out[rows, :], in_=acc[:]), reads=[d_acc])
                else:
                    kb.dma("sp", lambda q: q.dma_start(out=xs[rows, :], in_=acc[:]), reads=[d_acc])
            kb.barrier()

    if dbg:
        for name, src, shape, dt in (("d_xs", xs, [S, D], F32), ("d_qT", qT_s, [512, S], F32), ("d_kT", kT_s, [512, S], F32),
                                     ("d_v", v_s, [S, 512], F32), ("d_ya", ya_s, [S, 512], F32), ("d_ybT", ybT_s, [512, S], F32),
                                     ):
            o = nc.dram_tensor(name, shape, dt, kind="ExternalOutput").ap()
            kb.dma("sp", lambda q: q.dma_start(out=o, in_=src))
        kb.barrier()
    print("instructions", kb.nins, "waits", kb.nwait)
    return nc


def prep_inputs(inp, b, L):
    f = lambda a: np.ascontiguousarray(a, dtype=np.float32)
    m = {
        "x": f(inp["x"][b]),
        "c": f(inp["c"][b].reshape(8, 128).T),
        "ada_w": f(inp["ada_w"][:L]),
        "ada_b": f(inp["ada_b"][:L]),
        "norm_mix": f(inp["norm_mix"][:L]),
        "norm_ffn": f(inp["norm_ffn"][:L]),
        "w_in": f(inp["w_in"][:L]),
        "gm_wsT": f(np.transpose(inp["gm_ws"][:L], (0, 3, 1, 2))),
        "gm_bsT": f(np.transpose(inp["gm_bs"][:L], (0, 2, 1))),
        "gm_vnorm": f(inp["gm_vnorm"][:L]),
        "out_norm_a": f(inp["out_norm_a"][:L]),
        "out_norm_bT": f(np.transpose(inp["out_norm_b"][:L].reshape(L, 4, 128), (0, 2, 1))),
        "w_out": f(inp["w_out"][:L]),
        "peer_wq": f(inp["peer_wq"][:L]),
        "peer_k1T": f(np.transpose(inp["peer_k1"][:L], (0, 2, 1))),
        "peer_k2T": f(np.transpose(inp["peer_k2"][:L], (0, 2, 1))),
        "peer_uv": np.concatenate([f(inp["peer_u"][:L]).reshape(L * NEXP, D), f(inp["peer_v"][:L]).reshape(L * NEXP, D)], axis=1),
        "final_norm": f(inp["final_norm"]),
        "consts": make_consts(),
    }
    return m


def kernel(**inputs):
    inp = {k: np.asarray(v) for k, v in inputs.items()}
    B, S, _ = inp["x"].shape
    L = inp["ada_w"].shape[0]
    nc = build(L, S)
    shared = prep_inputs(inp, 0, L)
    in_maps = []
    for b in range(B):
        m = dict(shared)
        m["x"] = np.ascontiguousarray(inp["x"][b], dtype=np.float32)
        m["c"] = np.ascontiguousarray(inp["c"][b].reshape(8, 128).T, dtype=np.float32)
        in_maps.append(m)
    res = run_bass_kernel_spmd(nc, in_maps, core_ids=list(range(B)))
    return np.stack([r["out"] for r in res.results], axis=0).astype(np.float32)
```
